# Optimizing a Trainium2 kernel written in Bass

```python
import jax, jax.numpy as jnp
from jax import lax
import numpy as np

D_MODEL = 1024
BATCH = 32
SEQ = 2048
DEPTH = 4

HEAD_DIM = 64
MLA_HEADS = 8
MLA_Q_RANK = 256
MLA_KV_RANK = 128
MLA_NOPE = 64
MLA_ROPE = 32
MLA_V = 64
DSA_HEADS = 4
IDX_HEADS = 8
IDX_DIM = 32
DSA_TOPK_MAX = 256
DIL_CONFIGS = ((128, 1), (512, 4), (2048, 16))
DIL_HEADS = 4
MIX_WIDTH = (MLA_HEADS + DSA_HEADS + DIL_HEADS) * HEAD_DIM
ROPE_THETA = 500000.0
ROT_HEAD_DIMS = HEAD_DIM // 4
ROT_IDX_DIMS = IDX_DIM // 4
Q_BLOCK = 128
N_GROUPS = 4
EXPERTS_PER_GROUP = 8
TOP_K_IN_GROUP = 2
EXPERT_HIDDEN = 256
DEEPNORM_ALPHA = (2.0 * DEPTH) ** 0.25
DEEPNORM_BETA = (8.0 * DEPTH) ** -0.25
LN_EPS = 1e-5
RMS_EPS = 1e-6
NEG_INF = -1e30
IN_SPLITS = (MLA_Q_RANK, MLA_KV_RANK, MLA_ROPE,
             DSA_HEADS * HEAD_DIM, HEAD_DIM, HEAD_DIM,
             IDX_HEADS * IDX_DIM, IDX_DIM, IDX_HEADS,
             3 * len(DIL_CONFIGS) * DIL_HEADS * HEAD_DIM)
IN_COLS = sum(IN_SPLITS)

kernel_name = "hybrid_mla_dsa_dilated_hmoe_deepnorm"


def layer_norm(x, g, b):
    xf = x.astype(jnp.float32)
    mu = jnp.mean(xf, -1, keepdims=True)
    var = jnp.mean(jnp.square(xf - mu), -1, keepdims=True)
    return ((xf - mu) * lax.rsqrt(var + LN_EPS) * g.astype(jnp.float32) + b.astype(jnp.float32)).astype(x.dtype)


def rms_norm(x, g):
    xf = x.astype(jnp.float32)
    y = xf * lax.rsqrt(jnp.mean(jnp.square(xf), -1, keepdims=True) + RMS_EPS)
    return (y * g.astype(jnp.float32)).astype(x.dtype)


def rope_table(positions, rot_dims):
    inv = ROPE_THETA ** (-jnp.arange(0, rot_dims, 2, dtype=jnp.float32) / rot_dims)
    ang = positions.astype(jnp.float32)[..., None] * inv
    return jnp.cos(ang), jnp.sin(ang)


def apply_rope(x, cos, sin):
    half = cos.shape[-1]
    r = 2 * half
    c, s = cos[:, :, None, :], sin[:, :, None, :]
    xf = x[..., :r].astype(jnp.float32)
    x1, x2 = xf[..., :half], xf[..., half:]
    rot = jnp.concatenate([x1 * c - x2 * s, x2 * c + x1 * s], -1).astype(x.dtype)
    return jnp.concatenate([rot, x[..., r:]], -1)


def _to_blocks(a):
    B, S = a.shape[:2]
    return jnp.moveaxis(a.reshape(B, S // Q_BLOCK, Q_BLOCK, *a.shape[2:]), 1, 0)


def _from_blocks(a):
    nb, B, qb = a.shape[:3]
    return jnp.moveaxis(a, 0, 1).reshape(B, nb * qb, *a.shape[3:])


def _block_starts(S):
    return jnp.arange(S // Q_BLOCK, dtype=jnp.int32) * Q_BLOCK


def mla_mixer(cq, ckv, kpe, q_norm, kv_norm, w_uq, w_ukv, cos, sin):
    B, S = cq.shape[:2]
    q = jnp.einsum('bsr,rc->bsc', rms_norm(cq, q_norm), w_uq).reshape(B, S, MLA_HEADS, MLA_NOPE + MLA_ROPE)
    q = jnp.concatenate([q[..., :MLA_NOPE], apply_rope(q[..., MLA_NOPE:], cos, sin)], -1)
    kv = jnp.einsum('bsr,rc->bsc', rms_norm(ckv, kv_norm), w_ukv).reshape(B, S, MLA_HEADS, MLA_NOPE + MLA_V)
    k_pe = apply_rope(kpe[:, :, None, :], cos, sin)
    k = jnp.concatenate([kv[..., :MLA_NOPE], jnp.broadcast_to(k_pe, (B, S, MLA_HEADS, MLA_ROPE))], -1)
    v = kv[..., MLA_NOPE:]
    scale = (MLA_NOPE + MLA_ROPE) ** -0.5
    key_pos = jnp.arange(S, dtype=jnp.int32)

    def block(args):
        q0, qb = args
        qpos = q0 + jnp.arange(Q_BLOCK, dtype=jnp.int32)
        s = jnp.einsum('bqhd,bkhd->bhqk', qb, k, preferred_element_type=jnp.float32) * scale
        s = jnp.where(key_pos[None, :] <= qpos[:, None], s, NEG_INF)
        p = jax.nn.softmax(s, -1).astype(v.dtype)
        return jnp.einsum('bhqk,bkhd->bqhd', p, v)

    o = _from_blocks(lax.map(block, (_block_starts(S), _to_blocks(q))))
    return o.reshape(B, S, MLA_HEADS * MLA_V)


def dsa_mixer(q, k, v, qi, ki, wi, cos_h, sin_h, cos_i, sin_i):
    B, S = q.shape[:2]
    q = apply_rope(q.reshape(B, S, DSA_HEADS, HEAD_DIM), cos_h, sin_h)
    k = apply_rope(k[:, :, None, :], cos_h, sin_h)[:, :, 0]
    qi = apply_rope(qi.reshape(B, S, IDX_HEADS, IDX_DIM), cos_i, sin_i)
    ki = apply_rope(ki[:, :, None, :], cos_i, sin_i)[:, :, 0]
    top_k = min(DSA_TOPK_MAX, S // 4)
    key_pos = jnp.arange(S, dtype=jnp.int32)
    gather = jax.vmap(lambda arr, idx: arr[idx])
    idx_scale = IDX_DIM ** -0.5 * IDX_HEADS ** -0.5

    def block(args):
        q0, qb, qib, wb = args
        qpos = q0 + jnp.arange(Q_BLOCK, dtype=jnp.int32)
        rel = jax.nn.relu(jnp.einsum('bqhd,bsd->bqhs', qib, ki, preferred_element_type=jnp.float32))
        score = jnp.einsum('bqhs,bqh->bqs', rel, wb.astype(jnp.float32)) * idx_scale
        score = jnp.where(key_pos[None, None, :] <= qpos[None, :, None], score, NEG_INF)
        _, sel = lax.top_k(score, top_k)
        k_sel = gather(k, sel)
        v_sel = gather(v, sel)
        valid = sel <= qpos[None, :, None]
        s = jnp.einsum('bqhd,bqkd->bhqk', qb, k_sel, preferred_element_type=jnp.float32) * HEAD_DIM ** -0.5
        s = jnp.where(valid[:, None], s, NEG_INF)
        p = jax.nn.softmax(s, -1).astype(v.dtype)
        return jnp.einsum('bhqk,bqkd->bqhd', p, v_sel)

    o = _from_blocks(lax.map(block, (_block_starts(S), _to_blocks(q), _to_blocks(qi), _to_blocks(wi))))
    return o.reshape(B, S, DSA_HEADS * HEAD_DIM)


def dilated_window_attention(q, k, v, window, dilation):
    S = q.shape[1]
    n_keys = window // dilation + 1
    offs = dilation * jnp.arange(n_keys, dtype=jnp.int32)

    def block(args):
        q0, qb = args
        qpos = q0 + jnp.arange(Q_BLOCK, dtype=jnp.int32)
        idx = qpos[:, None] - offs[None, :]
        valid = idx >= 0
        idx = jnp.maximum(idx, 0)
        k_sel = k[:, idx]
        v_sel = v[:, idx]
        s = jnp.einsum('bqhd,bqkhd->bqhk', qb, k_sel, preferred_element_type=jnp.float32) * HEAD_DIM ** -0.5
        s = jnp.where(valid[None, :, None, :], s, NEG_INF)
        lse = jax.nn.logsumexp(s, -1)
        p = jnp.exp(s - lse[..., None]).astype(v.dtype)
        return jnp.einsum('bqhk,bqkhd->bqhd', p, v_sel), lse

    o, lse = lax.map(block, (_block_starts(S), _to_blocks(q)))
    return _from_blocks(o), _from_blocks(lse)


def dilated_mixer(qkv, cos, sin):
    B, S = qkv.shape[:2]
    n_cfg = len(DIL_CONFIGS)
    qkv = qkv.reshape(B, S, 3, n_cfg * DIL_HEADS, HEAD_DIM)
    q = apply_rope(qkv[:, :, 0], cos, sin)
    k = apply_rope(qkv[:, :, 1], cos, sin)
    v = qkv[:, :, 2]
    outs, lses = [], []
    for g, (window, dilation) in enumerate(DIL_CONFIGS):
        sl = slice(g * DIL_HEADS, (g + 1) * DIL_HEADS)
        o, lse = dilated_window_attention(q[:, :, sl], k[:, :, sl], v[:, :, sl], window, dilation)
        outs.append(o)
        lses.append(lse)
    wts = jax.nn.softmax(jnp.stack(lses, 0), axis=0)
    o = jnp.sum(wts[..., None] * jnp.stack(outs, 0).astype(jnp.float32), 0).astype(qkv.dtype)
    return o.reshape(B, S, DIL_HEADS * HEAD_DIM)


def hybrid_mixer(x, tabs, w_in, q_norm, kv_norm, w_uq, w_ukv, w_o):
    (cos_m, sin_m), (cos_h, sin_h), (cos_i, sin_i) = tabs
    u = jnp.einsum('bsd,dc->bsc', x, w_in)
    cuts = [int(c) for c in np.cumsum(IN_SPLITS)[:-1]]
    a_cq, a_ckv, a_kpe, b_q, b_k, b_v, b_qi, b_ki, b_wi, c_qkv = jnp.split(u, cuts, axis=-1)
    o_a = mla_mixer(a_cq, a_ckv, a_kpe, q_norm, kv_norm, w_uq, w_ukv, cos_m, sin_m)
    o_b = dsa_mixer(b_q, b_k, b_v, b_qi, b_ki, b_wi, cos_h, sin_h, cos_i, sin_i)
    o_c = dilated_mixer(c_qkv, cos_h, sin_h)
    o = jnp.concatenate([o_a, o_b, o_c], -1)
    return jnp.einsum('bsc,cd->bsd', o, w_o)


def hier_moe(x, rg_w, rg_b, re_w, re_b, w_gate, w_up, w_down):
    B, S, D = x.shape
    t = x.reshape(B * S, D)
    lg = jnp.einsum('td,dg->tg', t, rg_w, preferred_element_type=jnp.float32) + rg_b.astype(jnp.float32)
    g_star = jnp.argmax(lg, -1)
    g_hot = jax.nn.one_hot(g_star, N_GROUPS, dtype=jnp.float32)
    p_top = jnp.sum(jax.nn.softmax(lg, -1) * g_hot, -1, keepdims=True)
    le = jnp.einsum('td,gde->tge', t, re_w, preferred_element_type=jnp.float32) + re_b.astype(jnp.float32)
    le = jnp.einsum('tge,tg->te', le, g_hot)
    top_v, top_i = lax.top_k(le, TOP_K_IN_GROUP)
    w_sel = jax.nn.softmax(top_v, -1)
    gate_e = jnp.sum(jax.nn.one_hot(top_i, EXPERTS_PER_GROUP, dtype=jnp.float32) * w_sel[..., None], 1)
    gate = (g_hot[:, :, None] * (p_top * gate_e)[:, None, :]).astype(x.dtype)
    y = jnp.zeros_like(t)
    for g in range(N_GROUPS):
        h = jax.nn.silu(jnp.einsum('td,edf->tef', t, w_gate[g])) * jnp.einsum('td,edf->tef', t, w_up[g])
        y = y + jnp.einsum('tef,efd->td', h * gate[:, g, :, None], w_down[g])
    return y.reshape(B, S, D)


def setup_inputs(seed: int = 0) -> dict:
    key = jax.random.key(seed)
    ks = jax.random.split(key, 20)

    def nrm(k, shape, scale):
        return jax.random.normal(k, shape, jnp.float32) * scale

    x = nrm(ks[0], (BATCH, SEQ, D_MODEL), 1.0)
    positions = (jax.random.randint(ks[1], (BATCH, 1), 0, 4096, dtype=jnp.int32)
                 + jnp.arange(SEQ, dtype=jnp.int32)[None, :])
    G, E, F = N_GROUPS, EXPERTS_PER_GROUP, EXPERT_HIDDEN
    return {
        "x": x,
        "positions": positions,
        "w_in": nrm(ks[2], (DEPTH, D_MODEL, IN_COLS), D_MODEL ** -0.5),
        "mla_q_norm": 1.0 + nrm(ks[3], (DEPTH, MLA_Q_RANK), 0.02),
        "mla_kv_norm": 1.0 + nrm(ks[4], (DEPTH, MLA_KV_RANK), 0.02),
        "mla_w_uq": nrm(ks[5], (DEPTH, MLA_Q_RANK, MLA_HEADS * (MLA_NOPE + MLA_ROPE)), MLA_Q_RANK ** -0.5),
        "mla_w_ukv": nrm(ks[6], (DEPTH, MLA_KV_RANK, MLA_HEADS * (MLA_NOPE + MLA_V)), MLA_KV_RANK ** -0.5),
        "w_o": nrm(ks[7], (DEPTH, MIX_WIDTH, D_MODEL), MIX_WIDTH ** -0.5 * DEEPNORM_BETA),
        "ln1_g": 1.0 + nrm(ks[8], (DEPTH, D_MODEL), 0.02),
        "ln1_b": nrm(ks[9], (DEPTH, D_MODEL), 0.02),
        "router_group_w": nrm(ks[10], (DEPTH, D_MODEL, G), D_MODEL ** -0.5),
        "router_group_b": nrm(ks[11], (DEPTH, G), 0.01),
        "router_expert_w": nrm(ks[12], (DEPTH, G, D_MODEL, E), D_MODEL ** -0.5),
        "router_expert_b": nrm(ks[13], (DEPTH, G, E), 0.01),
        "expert_w_gate": nrm(ks[14], (DEPTH, G, E, D_MODEL, F), D_MODEL ** -0.5),
        "expert_w_up": nrm(ks[15], (DEPTH, G, E, D_MODEL, F), D_MODEL ** -0.5),
        "expert_w_down": nrm(ks[16], (DEPTH, G, E, F, D_MODEL), F ** -0.5 * DEEPNORM_BETA),
        "ln2_g": 1.0 + nrm(ks[17], (DEPTH, D_MODEL), 0.02),
        "ln2_b": nrm(ks[18], (DEPTH, D_MODEL), 0.02),
    }


def reference(x, positions, w_in, mla_q_norm, mla_kv_norm, mla_w_uq, mla_w_ukv, w_o, ln1_g, ln1_b,
              router_group_w, router_group_b, router_expert_w, router_expert_b,
              expert_w_gate, expert_w_up, expert_w_down, ln2_g, ln2_b):
    tabs = (rope_table(positions, MLA_ROPE),
            rope_table(positions, ROT_HEAD_DIMS),
            rope_table(positions, ROT_IDX_DIMS))
    for l in range(DEPTH):
        mix = hybrid_mixer(x, tabs, w_in[l], mla_q_norm[l], mla_kv_norm[l], mla_w_uq[l], mla_w_ukv[l], w_o[l])
        x = layer_norm(DEEPNORM_ALPHA * x + mix, ln1_g[l], ln1_b[l])
        moe = hier_moe(x, router_group_w[l], router_group_b[l], router_expert_w[l], router_expert_b[l],
                       expert_w_gate[l], expert_w_up[l], expert_w_down[l])
        x = layer_norm(DEEPNORM_ALPHA * x + moe, ln2_g[l], ln2_b[l])
    return x
```

```python
import contextlib
import numpy as np
import ml_dtypes
import concourse.bass as bass
import concourse.mybir as mybir
from concourse.bass_utils import run_bass_kernel_spmd

F32 = mybir.dt.float32
BF16 = mybir.dt.bfloat16
I32 = mybir.dt.int32
ALU = mybir.AluOpType
AF = mybir.ActivationFunctionType
AX = mybir.AxisListType

D = 1024
S = 2048
NT = S // 128
DEPTH = 4
INC = 3400
ALPHA = float((2.0 * DEPTH) ** 0.25)
LN_EPS = 1e-5
RMS_EPS = 1e-6
TWO_PI = 6.283185307179586
C1 = 6.28125
C2 = TWO_PI - C1
MAGIC = 12582912.0
NBIS = 22


class Track:
    __slots__ = ("w", "r", "dsem", "dcnt")

    def __init__(self):
        self.w = {}
        self.r = {}
        self.dsem = None
        self.dcnt = 0


class Eng:
    def __init__(self, name, e, sem):
        self.name = name
        self.e = e
        self.sem = sem
        self.n = 0
        self.seen = {}


class Buf:
    def __init__(self, t):
        self.t = t
        self.tr = Track()
        self.sub = {}

    def k(self, key):
        if key not in self.sub:
            self.sub[key] = Track()
        return self.sub[key]

    def all(self):
        return [self.tr] + list(self.sub.values())

    def __getitem__(self, idx):
        return self.t[idx]


class KB:
    def __init__(self):
        self.nc = bass.Bass("TRN2", target_bir_lowering=False)
        self.es = contextlib.ExitStack()
        nc = self.nc
        self.E = {}
        for name, e in (("pe", nc.tensor), ("act", nc.scalar), ("dve", nc.vector),
                        ("pool", nc.gpsimd), ("sp", nc.sync)):
            sem = self.es.enter_context(nc.semaphore("sem_" + name))
            self.E[name] = Eng(name, e, sem)
        self.dsems = []
        self.ninst = 0
        self.pool_ds = []
        self.pool_i = 0

    def sbuf(self, name, shape, dtype, es=None):
        self.uid = getattr(self, "uid", 0) + 1
        return Buf((es or self.es).enter_context(self.nc.sbuf_tensor("sb%d_%s" % (self.uid, name), list(shape), dtype)))

    def psum(self, name, shape, dtype):
        return Buf(self.es.enter_context(self.nc.psum_tensor(name, list(shape), dtype)))

    def dram(self, name, shape, dtype, kind="Internal"):
        return Buf(self.nc.dram_tensor(name, list(shape), dtype, kind=kind))

    def _need(self, E, sem, val, owner, raw):
        if owner is E and (not raw or E.name == "pe"):
            return
        if E.seen.get(sem, 0) >= val:
            return
        E.e.wait_ge(sem, val)
        E.seen[sem] = val

    def _deps(self, E, R, W, Wa):
        for t in R:
            for sem, (val, owner) in t.w.items():
                self._need(E, sem, val, owner, True)
        for t in W:
            for sem, (val, owner) in t.w.items():
                self._need(E, sem, val, owner, False)
            for sem, (val, owner) in t.r.items():
                self._need(E, sem, val, owner, False)
        for t in Wa:
            for sem, (val, owner) in t.r.items():
                self._need(E, sem, val, owner, False)

    def _post(self, sem, val, owner, R, W, Wa):
        for t in R:
            t.r[sem] = (val, owner)
        for t in W:
            t.w = {sem: (val, owner)}
            t.r = {}
        for t in Wa:
            t.w[sem] = (val, owner)

    def op(self, eng, fn, R=(), W=(), Wa=()):
        E = self.E[eng]
        self._deps(E, R, W, Wa)
        inst = fn(E.e)
        E.n += 1
        inst.then_inc(E.sem, 1)
        self._post(E.sem, E.n, E, R, W, Wa)
        self.ninst += 1
        return inst

    def dma(self, q, out_ap, in_ap, R=(), W=(), Wa=(), **kw):
        E = self.E[q]
        self._deps(E, R, W, Wa)
        tgt = (list(W) + list(Wa))[0]
        if tgt.dsem is None:
            if len(self.pool_ds) < 40:
                ds = [self.es.enter_context(self.nc.semaphore("ds%d" % len(self.dsems))), 0]
                self.pool_ds.append(ds)
                self.dsems.append(ds)
            else:
                ds = self.pool_ds[self.pool_i % 40]
                self.pool_i += 1
            tgt.dsem = ds
        ds = tgt.dsem
        inst = E.e.dma_start(out=out_ap, in_=in_ap, **kw)
        ds[1] += 16
        inst.then_inc(ds[0], 16)
        self._post(ds[0], ds[1], None, R, W, Wa)
        self.ninst += 1
        return inst

    def dedicate(self, track):
        ds = [self.es.enter_context(self.nc.semaphore("dd%d" % len(self.dsems))), 0]
        self.dsems.append(ds)
        track.dsem = ds

    def barrier(self):
        for E in self.E.values():
            for E2 in self.E.values():
                if E2 is not E and E2.n > 0:
                    self._need(E, E2.sem, E2.n, E2, True)
            for ds in self.dsems:
                if ds[1] > 0:
                    self._need(E, ds[0], ds[1], None, True)

    def finish(self, out_tracks):
        E = self.E["sp"]
        for t in out_tracks:
            for sem, (val, owner) in t.w.items():
                self._need(E, sem, val, owner, True)
        self.barrier()


def host_consts():
    c = {}
    c["ident"] = np.eye(128, dtype=np.float32).astype(ml_dtypes.bfloat16)
    k = np.arange(128)[:, None]
    q = np.arange(128)[None, :]
    dm = np.zeros((128, 8, 128), np.float32)
    dm[:, 0, :] = (k <= q)
    dm[:, 1, :] = (q <= k)
    dm[:, 2, :] = ((q - k) % 4 == 0) & (q >= k)
    dm[:, 3, :] = ((q - k) % 4 == 0)
    dm[:, 4, :] = ((q - k) % 4 == 0) & (q <= k)
    dm[:, 5, :] = ((q - k) % 16 == 0) & (q >= k)
    dm[:, 6, :] = ((q - k) % 16 == 0)
    dm[:, 7, :] = 1.0
    c["dmask"] = dm.astype(ml_dtypes.bfloat16)
    qq = np.arange(128)[:, None]
    kk = np.arange(128)[None, :]
    c["tribias"] = np.where(kk <= qq, 0.0, -1e30).astype(np.float32)
    inv = []
    for rot in (32, 16, 8):
        inv.append((500000.0 ** (-np.arange(0, rot, 2, dtype=np.float32) / np.float32(rot))).astype(np.float32))
    inv = np.concatenate(inv).astype(np.float32)
    c["inv"] = np.tile(inv[None, :], (128, 1)).astype(np.float32)
    p = np.arange(128)
    sel = np.zeros((128, 8, 128), np.float32)
    for g in range(8):
        sel[p, g, 16 * g + (p % 16)] = 1.0
    c["selmask"] = sel.astype(ml_dtypes.bfloat16)
    rt = np.zeros((32, 128), np.float32)
    rt[p // 16, p] = 1.0
    c["rt"] = rt.astype(ml_dtypes.bfloat16)
    return c


WNAMES = ["w_in", "mla_q_norm", "mla_kv_norm", "mla_w_uq", "mla_w_ukv", "w_o", "ln1_g", "ln1_b",
          "router_group_w", "router_group_b", "router_expert_w", "router_expert_b",
          "expert_w_gate", "expert_w_up", "expert_w_down", "ln2_g", "ln2_b"]


class _Stop(Exception):
    pass


def build(nseq, layers, wshapes, taps=None, stop_after=None):
    kb = KB()
    nc = kb.nc
    taps = taps or []
    x_in = kb.dram("x", [nseq, S, D], F32, kind="ExternalInput")
    pos_in = kb.dram("positions", [nseq, S], I32, kind="ExternalInput")
    y_out = kb.dram("y", [nseq, S, D], F32, kind="ExternalOutput")
    Wd_ = {n: kb.dram(n, wshapes[n], F32, kind="ExternalInput") for n in WNAMES}
    hc = host_consts()
    Cd = {n: kb.dram("c_" + n, list(v.shape), BF16 if v.dtype != np.float32 else F32, kind="ExternalInput")
          for n, v in hc.items()}
    tapd = {}
    nl = len(layers)
    xs = [kb.dram("xs%d" % i, [nseq, S, D], F32) for i in range(2)] if nl > 1 else []
    u_d = kb.dram("u_d", [S, INC], F32)

    ident = kb.sbuf("ident", [128, 128], BF16)
    dmask = kb.sbuf("dmask", [128, 8, 128], BF16)
    tribias = kb.sbuf("tribias", [128, 128], F32)
    inv = kb.sbuf("inv", [128, 28], F32)
    selmask = kb.sbuf("selmask", [128, 8, 128], BF16)
    rt = kb.sbuf("rt", [32, 128], BF16)
    cosT = kb.sbuf("cosT", [128, nseq * NT, 28], F32)
    sinT = kb.sbuf("sinT", [128, nseq * NT, 28], F32)
    for b_, n in ((ident, "ident"), (dmask, "dmask"), (tribias, "tribias"), (inv, "inv"),
                  (selmask, "selmask"), (rt, "rt")):
        kb.dma("sp", b_.t[:], Cd[n].t[:], R=[Cd[n].tr], W=[b_.tr])

    PS = [kb.psum("ps%d" % i, [128, 1024], F32) for i in range(4)]

    def psb(i, h):
        return PS[i].t[:, h * 512:(h + 1) * 512], PS[i].k(h)

    def psbf(i, h):
        return PS[i].t.bitcast(BF16)[:, h * 1024:(h + 1) * 1024], PS[i].k(h)

    with contextlib.ExitStack() as es:
        posi = kb.sbuf("posi", [128, nseq * NT], I32, es)
        posf = kb.sbuf("posf", [128, nseq * NT], F32, es)
        ang = kb.sbuf("ang", [128, nseq * NT, 28], F32, es)
        tt = kb.sbuf("rp_t", [128, nseq * NT, 28], F32, es)
        kf = kb.sbuf("rp_k", [128, nseq * NT, 28], F32, es)
        rr = kb.sbuf("rp_r", [128, nseq * NT, 28], F32, es)
        kb.dma("sp", posi.t[:].rearrange("p (s t) -> p s t", s=nseq),
               pos_in.t[:].rearrange("s (t p) -> p s t", p=128), R=[pos_in.tr], W=[posi.tr],
               allow_slow_non_contiguous=True)
        kb.op("dve", lambda e: e.tensor_copy(out=posf.t[:], in_=posi.t[:]), R=[posi.tr], W=[posf.tr])
        nt_all = nseq * NT
        kb.op("dve", lambda e: e.tensor_tensor(
            out=ang.t[:], in0=posf.t[:].unsqueeze(2).to_broadcast([128, nt_all, 28]),
            in1=inv.t[:].unsqueeze(1).to_broadcast([128, nt_all, 28]), op=ALU.mult),
            R=[posf.tr, inv.tr], W=[ang.tr])
        for dst, shift in ((sinT, 0.0), (cosT, float(np.pi / 2))):
            kb.op("dve", lambda e: e.tensor_scalar(out=tt.t[:], in0=ang.t[:], scalar1=shift, scalar2=float(1.0 / TWO_PI),
                                                   op0=ALU.add, op1=ALU.mult), R=[ang.tr], W=[tt.tr])
            kb.op("dve", lambda e: e.tensor_scalar(out=kf.t[:], in0=tt.t[:], scalar1=MAGIC, scalar2=-MAGIC,
                                                   op0=ALU.add, op1=ALU.add), R=[tt.tr], W=[kf.tr])
            kb.op("dve", lambda e: e.scalar_tensor_tensor(out=rr.t[:], in0=kf.t[:], scalar=-C1, in1=ang.t[:],
                                                          op0=ALU.mult, op1=ALU.add), R=[kf.tr, ang.tr], W=[rr.tr])
            kb.op("dve", lambda e: e.scalar_tensor_tensor(out=tt.t[:], in0=kf.t[:], scalar=-C2, in1=rr.t[:],
                                                          op0=ALU.mult, op1=ALU.add), R=[kf.tr, rr.tr], W=[tt.tr])
            kb.op("dve", lambda e: e.tensor_scalar(out=rr.t[:], in0=tt.t[:], scalar1=shift, scalar2=None,
                                                   op0=ALU.add), R=[tt.tr], W=[rr.tr])
            kb.op("dve", lambda e: e.tensor_scalar(out=tt.t[:], in0=rr.t[:], scalar1=3.1415925, scalar2=-3.1415925,
                                                   op0=ALU.min, op1=ALU.max), R=[rr.tr], W=[tt.tr])
            kb.op("act", lambda e: e.activation(out=dst.t[:], in_=tt.t[:], func=AF.Sin), R=[tt.tr], W=[dst.tr])
        kb.barrier()

    def rope(eng, src, dst, col0, nh, hd, half, cs, sn, tmp1, tmp2, R, Wt):
        def v(buf, off):
            return buf.t[:, col0:col0 + nh * hd].rearrange("p (h d) -> p h d", h=nh)[:, :, off:off + half]
        cb = cs.unsqueeze(1).to_broadcast([128, nh, half])
        sb = sn.unsqueeze(1).to_broadcast([128, nh, half])
        t1 = tmp1.t[:, 0:nh * half].rearrange("p (h d) -> p h d", h=nh)
        t2 = tmp2.t[:, 0:nh * half].rearrange("p (h d) -> p h d", h=nh)
        x1, x2 = v(src, 0), v(src, half)
        o1, o2 = v(dst, 0), v(dst, half)
        kb.op(eng, lambda e: e.tensor_tensor(out=t1, in0=x1, in1=cb, op=ALU.mult), R=R, W=[tmp1.tr])
        kb.op(eng, lambda e: e.tensor_tensor(out=t2, in0=x2, in1=sb, op=ALU.mult), R=R, W=[tmp2.tr])
        kb.op(eng, lambda e: e.tensor_tensor(out=o1, in0=t1, in1=t2, op=ALU.subtract), R=[tmp1.tr, tmp2.tr], Wa=Wt)
        kb.op(eng, lambda e: e.tensor_tensor(out=t1, in0=x2, in1=cb, op=ALU.mult), R=R, W=[tmp1.tr])
        kb.op(eng, lambda e: e.tensor_tensor(out=t2, in0=x1, in1=sb, op=ALU.mult), R=R, W=[tmp2.tr])
        kb.op(eng, lambda e: e.tensor_tensor(out=o2, in0=t1, in1=t2, op=ALU.add), R=[tmp1.tr, tmp2.tr], Wa=Wt)

    def rope2(eng, sv, dv, nh, half, cs, sn, tmp1, tmp2, R, Wt):
        cb_ = cs.unsqueeze(1).to_broadcast([128, nh, half])
        sb_ = sn.unsqueeze(1).to_broadcast([128, nh, half])
        t1 = tmp1.t[:, 0:nh * half].rearrange("p (h d) -> p h d", h=nh)
        t2 = tmp2.t[:, 0:nh * half].rearrange("p (h d) -> p h d", h=nh)
        x1, x2 = sv[:, :, 0:half], sv[:, :, half:2 * half]
        o1, o2 = dv[:, :, 0:half], dv[:, :, half:2 * half]
        kb.op(eng, lambda e: e.tensor_tensor(out=t1, in0=x1, in1=cb_, op=ALU.mult), R=R, W=[tmp1.tr])
        kb.op(eng, lambda e: e.tensor_tensor(out=t2, in0=x2, in1=sb_, op=ALU.mult), R=R, W=[tmp2.tr])
        kb.op(eng, lambda e: e.tensor_tensor(out=o1, in0=t1, in1=t2, op=ALU.subtract), R=[tmp1.tr, tmp2.tr], Wa=Wt)
        kb.op(eng, lambda e: e.tensor_tensor(out=t1, in0=x2, in1=cb_, op=ALU.mult), R=R, W=[tmp1.tr])
        kb.op(eng, lambda e: e.tensor_tensor(out=t2, in0=x1, in1=sb_, op=ALU.mult), R=R, W=[tmp2.tr])
        kb.op(eng, lambda e: e.tensor_tensor(out=o2, in0=t1, in1=t2, op=ALU.add), R=[tmp1.tr, tmp2.tr], Wa=Wt)

    def tap(name, ap, tracks, shape, dtype):
        if name in taps:
            d = kb.dram("tap_" + name, list(shape), dtype, kind="ExternalOutput")
            tapd[name] = d
            n0 = shape[0]
            for r0 in range(0, n0, 128):
                r1 = min(n0, r0 + 128)
                kb.dma("sp", d.t[r0:r1], ap[r0:r1], R=tracks, Wa=[d.tr])

    out_tracks = []
    kb.dedicate(u_d.tr)
    O_d = kb.dram("O_d", [S, D], BF16)
    kb.dedicate(O_d.tr)
    for xx in xs:
        for s_ in range(nseq):
            kb.dedicate(xx.k(s_))
    for s_ in range(nseq):
        kb.dedicate(y_out.k(s_))

    def mm(out, lhsT, rhs, start, stop, R, pt, first):
        kb.op("pe", lambda e: e.matmul(out, lhsT=lhsT, rhs=rhs, start=start, stop=stop),
              R=R, W=[pt] if first else (), Wa=() if first else [pt])

    def tp(out, in_, R, pt, first):
        kb.op("pe", lambda e: e.transpose(out, in_, ident.t[:]),
              R=list(R) + [ident.tr], W=[pt] if first else (), Wa=() if first else [pt])

    def attention(es, nheads, nch_fn, lhs_fn, rhs_fn, v_fn, scale, mask_fn, Ost, ocol0, Rops):
        if isinstance(es, tuple):
            Pb, rden = es
        else:
            Pb = [kb.sbuf("Pb%d" % i, [128, 512], BF16, es) for i in range(2)]
            rden = kb.sbuf("rden", [128, 4], F32, es)
        it = 0
        for h in range(nheads):
            for c in (nch_fn if nch_fn is not None else range(4)):
                accv, acct = psb(1, (h * 4 + c) % 2)
                acc3 = accv[:, 0:260].rearrange("p (j d) -> p j d", j=4)
                for kb_ in range(4 * c + 4):
                    j0 = max(0, kb_ - 4 * c)
                    ncol = 512 - 128 * j0
                    sv_, st_ = psb(0, it % 2)
                    P = Pb[it % 2]
                    mm(sv_[:, 0:ncol], lhs_fn(h, kb_), rhs_fn(h, c * 512 + 128 * j0, (c + 1) * 512), True, True, Rops, st_, True)
                    kb.op("act", lambda e: e.activation(out=P.t[:, 0:ncol], in_=sv_[:, 0:ncol], func=AF.Exp, scale=scale),
                          R=[st_], W=[P.tr])
                    mk = mask_fn(c, kb_, j0)
                    if mk is not None:
                        map_, mtr, mw = mk
                        kb.op("dve", lambda e: e.tensor_tensor(out=P.t[:, 0:mw], in0=P.t[:, 0:mw], in1=map_, op=ALU.mult),
                              R=[P.tr] + mtr, W=[P.tr])
                    for j in range(j0, 4):
                        mm(acc3[:, j, :], P.t[:, (j - j0) * 128:(j - j0 + 1) * 128], v_fn(h, kb_), (kb_ == 0 and j == 0), (kb_ == 4 * c + 3 and j == 3),
                           [P.tr] + Rops, acct, (kb_ == 0 and j == 0))
                    it += 1
                kb.op("dve", lambda e: e.reciprocal(out=rden.t[:, 0:4], in_=acc3[:, :, 64]), R=[acct], W=[rden.tr])
                kb.op("dve", lambda e: e.tensor_tensor(
                    out=Ost.t[:, 4 * c:4 * c + 4, ocol0 + h * 64:ocol0 + (h + 1) * 64], in0=acc3[:, :, 0:64],
                    in1=rden.t[:, 0:4].unsqueeze(2).to_broadcast([128, 4, 64]), op=ALU.mult),
                    R=[acct, rden.tr], Wa=[Ost.tr])

    def STOP(name):
        if stop_after == name:
            kb.barrier()
            raise _Stop()

    def body():
      for li, l in enumerate(layers):
        x_src = x_in if li == 0 else xs[(li - 1) % 2]
        x_dst = y_out if li == nl - 1 else xs[li % 2]
        W = {n: Wd_[n].t[l] for n in WNAMES}
        Wtr = {n: Wd_[n].tr for n in WNAMES}
        for s in range(nseq):
            tb = s * NT
            with contextlib.ExitStack() as es:
                w_in = kb.sbuf("w_in", [128, 8, INC], BF16, es)
                for kc in range(8):
                    kb.dma("pool", w_in.t[:, kc, :], W["w_in"][kc * 128:(kc + 1) * 128, :], R=[Wtr["w_in"]],
                           Wa=[w_in.k(kc)], max_dma_last_dim=4096)
                xts = [kb.sbuf("xt%d" % i, [128, D], F32, es) for i in range(2)]
                xbs = [kb.sbuf("xb%d" % i, [128, D], BF16, es) for i in range(2)]
                xTs = [kb.sbuf("xT%d" % i, [128, 8, 128], BF16, es) for i in range(2)]
                u32s = [kb.sbuf("u32_%d" % i, [128, INC], F32, es) for i in range(2)]
                for t in range(NT):
                    xt, xb, xT, u32 = xts[t % 2], xbs[t % 2], xTs[t % 2], u32s[t % 2]
                    kb.dma("sp", xt.t[:], x_src.t[s, t * 128:(t + 1) * 128, :], R=[x_src.k(s)], W=[xt.tr])
                    kb.op("dve", lambda e: e.tensor_copy(out=xb.t[:], in_=xt.t[:]), R=[xt.tr], W=[xb.tr])
                    pa, pt = psbf(0, t % 2)
                    for kc in range(8):
                        tp(pa[:, kc * 128:(kc + 1) * 128], xb.t[:, kc * 128:(kc + 1) * 128], [xb.tr], pt, kc == 0)
                    kb.op("act", lambda e: e.copy(out=xT.t[:].rearrange("p k t -> p (k t)"), in_=pa), R=[pt], W=[xT.tr])
                    c0 = 0
                    gi = 0
                    while c0 < INC:
                        cw = min(512, INC - c0)
                        pm, pmt = psb(1 + gi % 2, (gi // 2) % 2)
                        for kc in range(8):
                            mm(pm[:, 0:cw], xT.t[:, kc, :], w_in.t[:, kc, c0:c0 + cw], kc == 0, kc == 7,
                               [xT.tr, w_in.k(kc)], pmt, kc == 0)
                        if gi % 2 == 0:
                            kb.op("act", lambda e: e.copy(out=u32.t[:, c0:c0 + cw], in_=pm[:, 0:cw]), R=[pmt], Wa=[u32.tr])
                        else:
                            kb.op("dve", lambda e: e.tensor_copy(out=u32.t[:, c0:c0 + cw], in_=pm[:, 0:cw]), R=[pmt], Wa=[u32.tr])
                        c0 += cw
                        gi += 1
                    kb.dma("sp", u_d.t[t * 128:(t + 1) * 128, :], u32.t[:], R=[u32.tr], Wa=[u_d.tr])
                kb.barrier()
            if li == 0 and s == 0:
                tap("u", u_d.t[:], [u_d.tr], [S, INC], F32)
            STOP("P1")

            with contextlib.ExitStack() as es:
                w_uq = kb.sbuf("w_uq", [128, 2, 768], BF16, es)
                w_ukv = kb.sbuf("w_ukv", [128, 1024], BF16, es)
                stg = kb.sbuf("stg", [128, 1024], F32, es)
                qn = kb.sbuf("qn", [128, 3], F32, es)
                kb.dma("sp", qn.t[:, 0:2], W["mla_q_norm"].rearrange("(k p) -> p k", p=128), R=[Wtr["mla_q_norm"]], W=[qn.tr],
                       allow_slow_non_contiguous=True)
                kb.dma("sp", qn.t[:, 2:3], W["mla_kv_norm"].rearrange("(p o) -> p o", o=1), R=[Wtr["mla_kv_norm"]], Wa=[qn.tr])
                STOP("P2a0")
                for kc in range(2):
                    kb.dma("sp", stg.t[:, 0:768], W["mla_w_uq"][kc * 128:(kc + 1) * 128, :], R=[Wtr["mla_w_uq"]], W=[stg.tr])
                    kb.op("dve", lambda e: e.tensor_scalar(out=w_uq.t[:, kc, :], in0=stg.t[:, 0:768], scalar1=qn.t[:, kc:kc + 1],
                                                           scalar2=None, op0=ALU.mult), R=[stg.tr, qn.tr], Wa=[w_uq.tr])
                kb.dma("sp", stg.t[:, :], W["mla_w_ukv"][:, :], R=[Wtr["mla_w_ukv"]], W=[stg.tr])
                kb.op("dve", lambda e: e.tensor_scalar(out=w_ukv.t[:], in0=stg.t[:], scalar1=qn.t[:, 2:3], scalar2=None,
                                                       op0=ALU.mult), R=[stg.tr, qn.tr], W=[w_ukv.tr])
                STOP("P2a")
                QT = kb.sbuf("QT", [96, 8, S], BF16, es)
                KT = kb.sbuf("KT", [96, 8, S], BF16, es)
                VA = kb.sbuf("VA", [128, NT, 8, 65], BF16, es)
                Ost = kb.sbuf("Ost", [128, NT, 512], BF16, es)
                kb.op("dve", lambda e: e.memset(VA.t[:].rearrange("p a b c -> p (a b c)"), 1.0), W=[VA.tr])
                uus = [kb.sbuf("uu%d" % i, [128, 416], F32, es) for i in range(2)]
                cbs = [kb.sbuf("cb%d" % i, [128, 480], BF16, es) for i in range(2)]
                for cb in cbs:
                    kb.op("dve", lambda e: e.memset(cb.t[:], 0.0), W=[cb.tr])
                junk = kb.sbuf("junk", [128, 384], F32, es)
                ssq = kb.sbuf("ssq", [128, 2], F32, es)
                rs = kb.sbuf("rs", [128, 2], F32, es)
                rs2 = kb.sbuf("rs2", [128, 2], F32, es)
                rstd = kb.sbuf("rstd", [128, 2], F32, es)
                tm1 = kb.sbuf("tm1", [128, 256], F32, es)
                tm2 = kb.sbuf("tm2", [128, 256], F32, es)
                cT = kb.sbuf("cT", [128, 384], BF16, es)
                kp = kb.sbuf("kp", [128, 128], BF16, es)
                q32 = kb.sbuf("q32", [128, 768], F32, es)
                qb = kb.sbuf("qb", [128, 768], BF16, es)
                for t in range(NT):
                    uu, cb = uus[t % 2], cbs[t % 2]
                    cm, sm = cosT.t[:, tb + t, 0:16], sinT.t[:, tb + t, 0:16]
                    kb.dma("sp", uu.t[:], u_d.t[t * 128:(t + 1) * 128, 0:416], R=[u_d.tr], W=[uu.tr])
                    kb.op("act", lambda e: e.activation(out=junk.t[:, 0:384], in_=uu.t[:, 0:384], func=AF.Square),
                          R=[uu.tr], W=[junk.tr])
                    kb.op("dve", lambda e: e.reduce_sum(out=ssq.t[:, 0:1], in_=junk.t[:, 0:256], axis=AX.X), R=[junk.tr], W=[ssq.tr])
                    kb.op("dve", lambda e: e.reduce_sum(out=ssq.t[:, 1:2], in_=junk.t[:, 256:384], axis=AX.X), R=[junk.tr], Wa=[ssq.tr])
                    kb.op("dve", lambda e: e.tensor_scalar(out=rs.t[:, 0:1], in0=ssq.t[:, 0:1], scalar1=1.0 / 256, scalar2=RMS_EPS,
                                                           op0=ALU.mult, op1=ALU.add), R=[ssq.tr], W=[rs.tr])
                    kb.op("dve", lambda e: e.tensor_scalar(out=rs.t[:, 1:2], in0=ssq.t[:, 1:2], scalar1=1.0 / 128, scalar2=RMS_EPS,
                                                           op0=ALU.mult, op1=ALU.add), R=[ssq.tr], Wa=[rs.tr])
                    if t == 0:
                        STOP("P2b1")
                    kb.op("act", lambda e: e.activation(out=rs2.t[:], in_=rs.t[:], func=AF.Sqrt), R=[rs.tr], W=[rs2.tr])
                    kb.op("dve", lambda e: e.reciprocal(out=rstd.t[:], in_=rs2.t[:]), R=[rs2.tr], W=[rstd.tr])
                    kb.op("dve", lambda e: e.tensor_scalar(out=cb.t[:, 0:256], in0=uu.t[:, 0:256], scalar1=rstd.t[:, 0:1], scalar2=None,
                                                           op0=ALU.mult), R=[uu.tr, rstd.tr], W=[cb.tr])
                    kb.op("dve", lambda e: e.tensor_scalar(out=cb.t[:, 256:384], in0=uu.t[:, 256:384], scalar1=rstd.t[:, 1:2], scalar2=None,
                                                           op0=ALU.mult), R=[uu.tr, rstd.tr], Wa=[cb.tr])
                    if t == 0:
                        STOP("P2b3")
                    rope2("dve", uu.t[:, 384:416].rearrange("p (h d) -> p h d", h=1), cb.t[:, 448:480].rearrange("p (h d) -> p h d", h=1),
                          1, 16, cm, sm, tm1, tm2, [uu.tr], [cb.tr])
                    if t == 0:
                        STOP("P2b4")
                    pa, pt = psbf(0, 0)
                    tp(pa[:, 0:128], cb.t[:, 0:128], [cb.tr], pt, True)
                    tp(pa[:, 128:256], cb.t[:, 128:256], [cb.tr], pt, False)
                    tp(pa[:, 256:384], cb.t[:, 256:384], [cb.tr], pt, False)
                    tp(pa[0:96, 384:512], cb.t[:, 384:480], [cb.tr], pt, False)
                    kb.op("act", lambda e: e.copy(out=cT.t[:], in_=pa[:, 0:384]), R=[pt], W=[cT.tr])
                    if t == 0:
                        STOP("P2b6")
                    kb.op("act", lambda e: e.copy(out=kp.t[64:96, :], in_=pa[64:96, 384:512]), R=[pt], W=[kp.tr])
                    for h in range(8):
                        kb.op("dve", lambda e: e.tensor_copy(out=KT.t[64:96, h, t * 128:(t + 1) * 128], in_=kp.t[64:96, :]),
                              R=[kp.tr], Wa=[KT.tr])
                    if t == 0:
                        STOP("P2b")
                    for half, cw in ((0, 512), (1, 256)):
                        pq, pqt = psb(1, half)
                        for kc in range(2):
                            mm(pq[:, 0:cw], cT.t[:, kc * 128:(kc + 1) * 128], w_uq.t[:, kc, half * 512:half * 512 + cw], kc == 0, kc == 1,
                               [cT.tr, w_uq.tr], pqt, kc == 0)
                        kb.op("act", lambda e: e.copy(out=q32.t[:, half * 512:half * 512 + cw], in_=pq[:, 0:cw]), R=[pqt],
                              W=[q32.tr] if half == 0 else (), Wa=[q32.tr] if half else ())
                    kb.op("dve", lambda e: e.tensor_copy(out=qb.t[:], in_=q32.t[:]), R=[q32.tr], W=[qb.tr])
                    rope2("dve", q32.t[:].rearrange("p (h d) -> p h d", h=8)[:, :, 64:96],
                          qb.t[:].rearrange("p (h d) -> p h d", h=8)[:, :, 64:96], 8, 16, cm, sm, tm1, tm2, [q32.tr], [qb.tr])
                    pa2, pt2 = psbf(0, 1)
                    for h in range(8):
                        tp(pa2[0:96, h * 128:(h + 1) * 128], qb.t[:, h * 96:(h + 1) * 96], [qb.tr], pt2, h == 0)
                    kb.op("act", lambda e: e.copy(out=QT.t[0:96, :, t * 128:(t + 1) * 128],
                                                  in_=pa2[0:96, :].rearrange("p (h t) -> p h t", h=8)), R=[pt2], Wa=[QT.tr])
                    if t == 0:
                        STOP("P2c")
                    for hb in range(2):
                        pk, pkt = psb(2, hb)
                        for hh in range(4):
                            h = hb * 4 + hh
                            mm(pk[0:64, hh * 128:(hh + 1) * 128], w_ukv.t[:, h * 128:h * 128 + 64], cT.t[:, 256:384], True, True,
                               [cT.tr, w_ukv.tr], pkt, hh == 0)
                        kb.op("dve", lambda e: e.tensor_copy(out=KT.t[0:64, hb * 4:hb * 4 + 4, t * 128:(t + 1) * 128],
                                                             in_=pk[0:64, :].rearrange("p (h t) -> p h t", h=4)), R=[pkt], Wa=[KT.tr])
                    pv, pvt = psb(3, 0)
                    for h in range(8):
                        mm(pv[:, h * 64:(h + 1) * 64], cT.t[:, 256:384], w_ukv.t[:, h * 128 + 64:h * 128 + 128], True, True,
                           [cT.tr, w_ukv.tr], pvt, h == 0)
                    kb.op("act", lambda e: e.copy(out=VA.t[:, t, :, 0:64], in_=pv[:, 0:512].rearrange("p (h d) -> p h d", h=8)),
                          R=[pvt], Wa=[VA.tr])
                STOP("P2d")

                def m_mask(c, kb_, j0):
                    if kb_ >= 4 * c:
                        return dmask.t[:, 0, :], [dmask.tr], 128
                    return None
                attention(es, 8, None, lambda h, kb_: KT.t[0:96, h, kb_ * 128:(kb_ + 1) * 128],
                          lambda h, q0, q1: QT.t[0:96, h, q0:q1], lambda h, kb_: VA.t[:, kb_, h, :],
                          float(96 ** -0.5), m_mask, Ost, 0, [QT.tr, KT.tr, VA.tr])
                for t in range(NT):
                    kb.dma("sp", O_d.t[t * 128:(t + 1) * 128, 0:512], Ost.t[:, t, :], R=[Ost.tr], Wa=[O_d.tr])
                kb.barrier()
            if li == 0 and s == 0:
                tap("O_mla", O_d.t[:, 0:512], [O_d.tr], [S, 512], BF16)
            STOP("P2")

            with contextlib.ExitStack() as es:
                qTd = kb.sbuf("qTd", [64, 4, S], BF16, es)
                kTd = kb.sbuf("kTd", [64, S], BF16, es)
                VAd = kb.sbuf("VAd", [128, NT, 65], BF16, es)
                Osd = kb.sbuf("Osd", [128, NT, 256], BF16, es)
                offs = {}
                o_ = 0
                for b in range(2, NT):
                    offs[b] = o_
                    o_ += 128 * (b + 1)
                SC = kb.sbuf("SC", [128, o_], F32, es)
                lo = kb.sbuf("bs_lo", [128, 14], F32, es)
                hi = kb.sbuf("bs_hi", [128, 14], F32, es)
                wv = kb.sbuf("bs_w", [128, 14], F32, es)
                mid = kb.sbuf("bs_mid", [128, 14], F32, es)
                cnt = kb.sbuf("bs_cnt", [128, 14], F32, es)
                gef = kb.sbuf("bs_ge", [128, 14], F32, es)
                kb.op("dve", lambda e: e.memset(VAd.t[:].rearrange("p a b -> p (a b)"), 1.0), W=[VAd.tr])
                with contextlib.ExitStack() as es2:
                    QIT = kb.sbuf("QIT", [32, 8, S], BF16, es2)
                    kiT = kb.sbuf("kiT", [32, S], BF16, es2)
                    wiT = kb.sbuf("wiT", [32, NT, 128], BF16, es2)
                    kb.op("dve", lambda e: e.memset(wiT.t[:].rearrange("p a b -> p (a b)"), 0.0), W=[wiT.tr])
                    uds = [kb.sbuf("ud%d" % i_, [128, 680], F32, es2) for i_ in range(2)]
                    dbs = [kb.sbuf("db%d" % i_, [128, 680], BF16, es2) for i_ in range(2)]
                    tm1 = kb.sbuf("tm1d", [128, 64], F32, es2)
                    tm2 = kb.sbuf("tm2d", [128, 64], F32, es2)
                    Rg = kb.sbuf("Rg", [128, 8, 512], BF16, es2)
                    Wdb = kb.sbuf("Wdb", [128, 8, 128], BF16, es2)
                    wrep = kb.sbuf("wrep", [128, 128], BF16, es2)
                    ZLb = kb.sbuf("ZLb", [32, 8, 128], BF16, es2)
                    for t in range(NT):
                        ud, db = uds[t % 2], dbs[t % 2]
                        kb.dma("sp", ud.t[:], u_d.t[t * 128:(t + 1) * 128, 416:1096], R=[u_d.tr], W=[ud.tr])
                        kb.op("dve", lambda e: e.tensor_copy(out=db.t[:], in_=ud.t[:]), R=[ud.tr], W=[db.tr])
                        rope2("dve", ud.t[:, 0:320].rearrange("p (h d) -> p h d", h=5), db.t[:, 0:320].rearrange("p (h d) -> p h d", h=5),
                              5, 8, cosT.t[:, tb + t, 16:24], sinT.t[:, tb + t, 16:24], tm1, tm2, [ud.tr], [db.tr])
                        rope2("dve", ud.t[:, 384:672].rearrange("p (h d) -> p h d", h=9), db.t[:, 384:672].rearrange("p (h d) -> p h d", h=9),
                              9, 4, cosT.t[:, tb + t, 24:28], sinT.t[:, tb + t, 24:28], tm1, tm2, [ud.tr], [db.tr])
                        kb.op("act", lambda e: e.copy(out=VAd.t[:, t, 0:64], in_=ud.t[:, 320:384]), R=[ud.tr], Wa=[VAd.tr])
                        pA, ptA = psbf(0, 0)
                        pB, ptB = psbf(0, 1)
                        for h in range(4):
                            tp(pA[0:64, h * 128:(h + 1) * 128], db.t[:, h * 64:(h + 1) * 64], [db.tr], ptA, h == 0)
                        tp(pA[0:64, 512:640], db.t[:, 256:320], [db.tr], ptA, False)
                        tp(pA[0:32, 640:768], db.t[:, 640:672], [db.tr], ptA, False)
                        tp(pA[0:8, 768:896], db.t[:, 672:680], [db.tr], ptA, False)
                        for h in range(8):
                            tp(pB[0:32, h * 128:(h + 1) * 128], db.t[:, 384 + 32 * h:416 + 32 * h], [db.tr], ptB, h == 0)
                        kb.op("act", lambda e: e.copy(out=qTd.t[0:64, :, t * 128:(t + 1) * 128],
                                                      in_=pA[0:64, 0:512].rearrange("p (k t) -> p k t", k=4)), R=[ptA], Wa=[qTd.tr])
                        kb.op("act", lambda e: e.copy(out=kTd.t[0:64, t * 128:(t + 1) * 128], in_=pA[0:64, 512:640]), R=[ptA], Wa=[kTd.tr])
                        kb.op("act", lambda e: e.copy(out=kiT.t[0:32, t * 128:(t + 1) * 128], in_=pA[0:32, 640:768]), R=[ptA], Wa=[kiT.tr])
                        kb.op("act", lambda e: e.copy(out=wiT.t[0:8, t, :], in_=pA[0:8, 768:896]), R=[ptA], Wa=[wiT.tr])
                        kb.op("act", lambda e: e.copy(out=QIT.t[0:32, :, t * 128:(t + 1) * 128],
                                                      in_=pB[0:32, 0:1024].rearrange("p (k t) -> p k t", k=8)), R=[ptB], Wa=[QIT.tr])
                    STOP("P3a")
                    ev = 0
                    for b in range(2, NT):
                        N = 128 * (b + 1)
                        pw, pwt = psb(3, 0)
                        mm(pw[:, 0:128], rt.t[0:32, :], wiT.t[0:32, b, :], True, True, [rt.tr, wiT.tr], pwt, True)
                        kb.op("act", lambda e: e.copy(out=wrep.t[:], in_=pw[:, 0:128]), R=[pwt], W=[wrep.tr])
                        kb.op("dve", lambda e: e.tensor_tensor(out=Wdb.t[:], in0=wrep.t[:].unsqueeze(1).to_broadcast([128, 8, 128]),
                                                               in1=selmask.t[:], op=ALU.mult), R=[wrep.tr, selmask.tr], W=[Wdb.tr])
                        for g in range(8):
                            kb.op("dve", lambda e: e.tensor_copy(
                                out=ZLb.t[0:32, g, :].rearrange("p (h q) -> p h q", h=8),
                                in_=QIT.t[0:32, :, b * 128 + 16 * g:b * 128 + 16 * g + 16]), R=[QIT.tr],
                                W=[ZLb.tr] if g == 0 else (), Wa=[ZLb.tr] if g else ())
                        if b == 2:
                            STOP("P3b0")
                        for kc in range((N + 511) // 512):
                            k0 = kc * 512
                            ncol = min(512, N - k0)
                            for g in range(8):
                                zv, zt_ = psb(0, g % 2)
                                mm(zv[:, 0:ncol], ZLb.t[0:32, g, :], kiT.t[0:32, k0:k0 + ncol], True, True, [ZLb.tr, kiT.tr], zt_, True)
                                if g % 2 == 0:
                                    kb.op("act", lambda e: e.activation(out=Rg.t[:, g, 0:ncol], in_=zv[:, 0:ncol], func=AF.Relu),
                                          R=[zt_], Wa=[Rg.k(g)])
                                else:
                                    kb.op("dve", lambda e: e.tensor_scalar(out=Rg.t[:, g, 0:ncol], in0=zv[:, 0:ncol], scalar1=0.0, scalar2=None,
                                                                           op0=ALU.max), R=[zt_], Wa=[Rg.k(g)])
                            if b == 2 and kc == 0:
                                STOP("P3b1")
                            scv, sct = psb(1, ev % 2)
                            ev += 1
                            for g in range(8):
                                mm(scv[:, 0:ncol], Wdb.t[:, g, :], Rg.t[:, g, 0:ncol], g == 0, g == 7, [Wdb.tr, Rg.k(g)], sct, g == 0)
                            if b == 2 and kc == 0:
                                STOP("P3b2")
                            last = (k0 + ncol == N)
                            nplain = ncol - 128 if last else ncol
                            if nplain > 0:
                                kb.op("act", lambda e: e.copy(out=SC.t[:, offs[b] + k0:offs[b] + k0 + nplain], in_=scv[:, 0:nplain]),
                                      R=[sct], Wa=[SC.k(b)])
                            if b == 2 and kc == 0:
                                STOP("P3b2a")
                            if last:
                                kb.op("act", lambda e: e.copy(out=SC.t[:, offs[b] + N - 128:offs[b] + N], in_=scv[:, ncol - 128:ncol]),
                                      R=[sct], Wa=[SC.k(b)])
                                kb.op("pool", lambda e: e.tensor_tensor(out=SC.t[:, offs[b] + N - 128:offs[b] + N],
                                                                       in0=SC.t[:, offs[b] + N - 128:offs[b] + N],
                                                                       in1=tribias.t[:], op=ALU.add), R=[SC.k(b), tribias.tr], Wa=[SC.k(b)])
                        if b == 2:
                            STOP("P3b3")
                        if b == 4:
                            STOP("P3b4")
                    kb.barrier()
                STOP("P3b")
                junkb = kb.sbuf("junkb", [128, S], BF16, es)
                tmpb = kb.sbuf("bs_tmp", [128, 14], F32, es)
                sct_all = [SC.k(b) for b in range(2, NT)]
                for i_, b in enumerate(range(2, NT)):
                    N = 128 * (b + 1)
                    kb.op("dve", lambda e: e.reduce_max(out=hi.t[:, i_:i_ + 1], in_=SC.t[:, offs[b]:offs[b] + N], axis=AX.X),
                          R=[SC.k(b)], Wa=[hi.tr])
                    kb.op("dve", lambda e: e.tensor_reduce(out=lo.t[:, i_:i_ + 1], in_=SC.t[:, offs[b]:offs[b] + N - 128], axis=AX.X, op=ALU.min),
                          R=[SC.k(b)], Wa=[lo.tr])
                kb.op("dve", lambda e: e.tensor_scalar(out=lo.t[:], in0=lo.t[:], scalar1=-1.0, scalar2=None, op0=ALU.add), R=[lo.tr], W=[lo.tr])
                kb.op("dve", lambda e: e.tensor_scalar(out=hi.t[:], in0=hi.t[:], scalar1=1.0, scalar2=None, op0=ALU.add), R=[hi.tr], W=[hi.tr])
                kb.op("dve", lambda e: e.tensor_tensor(out=wv.t[:], in0=hi.t[:], in1=lo.t[:], op=ALU.subtract), R=[hi.tr, lo.tr], W=[wv.tr])
                for itb in range(NBIS):
                    kb.op("dve", lambda e: e.tensor_scalar(out=wv.t[:], in0=wv.t[:], scalar1=0.5, scalar2=None, op0=ALU.mult), R=[wv.tr], W=[wv.tr])
                    kb.op("dve", lambda e: e.tensor_tensor(out=mid.t[:], in0=lo.t[:], in1=wv.t[:], op=ALU.add), R=[lo.tr, wv.tr], W=[mid.tr])
                    for i_, b in enumerate(range(2, NT)):
                        N = 128 * (b + 1)
                        kb.op("dve", lambda e: e.tensor_scalar(out=junkb.t[:, 0:N], in0=SC.t[:, offs[b]:offs[b] + N], scalar1=mid.t[:, i_:i_ + 1],
                                                               scalar2=None, op0=ALU.is_ge, op1=ALU.add, accum_out=cnt.t[:, i_:i_ + 1]),
                              R=[SC.k(b), mid.tr], W=[junkb.tr], Wa=[cnt.tr])
                    kb.op("dve", lambda e: e.tensor_scalar(out=gef.t[:], in0=cnt.t[:], scalar1=255.5, scalar2=None, op0=ALU.is_ge),
                          R=[cnt.tr], W=[gef.tr])
                    kb.op("dve", lambda e: e.tensor_tensor(out=tmpb.t[:], in0=gef.t[:], in1=wv.t[:], op=ALU.mult), R=[gef.tr, wv.tr], W=[tmpb.tr])
                    kb.op("dve", lambda e: e.tensor_tensor(out=lo.t[:], in0=lo.t[:], in1=tmpb.t[:], op=ALU.add), R=[lo.tr, tmpb.tr], W=[lo.tr])
                STOP("P3c")
                mq = kb.sbuf("mq", [128, S], BF16, es)
                maskT = kb.sbuf("maskT", [128, NT, 512], BF16, es)
                Pb = [kb.sbuf("Pbd%d" % i_, [128, 512], BF16, es) for i_ in range(2)]
                rden = kb.sbuf("rdend", [128, 4], F32, es)
                tq = 0
                for c in range(4):
                    for j in range(4):
                        b = 4 * c + j
                        N = 128 * (b + 1)
                        if b >= 2:
                            kb.op("dve", lambda e: e.tensor_scalar(out=mq.t[:, 0:N], in0=SC.t[:, offs[b]:offs[b] + N], scalar1=lo.t[:, b - 2:b - 1],
                                                                   scalar2=None, op0=ALU.is_ge), R=[SC.k(b), lo.tr], W=[mq.tr])
                            for kb0 in range(0, b + 1, 8):
                                n_ = min(8, b + 1 - kb0)
                                pm_, pmt_ = psbf(2, tq % 2)
                                tq += 1
                                for i_ in range(n_):
                                    tp(pm_[:, i_ * 128:(i_ + 1) * 128], mq.t[:, (kb0 + i_) * 128:(kb0 + i_ + 1) * 128], [mq.tr], pmt_, i_ == 0)
                                kb.op("act", lambda e: e.copy(out=maskT.t[:, kb0:kb0 + n_, j * 128:(j + 1) * 128],
                                                              in_=pm_[:, 0:n_ * 128].rearrange("p (k q) -> p k q", k=n_)), R=[pmt_], Wa=[maskT.tr])
                        else:
                            for kb_ in range(b + 1):
                                kb.op("dve", lambda e: e.tensor_copy(out=maskT.t[:, kb_, j * 128:(j + 1) * 128],
                                                                     in_=dmask.t[:, 0 if kb_ == b else 7, :]), R=[dmask.tr], Wa=[maskT.tr])

                    def d_mask(c_, kb_, j0):
                        return maskT.t[:, kb_, 128 * j0:512], [maskT.tr], 512 - 128 * j0
                    attention((Pb, rden), 4, [c], lambda h, kb_: kTd.t[0:64, kb_ * 128:(kb_ + 1) * 128],
                              lambda h, q0, q1: qTd.t[0:64, h, q0:q1], lambda h, kb_: VAd.t[:, kb_, :],
                              0.125, d_mask, Osd, 0, [qTd.tr, kTd.tr, VAd.tr])
                for t in range(NT):
                    kb.dma("sp", O_d.t[t * 128:(t + 1) * 128, 512:768], Osd.t[:, t, :], R=[Osd.tr], Wa=[O_d.tr])
                kb.barrier()
            if li == 0 and s == 0:
                tap("O_dsa", O_d.t[:, 512:768], [O_d.tr], [S, 256], BF16)
            STOP("P3")

            with contextlib.ExitStack() as es:
                qTc = kb.sbuf("qTc", [64, 12, S], BF16, es)
                kTc = kb.sbuf("kTc", [64, 12, S], BF16, es)
                VAc = kb.sbuf("VAc", [128, NT, 12, 65], BF16, es)
                Osc = kb.sbuf("Osc", [128, NT, 256], BF16, es)
                kb.op("dve", lambda e: e.memset(VAc.t[:].rearrange("p a b c -> p (a b c)"), 1.0), W=[VAc.tr])
                ucs = [kb.sbuf("uc%d" % i_, [128, 2304], F32, es) for i_ in range(2)]
                cbfs = [kb.sbuf("cbf%d" % i_, [128, 1536], BF16, es) for i_ in range(2)]
                tm1 = kb.sbuf("tm1c", [128, 256], F32, es)
                tm2 = kb.sbuf("tm2c", [128, 256], F32, es)
                for t in range(NT):
                    uc, cbf = ucs[t % 2], cbfs[t % 2]
                    ch, sh_ = cosT.t[:, tb + t, 16:24], sinT.t[:, tb + t, 16:24]
                    kb.dma("sp", uc.t[:], u_d.t[t * 128:(t + 1) * 128, 1096:3400], R=[u_d.tr], W=[uc.tr])
                    kb.op("dve", lambda e: e.tensor_copy(out=cbf.t[:], in_=uc.t[:, 0:1536]), R=[uc.tr], W=[cbf.tr])
                    rope2("dve", uc.t[:, 0:1536].rearrange("p (h d) -> p h d", h=24), cbf.t[:].rearrange("p (h d) -> p h d", h=24),
                          24, 8, ch, sh_, tm1, tm2, [uc.tr], [cbf.tr])
                    if t == 0:
                        STOP("P4a1")
                    kb.op("act", lambda e: e.copy(out=VAc.t[:, t, :, 0:64], in_=uc.t[:, 1536:2304].rearrange("p (h d) -> p h d", h=12)),
                          R=[uc.tr], Wa=[VAc.tr])
                    for (src0, dstb, bk) in ((0, qTc, 0), (768, kTc, 2)):
                        pA, ptA = psbf(bk, 0)
                        pB, ptB = psbf(bk, 1)
                        for h in range(12):
                            if h < 8:
                                tp(pA[0:64, h * 128:(h + 1) * 128], cbf.t[:, src0 + h * 64:src0 + (h + 1) * 64], [cbf.tr], ptA, h == 0)
                            else:
                                tp(pB[0:64, (h - 8) * 128:(h - 7) * 128], cbf.t[:, src0 + h * 64:src0 + (h + 1) * 64], [cbf.tr], ptB, h == 8)
                        kb.op("act", lambda e: e.copy(out=dstb.t[0:64, 0:8, t * 128:(t + 1) * 128],
                                                      in_=pA[0:64, 0:1024].rearrange("p (k t) -> p k t", k=8)), R=[ptA], Wa=[dstb.tr])
                        kb.op("act", lambda e: e.copy(out=dstb.t[0:64, 8:12, t * 128:(t + 1) * 128],
                                                      in_=pB[0:64, 0:512].rearrange("p (k t) -> p k t", k=4)), R=[ptB], Wa=[dstb.tr])
                STOP("P4a")
                Pc = [kb.sbuf("Pc%d" % i_, [128, 512], BF16, es) for i_ in range(2)]
                rdc = kb.sbuf("rdc", [128, 4], F32, es)
                it = 0
                Rops = [qTc.tr, kTc.tr, VAc.tr]
                for b in range(NT):
                    blocks = []
                    for kb_ in range(max(0, b - 1), b + 1):
                        blocks.append((0, kb_, 0 if kb_ == b else 1))
                    for kb_ in range(max(0, b - 4), b + 1):
                        d_ = b - kb_
                        blocks.append((1, kb_, 2 if d_ == 0 else (4 if d_ == 4 else 3)))
                    for kb_ in range(0, b + 1):
                        blocks.append((2, kb_, 5 if kb_ == b else 6))
                    accv, acct = psb(1, b % 2)
                    acc3 = accv[:, 0:260].rearrange("p (j d) -> p j d", j=4)
                    for bi, (g, kb_, midx) in enumerate(blocks):
                        sv_, st_ = psb(2, it % 2)
                        P = Pc[it % 2]
                        for jj in range(4):
                            hh = 4 * g + jj
                            mm(sv_[:, jj * 128:(jj + 1) * 128], kTc.t[0:64, hh, kb_ * 128:(kb_ + 1) * 128],
                               qTc.t[0:64, hh, b * 128:(b + 1) * 128], True, True, Rops, st_, jj == 0)
                        kb.op("act", lambda e: e.activation(out=P.t[:], in_=sv_[:, 0:512], func=AF.Exp, scale=0.125), R=[st_], W=[P.tr])
                        kb.op("dve", lambda e: e.tensor_tensor(out=P.t[:].rearrange("p (j q) -> p j q", j=4),
                                                               in0=P.t[:].rearrange("p (j q) -> p j q", j=4),
                                                               in1=dmask.t[:, midx, :].unsqueeze(1).to_broadcast([128, 4, 128]), op=ALU.mult),
                              R=[P.tr, dmask.tr], W=[P.tr])
                        for jj in range(4):
                            mm(acc3[:, jj, :], P.t[:, jj * 128:(jj + 1) * 128], VAc.t[:, kb_, 4 * g + jj, :],
                               (bi == 0 and jj == 0), (bi == len(blocks) - 1 and jj == 3), [P.tr] + Rops, acct, (bi == 0 and jj == 0))
                        it += 1
                    kb.op("dve", lambda e: e.reciprocal(out=rdc.t[:, 0:4], in_=acc3[:, :, 64]), R=[acct], W=[rdc.tr])
                    kb.op("dve", lambda e: e.tensor_tensor(out=Osc.t[:, b, :].rearrange("p (j d) -> p j d", j=4), in0=acc3[:, :, 0:64],
                                                           in1=rdc.t[:, 0:4].unsqueeze(2).to_broadcast([128, 4, 64]), op=ALU.mult),
                          R=[acct, rdc.tr], Wa=[Osc.tr])
                for t in range(NT):
                    kb.dma("sp", O_d.t[t * 128:(t + 1) * 128, 768:1024], Osc.t[:, t, :], R=[Osc.tr], Wa=[O_d.tr])
                kb.barrier()
            if li == 0 and s == 0:
                tap("O_dil", O_d.t[:, 768:1024], [O_d.tr], [S, 256], BF16)
            STOP("P4")

            with contextlib.ExitStack() as es:
                yacc = kb.sbuf("yacc", [128, NT, D], F32, es)
                x1T = kb.sbuf("x1T", [128, 8, S], BF16, es)
                gate = kb.sbuf("gate", [128, NT, 32], F32, es)
                lng = kb.sbuf("lng", [128, 4, D], F32, es)
                for i_, n_ in enumerate(("ln1_g", "ln1_b", "ln2_g", "ln2_b")):
                    kb.dma("sp", lng.t[:, i_, :], W[n_].rearrange("(o d) -> o d", o=1).partition_broadcast(128), R=[Wtr[n_]], Wa=[lng.tr])
                zb = kb.sbuf("zb", [128, D], F32, es)
                st6 = kb.sbuf("st6", [128, 2, 6], F32, es)
                mv = kb.sbuf("mv", [128, 2], F32, es)
                r1 = kb.sbuf("r1", [128, 1], F32, es)
                r2 = kb.sbuf("r2", [128, 1], F32, es)
                r3 = kb.sbuf("r3", [128, 1], F32, es)

                def layer_norm(src_ap, src_tr, gi, out_ap, out_tr):
                    for hh in range(2):
                        kb.op("dve", lambda e: e.bn_stats(out=st6.t[:, hh, :], in_=src_ap[:, hh * 512:(hh + 1) * 512]),
                              R=src_tr, W=[st6.tr] if hh == 0 else (), Wa=[st6.tr] if hh else ())
                    kb.op("dve", lambda e: e.bn_aggr(out=mv.t[:], in_=st6.t[:].rearrange("p a b -> p (a b)")), R=[st6.tr], W=[mv.tr])
                    kb.op("dve", lambda e: e.tensor_scalar(out=r1.t[:], in0=mv.t[:, 1:2], scalar1=LN_EPS, scalar2=None, op0=ALU.add),
                          R=[mv.tr], W=[r1.tr])
                    kb.op("act", lambda e: e.activation(out=r2.t[:], in_=r1.t[:], func=AF.Sqrt), R=[r1.tr], W=[r2.tr])
                    kb.op("dve", lambda e: e.reciprocal(out=r3.t[:], in_=r2.t[:]), R=[r2.tr], W=[r3.tr])
                    kb.op("dve", lambda e: e.tensor_scalar(out=zb.t[:], in0=src_ap, scalar1=mv.t[:, 0:1], scalar2=r3.t[:, 0:1],
                                                           op0=ALU.subtract, op1=ALU.mult), R=list(src_tr) + [mv.tr, r3.tr], W=[zb.tr])
                    kb.op("dve", lambda e: e.tensor_tensor(out=zb.t[:], in0=zb.t[:], in1=lng.t[:, gi, :], op=ALU.mult),
                          R=[zb.tr, lng.tr], W=[zb.tr])
                    kb.op("dve", lambda e: e.tensor_tensor(out=out_ap, in0=zb.t[:], in1=lng.t[:, gi + 1, :], op=ALU.add),
                          R=[zb.tr, lng.tr], W=out_tr)

                with contextlib.ExitStack() as es5:
                    w_o = kb.sbuf("w_o", [128, 8, D], BF16, es5)
                    for kc in range(8):
                        kb.dma("pool", w_o.t[:, kc, :], W["w_o"][kc * 128:(kc + 1) * 128, :], R=[Wtr["w_o"]], Wa=[w_o.tr],
                               max_dma_last_dim=4096)
                    rwf = kb.sbuf("rwf", [128, 8, 36], F32, es5)
                    rw = kb.sbuf("rw", [128, 8, 36], BF16, es5)
                    rbias = kb.sbuf("rbias", [128, 36], F32, es5)
                    kb.dma("sp", rwf.t[:, :, 0:4], W["router_group_w"].rearrange("(k p) g -> p k g", p=128), R=[Wtr["router_group_w"]],
                           Wa=[rwf.tr])
                    for g_ in range(4):
                        kb.dma("sp", rwf.t[:, :, 4 + 8 * g_:12 + 8 * g_], W["router_expert_w"][g_].rearrange("(k p) e -> p k e", p=128),
                               R=[Wtr["router_expert_w"]], Wa=[rwf.tr])
                    kb.op("dve", lambda e: e.tensor_copy(out=rw.t[:], in_=rwf.t[:]), R=[rwf.tr], W=[rw.tr])
                    kb.dma("sp", rbias.t[:, 0:4], W["router_group_b"].rearrange("(o g) -> o g", o=1).partition_broadcast(128),
                           R=[Wtr["router_group_b"]], Wa=[rbias.tr])
                    kb.dma("sp", rbias.t[:, 4:36], W["router_expert_b"].rearrange("(o g) e -> o (g e)", o=1).partition_broadcast(128),
                           R=[Wtr["router_expert_b"]], Wa=[rbias.tr])
                    Ots = [kb.sbuf("Ot%d" % i, [128, D], BF16, es5) for i in range(2)]
                    OTs = [kb.sbuf("OT%d" % i, [128, 8, 128], BF16, es5) for i in range(2)]
                    xts = [kb.sbuf("xq%d" % i, [128, D], F32, es5) for i in range(2)]
                    zs = kb.sbuf("zs", [128, D], F32, es5)
                    x1 = kb.sbuf("x1", [128, D], F32, es5)
                    x1b = kb.sbuf("x1b", [128, D], BF16, es5)
                    lgt = kb.sbuf("lgt", [128, 36], F32, es5)
                    sm = {n_: kb.sbuf("rt_" + n_, [128, w_], F32, es5) for n_, w_ in
                          (("gm", 1), ("ghot", 4), ("ng", 1), ("eg", 4), ("sg", 1), ("ptop", 1), ("tmp", 32), ("le", 8),
                           ("m8", 8), ("nm1", 1), ("ex", 8), ("exm", 8), ("den", 1), ("rd", 1), ("fac", 1), ("ge", 8))}
                    for t in range(NT):
                        Ot, OT, xt = Ots[t % 2], OTs[t % 2], xts[t % 2]
                        kb.dma("sp", Ot.t[:], O_d.t[t * 128:(t + 1) * 128, :], R=[O_d.tr], W=[Ot.tr])
                        kb.dma("sp", xt.t[:], x_src.t[s, t * 128:(t + 1) * 128, :], R=[x_src.k(s)], W=[xt.tr])
                        pa, pt = psbf(0, t % 2)
                        for kc in range(8):
                            tp(pa[:, kc * 128:(kc + 1) * 128], Ot.t[:, kc * 128:(kc + 1) * 128], [Ot.tr], pt, kc == 0)
                        kb.op("act", lambda e: e.copy(out=OT.t[:].rearrange("p k t -> p (k t)"), in_=pa), R=[pt], W=[OT.tr])
                        for half in range(2):
                            pm, pmt = psb(1, half)
                            for kc in range(8):
                                mm(pm[:, 0:512], OT.t[:, kc, :], w_o.t[:, kc, half * 512:(half + 1) * 512], kc == 0, kc == 7,
                                   [OT.tr, w_o.tr], pmt, kc == 0)
                            kb.op("dve", lambda e: e.scalar_tensor_tensor(out=zs.t[:, half * 512:(half + 1) * 512],
                                                                          in0=xt.t[:, half * 512:(half + 1) * 512], scalar=ALPHA,
                                                                          in1=pm[:, 0:512], op0=ALU.mult, op1=ALU.add),
                                  R=[xt.tr, pmt], W=[zs.tr] if half == 0 else (), Wa=[zs.tr] if half else ())
                        layer_norm(zs.t[:], [zs.tr], 0, x1.t[:], [x1.tr])
                        kb.op("act", lambda e: e.mul(out=yacc.t[:, t, :], in_=x1.t[:], mul=ALPHA), R=[x1.tr], W=[yacc.k(t)])
                        kb.op("dve", lambda e: e.tensor_copy(out=x1b.t[:], in_=x1.t[:]), R=[x1.tr], W=[x1b.tr])
                        pa2, pt2 = psbf(2, t % 2)
                        for kc in range(8):
                            tp(pa2[:, kc * 128:(kc + 1) * 128], x1b.t[:, kc * 128:(kc + 1) * 128], [x1b.tr], pt2, kc == 0)
                        kb.op("act", lambda e: e.copy(out=x1T.t[:, :, t * 128:(t + 1) * 128],
                                                      in_=pa2.rearrange("p (k t) -> p k t", k=8)), R=[pt2], Wa=[x1T.k(t)])
                        pr, prt = psb(3, t % 2)
                        for kc in range(8):
                            mm(pr[:, 0:36], x1T.t[:, kc, t * 128:(t + 1) * 128], rw.t[:, kc, :], kc == 0, kc == 7, [x1T.k(t), rw.tr], prt, kc == 0)
                        def so(eng, fn, R, Wn):
                            kb.op(eng, fn, R=[sm[r_].tr if isinstance(r_, str) else r_ for r_ in R], W=[sm[Wn].tr])
                        kb.op("dve", lambda e: e.tensor_tensor(out=lgt.t[:], in0=pr[:, 0:36], in1=rbias.t[:], op=ALU.add),
                              R=[prt, rbias.tr], W=[lgt.tr])
                        so("dve", lambda e: e.reduce_max(out=sm["gm"].t[:], in_=lgt.t[:, 0:4], axis=AX.X), [lgt.tr], "gm")
                        so("dve", lambda e: e.tensor_scalar(out=sm["ghot"].t[:], in0=lgt.t[:, 0:4], scalar1=sm["gm"].t[:, 0:1], scalar2=None,
                                                            op0=ALU.is_ge), [lgt.tr, "gm"], "ghot")
                        so("dve", lambda e: e.tensor_scalar(out=sm["ng"].t[:], in0=sm["gm"].t[:], scalar1=-1.0, scalar2=None, op0=ALU.mult),
                           ["gm"], "ng")
                        so("act", lambda e: e.activation(out=sm["eg"].t[:], in_=lgt.t[:, 0:4], func=AF.Exp, bias=sm["ng"].t[:, 0:1]),
                           [lgt.tr, "ng"], "eg")
                        so("dve", lambda e: e.reduce_sum(out=sm["sg"].t[:], in_=sm["eg"].t[:], axis=AX.X), ["eg"], "sg")
                        so("dve", lambda e: e.reciprocal(out=sm["ptop"].t[:], in_=sm["sg"].t[:]), ["sg"], "ptop")
                        so("dve", lambda e: e.tensor_tensor(out=sm["tmp"].t[:].rearrange("p (g e) -> p g e", g=4),
                                                            in0=lgt.t[:, 4:36].rearrange("p (g e) -> p g e", g=4),
                                                            in1=sm["ghot"].t[:].unsqueeze(2).to_broadcast([128, 4, 8]), op=ALU.mult),
                           [lgt.tr, "ghot"], "tmp")
                        so("dve", lambda e: e.tensor_reduce(out=sm["le"].t[:], in_=sm["tmp"].t[:].rearrange("p (g e) -> p e g", g=4),
                                                            axis=AX.X, op=ALU.add), ["tmp"], "le")
                        so("dve", lambda e: e.max(out=sm["m8"].t[:], in_=sm["le"].t[:]), ["le"], "m8")
                        so("dve", lambda e: e.tensor_scalar(out=sm["nm1"].t[:], in0=sm["m8"].t[:, 0:1], scalar1=-1.0, scalar2=None,
                                                            op0=ALU.mult), ["m8"], "nm1")
                        so("act", lambda e: e.activation(out=sm["ex"].t[:], in_=sm["le"].t[:], func=AF.Exp, bias=sm["nm1"].t[:, 0:1]),
                           ["le", "nm1"], "ex")
                        so("dve", lambda e: e.scalar_tensor_tensor(out=sm["exm"].t[:], in0=sm["le"].t[:], scalar=sm["m8"].t[:, 1:2],
                                                                   in1=sm["ex"].t[:], op0=ALU.is_ge, op1=ALU.mult), ["le", "m8", "ex"], "exm")
                        so("dve", lambda e: e.reduce_sum(out=sm["den"].t[:], in_=sm["exm"].t[:], axis=AX.X), ["exm"], "den")
                        so("dve", lambda e: e.reciprocal(out=sm["rd"].t[:], in_=sm["den"].t[:]), ["den"], "rd")
                        so("dve", lambda e: e.tensor_tensor(out=sm["fac"].t[:], in0=sm["rd"].t[:], in1=sm["ptop"].t[:], op=ALU.mult),
                           ["rd", "ptop"], "fac")
                        so("dve", lambda e: e.tensor_scalar(out=sm["ge"].t[:], in0=sm["exm"].t[:], scalar1=sm["fac"].t[:, 0:1], scalar2=None,
                                                            op0=ALU.mult), ["exm", "fac"], "ge")
                        kb.op("dve", lambda e: e.tensor_tensor(out=gate.t[:, t, :].rearrange("p (g e) -> p g e", g=4),
                                                               in0=sm["ghot"].t[:].unsqueeze(2).to_broadcast([128, 4, 8]),
                                                               in1=sm["ge"].t[:].unsqueeze(1).to_broadcast([128, 4, 8]), op=ALU.mult),
                              R=[sm["ghot"].tr, sm["ge"].tr], W=[gate.k(t)])
                    kb.barrier()
                STOP("P5")
                with contextlib.ExitStack() as es6:
                    wgs = [kb.sbuf("wg%d" % i, [128, 8, 256], BF16, es6) for i in range(2)]
                    wus = [kb.sbuf("wu%d" % i, [128, 8, 256], BF16, es6) for i in range(2)]
                    wds = [kb.sbuf("wd%d" % i, [128, 2, D], BF16, es6) for i in range(2)]
                    sgs = [kb.sbuf("sg%d" % i, [128, 512], F32, es6) for i in range(2)]
                    aTs = [kb.sbuf("aT%d" % i, [128, 512], BF16, es6) for i in range(4)]
                    for e_ in range(32):
                        g_, ee = divmod(e_, 8)
                        wg, wu, wd = wgs[e_ % 2], wus[e_ % 2], wds[e_ % 2]
                        kb.dma("pool", wg.t[:], W["expert_w_gate"][g_, ee].rearrange("(k p) f -> p k f", p=128), R=[Wtr["expert_w_gate"]], W=[wg.tr])
                        kb.dma("pool", wu.t[:], W["expert_w_up"][g_, ee].rearrange("(k p) f -> p k f", p=128), R=[Wtr["expert_w_up"]], W=[wu.tr])
                        kb.dma("pool", wd.t[:], W["expert_w_down"][g_, ee].rearrange("(k p) d -> p k d", p=128), R=[Wtr["expert_w_down"]], W=[wd.tr])
                        for c in range(4):
                            x1tr = [x1T.k(4 * c + j) for j in range(4)]
                            for fc in range(2):
                                hg, hgt = psb(0, fc)
                                hu, hut = psb(1, fc)
                                for kc in range(8):
                                    mm(hg[:, 0:512], wg.t[:, kc, fc * 128:(fc + 1) * 128], x1T.t[:, kc, c * 512:(c + 1) * 512], kc == 0, kc == 7,
                                       [wg.tr] + x1tr, hgt, kc == 0)
                                for kc in range(8):
                                    mm(hu[:, 0:512], wu.t[:, kc, fc * 128:(fc + 1) * 128], x1T.t[:, kc, c * 512:(c + 1) * 512], kc == 0, kc == 7,
                                       [wu.tr] + x1tr, hut, kc == 0)
                                sgb = sgs[fc]
                                aT = aTs[(c % 2) * 2 + fc]
                                kb.op("act", lambda e: e.activation(out=sgb.t[:], in_=hg[:, 0:512], func=AF.Silu), R=[hgt], W=[sgb.tr])
                                kb.op("dve", lambda e: e.tensor_tensor(out=aT.t[:], in0=sgb.t[:], in1=hu[:, 0:512], op=ALU.mult),
                                      R=[sgb.tr, hut], W=[aT.tr])
                            for j in range(4):
                                t = 4 * c + j
                                for half in range(2):
                                    py, pyt = psb(2 + j % 2, half)
                                    for fc in range(2):
                                        aT = aTs[(c % 2) * 2 + fc]
                                        mm(py[:, 0:512], aT.t[:, j * 128:(j + 1) * 128], wd.t[:, fc, half * 512:(half + 1) * 512], fc == 0, fc == 1,
                                           [aT.tr, wd.tr], pyt, fc == 0)
                                    kb.op("dve", lambda e: e.scalar_tensor_tensor(
                                        out=yacc.t[:, t, half * 512:(half + 1) * 512], in0=py[:, 0:512], scalar=gate.t[:, t, e_:e_ + 1],
                                        in1=yacc.t[:, t, half * 512:(half + 1) * 512], op0=ALU.mult, op1=ALU.add),
                                        R=[pyt, gate.k(t), yacc.k(t)], W=[yacc.k(t)])
                    outs = [kb.sbuf("ob%d" % i, [128, D], F32, es6) for i in range(2)]
                    for t in range(NT):
                        ob = outs[t % 2]
                        layer_norm(yacc.t[:, t, :], [yacc.k(t)], 2, ob.t[:], [ob.tr])
                        kb.dma("sp", x_dst.t[s, t * 128:(t + 1) * 128, :], ob.t[:], R=[ob.tr], Wa=[x_dst.k(s)])
                    kb.barrier()
            if li == nl - 1:
                out_tracks.append(y_out.k(s))

    try:
        body()
    except _Stop:
        pass
    kb.finish(out_tracks)
    return kb, tapd


def _wshapes(inputs):
    return {n: list(inputs[n].shape) for n in WNAMES}


def kernel(**inputs):
    x = np.ascontiguousarray(inputs["x"], dtype=np.float32)
    pos = np.ascontiguousarray(inputs["positions"], dtype=np.int32)
    ncores = 8
    nseq = x.shape[0] // ncores
    kb, _ = build(nseq, list(range(DEPTH)), _wshapes(inputs))
    hc = host_consts()
    in_maps = []
    for c in range(ncores):
        m = {"x": x[c * nseq:(c + 1) * nseq], "positions": pos[c * nseq:(c + 1) * nseq]}
        for n in WNAMES:
            m[n] = np.ascontiguousarray(inputs[n], dtype=np.float32)
        for n, v in hc.items():
            m["c_" + n] = v
        in_maps.append(m)
    res = run_bass_kernel_spmd(kb.nc, in_maps, core_ids=list(range(ncores)))
    return np.concatenate([r["y"] for r in res.results], axis=0)
```

```python
import contextlib
import numpy as np
import ml_dtypes
import concourse.bass as bass
import concourse.mybir as mybir
from concourse.bass_utils import run_bass_kernel_spmd

F32 = mybir.dt.float32
BF16 = mybir.dt.bfloat16
I32 = mybir.dt.int32
ALU = mybir.AluOpType
AF = mybir.ActivationFunctionType
AX = mybir.AxisListType

D = 1024
S = 2048
NT = S // 128
DEPTH = 4
INC = 3400
ALPHA = float((2.0 * DEPTH) ** 0.25)
LN_EPS = 1e-5
RMS_EPS = 1e-6
TWO_PI = 6.283185307179586
C1 = 6.28125
C2 = TWO_PI - C1
MAGIC = 12582912.0
NBIS = 22


class Track:
    __slots__ = ("w", "r", "dsem", "dcnt")

    def __init__(self):
        self.w = {}
        self.r = {}
        self.dsem = None
        self.dcnt = 0


class Eng:
    def __init__(self, name, e, sem):
        self.name = name
        self.e = e
        self.sem = sem
        self.n = 0
        self.seen = {}


class Buf:
    def __init__(self, t):
        self.t = t
        self.tr = Track()
        self.sub = {}

    def k(self, key):
        if key not in self.sub:
            self.sub[key] = Track()
        return self.sub[key]

    def all(self):
        return [self.tr] + list(self.sub.values())

    def __getitem__(self, idx):
        return self.t[idx]


class KB:
    def __init__(self):
        self.nc = bass.Bass("TRN2", target_bir_lowering=False)
        self.es = contextlib.ExitStack()
        nc = self.nc
        self.E = {}
        for name, e in (("pe", nc.tensor), ("act", nc.scalar), ("dve", nc.vector),
                        ("pool", nc.gpsimd), ("sp", nc.sync)):
            sem = self.es.enter_context(nc.semaphore("sem_" + name))
            self.E[name] = Eng(name, e, sem)
        self.dsems = []
        self.ninst = 0
        self.pool_ds = []
        self.pool_i = 0

    def sbuf(self, name, shape, dtype, es=None):
        self.uid = getattr(self, "uid", 0) + 1
        return Buf((es or self.es).enter_context(self.nc.sbuf_tensor("sb%d_%s" % (self.uid, name), list(shape), dtype)))

    def psum(self, name, shape, dtype):
        return Buf(self.es.enter_context(self.nc.psum_tensor(name, list(shape), dtype)))

    def dram(self, name, shape, dtype, kind="Internal"):
        return Buf(self.nc.dram_tensor(name, list(shape), dtype, kind=kind))

    def _need(self, E, sem, val, owner, raw):
        if owner is E and (not raw or E.name == "pe"):
            return
        if E.seen.get(sem, 0) >= val:
            return
        E.e.wait_ge(sem, val)
        E.seen[sem] = val

    def _deps(self, E, R, W, Wa):
        for t in R:
            for sem, (val, owner) in t.w.items():
                self._need(E, sem, val, owner, True)
        for t in W:
            for sem, (val, owner) in t.w.items():
                self._need(E, sem, val, owner, False)
            for sem, (val, owner) in t.r.items():
                self._need(E, sem, val, owner, False)
        for t in Wa:
            for sem, (val, owner) in t.r.items():
                self._need(E, sem, val, owner, False)

    def _post(self, sem, val, owner, R, W, Wa):
        for t in R:
            t.r[sem] = (val, owner)
        for t in W:
            t.w = {sem: (val, owner)}
            t.r = {}
        for t in Wa:
            t.w[sem] = (val, owner)

    def op(self, eng, fn, R=(), W=(), Wa=()):
        E = self.E[eng]
        self._deps(E, R, W, Wa)
        inst = fn(E.e)
        E.n += 1
        inst.then_inc(E.sem, 1)
        self._post(E.sem, E.n, E, R, W, Wa)
        self.ninst += 1
        return inst

    def dma(self, q, out_ap, in_ap, R=(), W=(), Wa=(), **kw):
        E = self.E[q]
        self._deps(E, R, W, Wa)
        tgt = (list(W) + list(Wa))[0]
        if tgt.dsem is None:
            if len(self.pool_ds) < 40:
                ds = [self.es.enter_context(self.nc.semaphore("ds%d" % len(self.dsems))), 0]
                self.pool_ds.append(ds)
                self.dsems.append(ds)
            else:
                ds = self.pool_ds[self.pool_i % 40]
                self.pool_i += 1
            tgt.dsem = ds
        ds = tgt.dsem
        inst = E.e.dma_start(out=out_ap, in_=in_ap, **kw)
        ds[1] += 16
        inst.then_inc(ds[0], 16)
        self._post(ds[0], ds[1], None, R, W, Wa)
        self.ninst += 1
        return inst

    def dedicate(self, track):
        ds = [self.es.enter_context(self.nc.semaphore("dd%d" % len(self.dsems))), 0]
        self.dsems.append(ds)
        track.dsem = ds

    def barrier(self):
        for E in self.E.values():
            for E2 in self.E.values():
                if E2 is not E and E2.n > 0:
                    self._need(E, E2.sem, E2.n, E2, True)
            for ds in self.dsems:
                if ds[1] > 0:
                    self._need(E, ds[0], ds[1], None, True)

    def finish(self, out_tracks):
        E = self.E["sp"]
        for t in out_tracks:
            for sem, (val, owner) in t.w.items():
                self._need(E, sem, val, owner, True)
        self.barrier()


def host_consts():
    c = {}
    c["ident"] = np.eye(128, dtype=np.float32).astype(ml_dtypes.bfloat16)
    k = np.arange(128)[:, None]
    q = np.arange(128)[None, :]
    dm = np.zeros((128, 8, 128), np.float32)
    dm[:, 0, :] = (k <= q)
    dm[:, 1, :] = (q <= k)
    dm[:, 2, :] = ((q - k) % 4 == 0) & (q >= k)
    dm[:, 3, :] = ((q - k) % 4 == 0)
    dm[:, 4, :] = ((q - k) % 4 == 0) & (q <= k)
    dm[:, 5, :] = ((q - k) % 16 == 0) & (q >= k)
    dm[:, 6, :] = ((q - k) % 16 == 0)
    dm[:, 7, :] = 1.0
    c["dmask"] = dm.astype(ml_dtypes.bfloat16)
    qq = np.arange(128)[:, None]
    kk = np.arange(128)[None, :]
    c["tribias"] = np.where(kk <= qq, 0.0, -1e30).astype(np.float32)
    inv = []
    for rot in (32, 16, 8):
        inv.append((500000.0 ** (-np.arange(0, rot, 2, dtype=np.float32) / np.float32(rot))).astype(np.float32))
    inv = np.concatenate(inv).astype(np.float32)
    c["inv"] = np.tile(inv[None, :], (128, 1)).astype(np.float32)
    p = np.arange(128)
    sel = np.zeros((128, 8, 128), np.float32)
    for g in range(8):
        sel[p, g, 16 * g + (p % 16)] = 1.0
    c["selmask"] = sel.astype(ml_dtypes.bfloat16)
    rt = np.zeros((32, 128), np.float32)
    rt[p // 16, p] = 1.0
    c["rt"] = rt.astype(ml_dtypes.bfloat16)
    return c


WNAMES = ["w_in", "mla_q_norm", "mla_kv_norm", "mla_w_uq", "mla_w_ukv", "w_o", "ln1_g", "ln1_b",
          "router_group_w", "router_group_b", "router_expert_w", "router_expert_b",
          "expert_w_gate", "expert_w_up", "expert_w_down", "ln2_g", "ln2_b"]


class _Stop(Exception):
    pass


def build(nseq, layers, wshapes, taps=None, stop_after=None):
    kb = KB()
    nc = kb.nc
    taps = taps or []
    x_in = kb.dram("x", [nseq, S, D], F32, kind="ExternalInput")
    pos_in = kb.dram("positions", [nseq, S], I32, kind="ExternalInput")
    y_out = kb.dram("y", [nseq, S, D], F32, kind="ExternalOutput")
    Wd_ = {n: kb.dram(n, wshapes[n], F32, kind="ExternalInput") for n in WNAMES}
    hc = host_consts()
    Cd = {n: kb.dram("c_" + n, list(v.shape), BF16 if v.dtype != np.float32 else F32, kind="ExternalInput")
          for n, v in hc.items()}
    tapd = {}
    nl = len(layers)
    xs = [kb.dram("xs%d" % i, [nseq, S, D], F32) for i in range(2)] if nl > 1 else []
    u_d = kb.dram("u_d", [S, INC], F32)

    ident = kb.sbuf("ident", [128, 128], BF16)
    dmask = kb.sbuf("dmask", [128, 8, 128], BF16)
    tribias = kb.sbuf("tribias", [128, 128], F32)
    inv = kb.sbuf("inv", [128, 28], F32)
    selmask = kb.sbuf("selmask", [128, 8, 128], BF16)
    rt = kb.sbuf("rt", [32, 128], BF16)
    cosT = kb.sbuf("cosT", [128, nseq * NT, 28], F32)
    sinT = kb.sbuf("sinT", [128, nseq * NT, 28], F32)
    for b_, n in ((ident, "ident"), (dmask, "dmask"), (tribias, "tribias"), (inv, "inv"),
                  (selmask, "selmask"), (rt, "rt")):
        kb.dma("sp", b_.t[:], Cd[n].t[:], R=[Cd[n].tr], W=[b_.tr])

    PS = [kb.psum("ps%d" % i, [128, 1024], F32) for i in range(4)]

    def psb(i, h):
        return PS[i].t[:, h * 512:(h + 1) * 512], PS[i].k(h)

    def psbf(i, h):
        return PS[i].t.bitcast(BF16)[:, h * 1024:(h + 1) * 1024], PS[i].k(h)

    with contextlib.ExitStack() as es:
        posi = kb.sbuf("posi", [128, nseq * NT], I32, es)
        posf = kb.sbuf("posf", [128, nseq * NT], F32, es)
        ang = kb.sbuf("ang", [128, nseq * NT, 28], F32, es)
        tt = kb.sbuf("rp_t", [128, nseq * NT, 28], F32, es)
        kf = kb.sbuf("rp_k", [128, nseq * NT, 28], F32, es)
        rr = kb.sbuf("rp_r", [128, nseq * NT, 28], F32, es)
        kb.dma("sp", posi.t[:].rearrange("p (s t) -> p s t", s=nseq),
               pos_in.t[:].rearrange("s (t p) -> p s t", p=128), R=[pos_in.tr], W=[posi.tr],
               allow_slow_non_contiguous=True)
        kb.op("dve", lambda e: e.tensor_copy(out=posf.t[:], in_=posi.t[:]), R=[posi.tr], W=[posf.tr])
        nt_all = nseq * NT
        kb.op("dve", lambda e: e.tensor_tensor(
            out=ang.t[:], in0=posf.t[:].unsqueeze(2).to_broadcast([128, nt_all, 28]),
            in1=inv.t[:].unsqueeze(1).to_broadcast([128, nt_all, 28]), op=ALU.mult),
            R=[posf.tr, inv.tr], W=[ang.tr])
        for dst, shift in ((sinT, 0.0), (cosT, float(np.pi / 2))):
            kb.op("dve", lambda e: e.tensor_scalar(out=tt.t[:], in0=ang.t[:], scalar1=shift, scalar2=float(1.0 / TWO_PI),
                                                   op0=ALU.add, op1=ALU.mult), R=[ang.tr], W=[tt.tr])
            kb.op("dve", lambda e: e.tensor_scalar(out=kf.t[:], in0=tt.t[:], scalar1=MAGIC, scalar2=-MAGIC,
                                                   op0=ALU.add, op1=ALU.add), R=[tt.tr], W=[kf.tr])
            kb.op("dve", lambda e: e.scalar_tensor_tensor(out=rr.t[:], in0=kf.t[:], scalar=-C1, in1=ang.t[:],
                                                          op0=ALU.mult, op1=ALU.add), R=[kf.tr, ang.tr], W=[rr.tr])
            kb.op("dve", lambda e: e.scalar_tensor_tensor(out=tt.t[:], in0=kf.t[:], scalar=-C2, in1=rr.t[:],
                                                          op0=ALU.mult, op1=ALU.add), R=[kf.tr, rr.tr], W=[tt.tr])
            kb.op("dve", lambda e: e.tensor_scalar(out=rr.t[:], in0=tt.t[:], scalar1=shift, scalar2=None,
                                                   op0=ALU.add), R=[tt.tr], W=[rr.tr])
            kb.op("dve", lambda e: e.tensor_scalar(out=tt.t[:], in0=rr.t[:], scalar1=3.1415925, scalar2=-3.1415925,
                                                   op0=ALU.min, op1=ALU.max), R=[rr.tr], W=[tt.tr])
            kb.op("act", lambda e: e.activation(out=dst.t[:], in_=tt.t[:], func=AF.Sin), R=[tt.tr], W=[dst.tr])
        kb.barrier()

    def rope(eng, src, dst, col0, nh, hd, half, cs, sn, tmp1, tmp2, R, Wt):
        def v(buf, off):
            return buf.t[:, col0:col0 + nh * hd].rearrange("p (h d) -> p h d", h=nh)[:, :, off:off + half]
        cb = cs.unsqueeze(1).to_broadcast([128, nh, half])
        sb = sn.unsqueeze(1).to_broadcast([128, nh, half])
        t1 = tmp1.t[:, 0:nh * half].rearrange("p (h d) -> p h d", h=nh)
        t2 = tmp2.t[:, 0:nh * half].rearrange("p (h d) -> p h d", h=nh)
        x1, x2 = v(src, 0), v(src, half)
        o1, o2 = v(dst, 0), v(dst, half)
        kb.op(eng, lambda e: e.tensor_tensor(out=t1, in0=x1, in1=cb, op=ALU.mult), R=R, W=[tmp1.tr])
        kb.op(eng, lambda e: e.tensor_tensor(out=t2, in0=x2, in1=sb, op=ALU.mult), R=R, W=[tmp2.tr])
        kb.op(eng, lambda e: e.tensor_tensor(out=o1, in0=t1, in1=t2, op=ALU.subtract), R=[tmp1.tr, tmp2.tr], Wa=Wt)
        kb.op(eng, lambda e: e.tensor_tensor(out=t1, in0=x2, in1=cb, op=ALU.mult), R=R, W=[tmp1.tr])
        kb.op(eng, lambda e: e.tensor_tensor(out=t2, in0=x1, in1=sb, op=ALU.mult), R=R, W=[tmp2.tr])
        kb.op(eng, lambda e: e.tensor_tensor(out=o2, in0=t1, in1=t2, op=ALU.add), R=[tmp1.tr, tmp2.tr], Wa=Wt)

    def rope2(eng, sv, dv, nh, half, cs, sn, tmp1, tmp2, R, Wt):
        cb_ = cs.unsqueeze(1).to_broadcast([128, nh, half])
        sb_ = sn.unsqueeze(1).to_broadcast([128, nh, half])
        t1 = tmp1.t[:, 0:nh * half].rearrange("p (h d) -> p h d", h=nh)
        t2 = tmp2.t[:, 0:nh * half].rearrange("p (h d) -> p h d", h=nh)
        x1, x2 = sv[:, :, 0:half], sv[:, :, half:2 * half]
        o1, o2 = dv[:, :, 0:half], dv[:, :, half:2 * half]
        kb.op(eng, lambda e: e.tensor_tensor(out=t1, in0=x1, in1=cb_, op=ALU.mult), R=R, W=[tmp1.tr])
        kb.op(eng, lambda e: e.tensor_tensor(out=t2, in0=x2, in1=sb_, op=ALU.mult), R=R, W=[tmp2.tr])
        kb.op(eng, lambda e: e.tensor_tensor(out=o1, in0=t1, in1=t2, op=ALU.subtract), R=[tmp1.tr, tmp2.tr], Wa=Wt)
        kb.op(eng, lambda e: e.tensor_tensor(out=t1, in0=x2, in1=cb_, op=ALU.mult), R=R, W=[tmp1.tr])
        kb.op(eng, lambda e: e.tensor_tensor(out=t2, in0=x1, in1=sb_, op=ALU.mult), R=R, W=[tmp2.tr])
        kb.op(eng, lambda e: e.tensor_tensor(out=o2, in0=t1, in1=t2, op=ALU.add), R=[tmp1.tr, tmp2.tr], Wa=Wt)

    def tap(name, ap, tracks, shape, dtype):
        if name in taps:
            d = kb.dram("tap_" + name, list(shape), dtype, kind="ExternalOutput")
            tapd[name] = d
            n0 = shape[0]
            for r0 in range(0, n0, 128):
                r1 = min(n0, r0 + 128)
                kb.dma("sp", d.t[r0:r1], ap[r0:r1], R=tracks, Wa=[d.tr])

    out_tracks = []
    kb.dedicate(u_d.tr)
    O_d = kb.dram("O_d", [S, D], BF16)
    kb.dedicate(O_d.tr)
    for xx in xs:
        for s_ in range(nseq):
            kb.dedicate(xx.k(s_))
    for s_ in range(nseq):
        kb.dedicate(y_out.k(s_))

    def mm(out, lhsT, rhs, start, stop, R, pt, first):
        kb.op("pe", lambda e: e.matmul(out, lhsT=lhsT, rhs=rhs, start=start, stop=stop),
              R=R, W=[pt] if first else (), Wa=() if first else [pt])

    def tp(out, in_, R, pt, first):
        kb.op("pe", lambda e: e.transpose(out, in_, ident.t[:]),
              R=list(R) + [ident.tr], W=[pt] if first else (), Wa=() if first else [pt])

    def attention(es, nheads, nch_fn, lhs_fn, rhs_fn, v_fn, scale, mask_fn, Ost, ocol0, Rops):
        if isinstance(es, tuple):
            Pb, rden = es
        else:
            Pb = [kb.sbuf("Pb%d" % i, [128, 512], BF16, es) for i in range(2)]
            rden = kb.sbuf("rden", [128, 4], F32, es)
        chunks_ = list(nch_fn) if nch_fn is not None else list(range(4))
        iters = [(h, c, kb_) for h in range(nheads) for c in chunks_ for kb_ in range(4 * c + 4)]

        def emit_qk(i):
            h, c, kb_ = iters[i]
            j0 = max(0, kb_ - 4 * c)
            ncol = 512 - 128 * j0
            sv_, st_ = psb(0, i % 2)
            mm(sv_[:, 0:ncol], lhs_fn(h, kb_), rhs_fn(h, c * 512 + 128 * j0, (c + 1) * 512), True, True, Rops, st_, True)

        emit_qk(0)
        for i, (h, c, kb_) in enumerate(iters):
            if i + 1 < len(iters):
                emit_qk(i + 1)
            hc_i = (h * len(chunks_) + chunks_.index(c))
            accv, acct = psb(1, hc_i % 2)
            acc3 = accv[:, 0:260].rearrange("p (j d) -> p j d", j=4)
            j0 = max(0, kb_ - 4 * c)
            ncol = 512 - 128 * j0
            sv_, st_ = psb(0, i % 2)
            P = Pb[i % 2]
            kb.op("act", lambda e: e.activation(out=P.t[:, 0:ncol], in_=sv_[:, 0:ncol], func=AF.Exp, scale=scale),
                  R=[st_], W=[P.tr])
            mk = mask_fn(c, kb_, j0)
            if mk is not None:
                map_, mtr, mw = mk
                kb.op("dve", lambda e: e.tensor_tensor(out=P.t[:, 0:mw], in0=P.t[:, 0:mw], in1=map_, op=ALU.mult),
                      R=[P.tr] + mtr, W=[P.tr])
            for j in range(j0, 4):
                mm(acc3[:, j, :], P.t[:, (j - j0) * 128:(j - j0 + 1) * 128], v_fn(h, kb_), (kb_ == 0 and j == 0), (kb_ == 4 * c + 3 and j == 3),
                   [P.tr] + Rops, acct, (kb_ == 0 and j == 0))
            if kb_ == 4 * c + 3:
                kb.op("dve", lambda e: e.reciprocal(out=rden.t[:, 0:4], in_=acc3[:, :, 64]), R=[acct], W=[rden.tr])
                kb.op("dve", lambda e: e.tensor_tensor(
                    out=Ost.t[:, 4 * c:4 * c + 4, ocol0 + h * 64:ocol0 + (h + 1) * 64], in0=acc3[:, :, 0:64],
                    in1=rden.t[:, 0:4].unsqueeze(2).to_broadcast([128, 4, 64]), op=ALU.mult),
                    R=[acct, rden.tr], Wa=[Ost.tr])

    def STOP(name):
        if stop_after == name:
            kb.barrier()
            raise _Stop()

    def body():
      for li, l in enumerate(layers):
        x_src = x_in if li == 0 else xs[(li - 1) % 2]
        x_dst = y_out if li == nl - 1 else xs[li % 2]
        W = {n: Wd_[n].t[l] for n in WNAMES}
        Wtr = {n: Wd_[n].tr for n in WNAMES}
        for s in range(nseq):
            tb = s * NT
            with contextlib.ExitStack() as es:
                xTa = kb.sbuf("xTa", [128, 8, S], BF16, es)
                wgr = [kb.sbuf("wgr%d" % i, [128, 8, 512], BF16, es) for i in range(2)]
                xts = [kb.sbuf("xt%d" % i, [128, D], F32, es) for i in range(2)]
                xbs = [kb.sbuf("xb%d" % i, [128, D], BF16, es) for i in range(2)]
                ugs = [kb.sbuf("ug%d" % i, [128, 512], F32, es) for i in range(4)]
                groups = []
                c0 = 0
                while c0 < INC:
                    groups.append((c0, min(512, INC - c0)))
                    c0 += 512

                def w_load(gi):
                    c0_, cw_ = groups[gi]
                    kb.dma("pool", wgr[gi % 2].t[:, :, 0:cw_], W["w_in"][:, c0_:c0_ + cw_].rearrange("(k p) c -> p k c", p=128),
                           R=[Wtr["w_in"]], W=[wgr[gi % 2].tr])
                w_load(0)
                w_load(1)
                for t in range(NT):
                    xt, xb = xts[t % 2], xbs[t % 2]
                    kb.dma("sp", xt.t[:], x_src.t[s, t * 128:(t + 1) * 128, :], R=[x_src.k(s)], W=[xt.tr])
                    kb.op("dve", lambda e: e.tensor_copy(out=xb.t[:], in_=xt.t[:]), R=[xt.tr], W=[xb.tr])
                    pa, pt = psbf(0, t % 2)
                    for kc in range(8):
                        tp(pa[:, kc * 128:(kc + 1) * 128], xb.t[:, kc * 128:(kc + 1) * 128], [xb.tr], pt, kc == 0)
                    kb.op("act", lambda e: e.copy(out=xTa.t[:, :, t * 128:(t + 1) * 128], in_=pa.rearrange("p (k t) -> p k t", k=8)),
                          R=[pt], Wa=[xTa.k(t)])
                it_ = 0
                for gi, (c0, cw) in enumerate(groups):
                    wg_ = wgr[gi % 2]
                    for t in range(NT):
                        pm, pmt = psb(1 + it_ % 2, (it_ // 2) % 2)
                        ug = ugs[it_ % 4]
                        for kc in range(8):
                            mm(pm[:, 0:cw], xTa.t[:, kc, t * 128:(t + 1) * 128], wg_.t[:, kc, 0:cw], kc == 0, kc == 7,
                               [xTa.k(t), wg_.tr], pmt, kc == 0)
                        if it_ % 2 == 0:
                            kb.op("act", lambda e: e.copy(out=ug.t[:, 0:cw], in_=pm[:, 0:cw]), R=[pmt], W=[ug.tr])
                        else:
                            kb.op("dve", lambda e: e.tensor_copy(out=ug.t[:, 0:cw], in_=pm[:, 0:cw]), R=[pmt], W=[ug.tr])
                        kb.dma("sp", u_d.t[t * 128:(t + 1) * 128, c0:c0 + cw], ug.t[:, 0:cw], R=[ug.tr], Wa=[u_d.tr])
                        it_ += 1
                    if gi + 2 < len(groups):
                        w_load(gi + 2)
                kb.barrier()
            if li == 0 and s == 0:
                tap("u", u_d.t[:], [u_d.tr], [S, INC], F32)
            STOP("P1")

            with contextlib.ExitStack() as es:
                w_uq = kb.sbuf("w_uq", [128, 2, 768], BF16, es)
                w_ukv = kb.sbuf("w_ukv", [128, 1024], BF16, es)
                stg = kb.sbuf("stg", [128, 1024], F32, es)
                qn = kb.sbuf("qn", [128, 3], F32, es)
                kb.dma("sp", qn.t[:, 0:2], W["mla_q_norm"].rearrange("(k p) -> p k", p=128), R=[Wtr["mla_q_norm"]], W=[qn.tr],
                       allow_slow_non_contiguous=True)
                kb.dma("sp", qn.t[:, 2:3], W["mla_kv_norm"].rearrange("(p o) -> p o", o=1), R=[Wtr["mla_kv_norm"]], Wa=[qn.tr])
                STOP("P2a0")
                for kc in range(2):
                    kb.dma("sp", stg.t[:, 0:768], W["mla_w_uq"][kc * 128:(kc + 1) * 128, :], R=[Wtr["mla_w_uq"]], W=[stg.tr])
                    kb.op("dve", lambda e: e.tensor_scalar(out=w_uq.t[:, kc, :], in0=stg.t[:, 0:768], scalar1=qn.t[:, kc:kc + 1],
                                                           scalar2=None, op0=ALU.mult), R=[stg.tr, qn.tr], Wa=[w_uq.tr])
                kb.dma("sp", stg.t[:, :], W["mla_w_ukv"][:, :], R=[Wtr["mla_w_ukv"]], W=[stg.tr])
                kb.op("dve", lambda e: e.tensor_scalar(out=w_ukv.t[:], in0=stg.t[:], scalar1=qn.t[:, 2:3], scalar2=None,
                                                       op0=ALU.mult), R=[stg.tr, qn.tr], W=[w_ukv.tr])
                STOP("P2a")
                QT = kb.sbuf("QT", [96, 8, S], BF16, es)
                KT = kb.sbuf("KT", [96, 8, S], BF16, es)
                VA = kb.sbuf("VA", [128, NT, 8, 65], BF16, es)
                Ost = kb.sbuf("Ost", [128, NT, 512], BF16, es)
                kb.op("dve", lambda e: e.memset(VA.t[:].rearrange("p a b c -> p (a b c)"), 1.0), W=[VA.tr])
                uus = [kb.sbuf("uu%d" % i, [128, 416], F32, es) for i in range(2)]
                cbs = [kb.sbuf("cb%d" % i, [128, 480], BF16, es) for i in range(2)]
                for cb in cbs:
                    kb.op("dve", lambda e: e.memset(cb.t[:], 0.0), W=[cb.tr])
                junk = kb.sbuf("junk", [128, 384], F32, es)
                ssq = kb.sbuf("ssq", [128, 2], F32, es)
                rs = kb.sbuf("rs", [128, 2], F32, es)
                rs2 = kb.sbuf("rs2", [128, 2], F32, es)
                rstd = kb.sbuf("rstd", [128, 2], F32, es)
                tm1 = kb.sbuf("tm1", [128, 256], F32, es)
                tm2 = kb.sbuf("tm2", [128, 256], F32, es)
                cT = kb.sbuf("cT", [128, 384], BF16, es)
                kp = kb.sbuf("kp", [128, 128], BF16, es)
                q32 = kb.sbuf("q32", [128, 768], F32, es)
                qb = kb.sbuf("qb", [128, 768], BF16, es)
                for t in range(NT):
                    uu, cb = uus[t % 2], cbs[t % 2]
                    cm, sm = cosT.t[:, tb + t, 0:16], sinT.t[:, tb + t, 0:16]
                    kb.dma("sp", uu.t[:], u_d.t[t * 128:(t + 1) * 128, 0:416], R=[u_d.tr], W=[uu.tr])
                    kb.op("act", lambda e: e.activation(out=junk.t[:, 0:384], in_=uu.t[:, 0:384], func=AF.Square),
                          R=[uu.tr], W=[junk.tr])
                    kb.op("dve", lambda e: e.reduce_sum(out=ssq.t[:, 0:1], in_=junk.t[:, 0:256], axis=AX.X), R=[junk.tr], W=[ssq.tr])
                    kb.op("dve", lambda e: e.reduce_sum(out=ssq.t[:, 1:2], in_=junk.t[:, 256:384], axis=AX.X), R=[junk.tr], Wa=[ssq.tr])
                    kb.op("dve", lambda e: e.tensor_scalar(out=rs.t[:, 0:1], in0=ssq.t[:, 0:1], scalar1=1.0 / 256, scalar2=RMS_EPS,
                                                           op0=ALU.mult, op1=ALU.add), R=[ssq.tr], W=[rs.tr])
                    kb.op("dve", lambda e: e.tensor_scalar(out=rs.t[:, 1:2], in0=ssq.t[:, 1:2], scalar1=1.0 / 128, scalar2=RMS_EPS,
                                                           op0=ALU.mult, op1=ALU.add), R=[ssq.tr], Wa=[rs.tr])
                    if t == 0:
                        STOP("P2b1")
                    kb.op("act", lambda e: e.activation(out=rs2.t[:], in_=rs.t[:], func=AF.Sqrt), R=[rs.tr], W=[rs2.tr])
                    kb.op("dve", lambda e: e.reciprocal(out=rstd.t[:], in_=rs2.t[:]), R=[rs2.tr], W=[rstd.tr])
                    kb.op("dve", lambda e: e.tensor_scalar(out=cb.t[:, 0:256], in0=uu.t[:, 0:256], scalar1=rstd.t[:, 0:1], scalar2=None,
                                                           op0=ALU.mult), R=[uu.tr, rstd.tr], W=[cb.tr])
                    kb.op("dve", lambda e: e.tensor_scalar(out=cb.t[:, 256:384], in0=uu.t[:, 256:384], scalar1=rstd.t[:, 1:2], scalar2=None,
                                                           op0=ALU.mult), R=[uu.tr, rstd.tr], Wa=[cb.tr])
                    if t == 0:
                        STOP("P2b3")
                    rope2("dve", uu.t[:, 384:416].rearrange("p (h d) -> p h d", h=1), cb.t[:, 448:480].rearrange("p (h d) -> p h d", h=1),
                          1, 16, cm, sm, tm1, tm2, [uu.tr], [cb.tr])
                    if t == 0:
                        STOP("P2b4")
                    pa, pt = psbf(0, 0)
                    tp(pa[:, 0:128], cb.t[:, 0:128], [cb.tr], pt, True)
                    tp(pa[:, 128:256], cb.t[:, 128:256], [cb.tr], pt, False)
                    tp(pa[:, 256:384], cb.t[:, 256:384], [cb.tr], pt, False)
                    tp(pa[0:96, 384:512], cb.t[:, 384:480], [cb.tr], pt, False)
                    kb.op("act", lambda e: e.copy(out=cT.t[:], in_=pa[:, 0:384]), R=[pt], W=[cT.tr])
                    if t == 0:
                        STOP("P2b6")
                    kb.op("act", lambda e: e.copy(out=kp.t[64:96, :], in_=pa[64:96, 384:512]), R=[pt], W=[kp.tr])
                    for h in range(8):
                        kb.op("dve", lambda e: e.tensor_copy(out=KT.t[64:96, h, t * 128:(t + 1) * 128], in_=kp.t[64:96, :]),
                              R=[kp.tr], Wa=[KT.tr])
                    if t == 0:
                        STOP("P2b")
                    for half, cw in ((0, 512), (1, 256)):
                        pq, pqt = psb(1, half)
                        for kc in range(2):
                            mm(pq[:, 0:cw], cT.t[:, kc * 128:(kc + 1) * 128], w_uq.t[:, kc, half * 512:half * 512 + cw], kc == 0, kc == 1,
                               [cT.tr, w_uq.tr], pqt, kc == 0)
                        kb.op("act", lambda e: e.copy(out=q32.t[:, half * 512:half * 512 + cw], in_=pq[:, 0:cw]), R=[pqt],
                              W=[q32.tr] if half == 0 else (), Wa=[q32.tr] if half else ())
                    kb.op("dve", lambda e: e.tensor_copy(out=qb.t[:], in_=q32.t[:]), R=[q32.tr], W=[qb.tr])
                    rope2("dve", q32.t[:].rearrange("p (h d) -> p h d", h=8)[:, :, 64:96],
                          qb.t[:].rearrange("p (h d) -> p h d", h=8)[:, :, 64:96], 8, 16, cm, sm, tm1, tm2, [q32.tr], [qb.tr])
                    pa2, pt2 = psbf(0, 1)
                    for h in range(8):
                        tp(pa2[0:96, h * 128:(h + 1) * 128], qb.t[:, h * 96:(h + 1) * 96], [qb.tr], pt2, h == 0)
                    kb.op("act", lambda e: e.copy(out=QT.t[0:96, :, t * 128:(t + 1) * 128],
                                                  in_=pa2[0:96, :].rearrange("p (h t) -> p h t", h=8)), R=[pt2], Wa=[QT.tr])
                    if t == 0:
                        STOP("P2c")
                    for hb in range(2):
                        pk, pkt = psb(2, hb)
                        for hh in range(4):
                            h = hb * 4 + hh
                            mm(pk[0:64, hh * 128:(hh + 1) * 128], w_ukv.t[:, h * 128:h * 128 + 64], cT.t[:, 256:384], True, True,
                               [cT.tr, w_ukv.tr], pkt, hh == 0)
                        kb.op("dve", lambda e: e.tensor_copy(out=KT.t[0:64, hb * 4:hb * 4 + 4, t * 128:(t + 1) * 128],
                                                             in_=pk[0:64, :].rearrange("p (h t) -> p h t", h=4)), R=[pkt], Wa=[KT.tr])
                    pv, pvt = psb(3, 0)
                    for h in range(8):
                        mm(pv[:, h * 64:(h + 1) * 64], cT.t[:, 256:384], w_ukv.t[:, h * 128 + 64:h * 128 + 128], True, True,
                           [cT.tr, w_ukv.tr], pvt, h == 0)
                    kb.op("act", lambda e: e.copy(out=VA.t[:, t, :, 0:64], in_=pv[:, 0:512].rearrange("p (h d) -> p h d", h=8)),
                          R=[pvt], Wa=[VA.tr])
                STOP("P2d")

                def m_mask(c, kb_, j0):
                    if kb_ >= 4 * c:
                        return dmask.t[:, 0, :], [dmask.tr], 128
                    return None
                attention(es, 8, None, lambda h, kb_: KT.t[0:96, h, kb_ * 128:(kb_ + 1) * 128],
                          lambda h, q0, q1: QT.t[0:96, h, q0:q1], lambda h, kb_: VA.t[:, kb_, h, :],
                          float(96 ** -0.5), m_mask, Ost, 0, [QT.tr, KT.tr, VA.tr])
                for t in range(NT):
                    kb.dma("sp", O_d.t[t * 128:(t + 1) * 128, 0:512], Ost.t[:, t, :], R=[Ost.tr], Wa=[O_d.tr])
                kb.barrier()
            if li == 0 and s == 0:
                tap("O_mla", O_d.t[:, 0:512], [O_d.tr], [S, 512], BF16)
            STOP("P2")

            with contextlib.ExitStack() as es:
                qTd = kb.sbuf("qTd", [64, 4, S], BF16, es)
                kTd = kb.sbuf("kTd", [64, S], BF16, es)
                VAd = kb.sbuf("VAd", [128, NT, 65], BF16, es)
                Osd = kb.sbuf("Osd", [128, NT, 256], BF16, es)
                offs = {}
                o_ = 0
                for b in range(2, NT):
                    offs[b] = o_
                    o_ += 128 * (b + 1)
                SC = kb.sbuf("SC", [128, o_], F32, es)
                lo = kb.sbuf("bs_lo", [128, 14], F32, es)
                hi = kb.sbuf("bs_hi", [128, 14], F32, es)
                wv = kb.sbuf("bs_w", [128, 14], F32, es)
                mid = kb.sbuf("bs_mid", [128, 14], F32, es)
                cnt = kb.sbuf("bs_cnt", [128, 14], F32, es)
                gef = kb.sbuf("bs_ge", [128, 14], F32, es)
                kb.op("dve", lambda e: e.memset(VAd.t[:].rearrange("p a b -> p (a b)"), 1.0), W=[VAd.tr])
                with contextlib.ExitStack() as es2:
                    QIT = kb.sbuf("QIT", [32, 8, S], BF16, es2)
                    kiT = kb.sbuf("kiT", [32, S], BF16, es2)
                    wiT = kb.sbuf("wiT", [32, NT, 128], BF16, es2)
                    kb.op("dve", lambda e: e.memset(wiT.t[:].rearrange("p a b -> p (a b)"), 0.0), W=[wiT.tr])
                    uds = [kb.sbuf("ud%d" % i_, [128, 680], F32, es2) for i_ in range(2)]
                    dbs = [kb.sbuf("db%d" % i_, [128, 680], BF16, es2) for i_ in range(2)]
                    tm1 = kb.sbuf("tm1d", [128, 64], F32, es2)
                    tm2 = kb.sbuf("tm2d", [128, 64], F32, es2)
                    Rg = kb.sbuf("Rg", [128, 8, 512], BF16, es2)
                    Wdb = kb.sbuf("Wdb", [128, 8, 128], BF16, es2)
                    wrep = kb.sbuf("wrep", [128, 128], BF16, es2)
                    ZLb = kb.sbuf("ZLb", [32, 8, 128], BF16, es2)
                    for t in range(NT):
                        ud, db = uds[t % 2], dbs[t % 2]
                        kb.dma("sp", ud.t[:], u_d.t[t * 128:(t + 1) * 128, 416:1096], R=[u_d.tr], W=[ud.tr])
                        kb.op("dve", lambda e: e.tensor_copy(out=db.t[:], in_=ud.t[:]), R=[ud.tr], W=[db.tr])
                        rope2("dve", ud.t[:, 0:320].rearrange("p (h d) -> p h d", h=5), db.t[:, 0:320].rearrange("p (h d) -> p h d", h=5),
                              5, 8, cosT.t[:, tb + t, 16:24], sinT.t[:, tb + t, 16:24], tm1, tm2, [ud.tr], [db.tr])
                        rope2("dve", ud.t[:, 384:672].rearrange("p (h d) -> p h d", h=9), db.t[:, 384:672].rearrange("p (h d) -> p h d", h=9),
                              9, 4, cosT.t[:, tb + t, 24:28], sinT.t[:, tb + t, 24:28], tm1, tm2, [ud.tr], [db.tr])
                        kb.op("act", lambda e: e.copy(out=VAd.t[:, t, 0:64], in_=ud.t[:, 320:384]), R=[ud.tr], Wa=[VAd.tr])
                        pA, ptA = psbf(0, 0)
                        pB, ptB = psbf(0, 1)
                        for h in range(4):
                            tp(pA[0:64, h * 128:(h + 1) * 128], db.t[:, h * 64:(h + 1) * 64], [db.tr], ptA, h == 0)
                        tp(pA[0:64, 512:640], db.t[:, 256:320], [db.tr], ptA, False)
                        tp(pA[0:32, 640:768], db.t[:, 640:672], [db.tr], ptA, False)
                        tp(pA[0:8, 768:896], db.t[:, 672:680], [db.tr], ptA, False)
                        for h in range(8):
                            tp(pB[0:32, h * 128:(h + 1) * 128], db.t[:, 384 + 32 * h:416 + 32 * h], [db.tr], ptB, h == 0)
                        kb.op("act", lambda e: e.copy(out=qTd.t[0:64, :, t * 128:(t + 1) * 128],
                                                      in_=pA[0:64, 0:512].rearrange("p (k t) -> p k t", k=4)), R=[ptA], Wa=[qTd.tr])
                        kb.op("act", lambda e: e.copy(out=kTd.t[0:64, t * 128:(t + 1) * 128], in_=pA[0:64, 512:640]), R=[ptA], Wa=[kTd.tr])
                        kb.op("act", lambda e: e.copy(out=kiT.t[0:32, t * 128:(t + 1) * 128], in_=pA[0:32, 640:768]), R=[ptA], Wa=[kiT.tr])
                        kb.op("act", lambda e: e.copy(out=wiT.t[0:8, t, :], in_=pA[0:8, 768:896]), R=[ptA], Wa=[wiT.tr])
                        kb.op("act", lambda e: e.copy(out=QIT.t[0:32, :, t * 128:(t + 1) * 128],
                                                      in_=pB[0:32, 0:1024].rearrange("p (k t) -> p k t", k=8)), R=[ptB], Wa=[QIT.tr])
                    STOP("P3a")
                    ev = 0
                    for b in range(2, NT):
                        N = 128 * (b + 1)
                        pw, pwt = psb(3, 0)
                        mm(pw[:, 0:128], rt.t[0:32, :], wiT.t[0:32, b, :], True, True, [rt.tr, wiT.tr], pwt, True)
                        kb.op("act", lambda e: e.copy(out=wrep.t[:], in_=pw[:, 0:128]), R=[pwt], W=[wrep.tr])
                        kb.op("dve", lambda e: e.tensor_tensor(out=Wdb.t[:], in0=wrep.t[:].unsqueeze(1).to_broadcast([128, 8, 128]),
                                                               in1=selmask.t[:], op=ALU.mult), R=[wrep.tr, selmask.tr], W=[Wdb.tr])
                        for g in range(8):
                            kb.op("dve", lambda e: e.tensor_copy(
                                out=ZLb.t[0:32, g, :].rearrange("p (h q) -> p h q", h=8),
                                in_=QIT.t[0:32, :, b * 128 + 16 * g:b * 128 + 16 * g + 16]), R=[QIT.tr],
                                W=[ZLb.tr] if g == 0 else (), Wa=[ZLb.tr] if g else ())
                        if b == 2:
                            STOP("P3b0")
                        for kc in range((N + 511) // 512):
                            k0 = kc * 512
                            ncol = min(512, N - k0)
                            for g in range(8):
                                zv, zt_ = psb(0, g % 2)
                                mm(zv[:, 0:ncol], ZLb.t[0:32, g, :], kiT.t[0:32, k0:k0 + ncol], True, True, [ZLb.tr, kiT.tr], zt_, True)
                                if g % 2 == 0:
                                    kb.op("act", lambda e: e.activation(out=Rg.t[:, g, 0:ncol], in_=zv[:, 0:ncol], func=AF.Relu),
                                          R=[zt_], Wa=[Rg.k(g)])
                                else:
                                    kb.op("dve", lambda e: e.tensor_scalar(out=Rg.t[:, g, 0:ncol], in0=zv[:, 0:ncol], scalar1=0.0, scalar2=None,
                                                                           op0=ALU.max), R=[zt_], Wa=[Rg.k(g)])
                            if b == 2 and kc == 0:
                                STOP("P3b1")
                            scv, sct = psb(1, ev % 2)
                            ev += 1
                            for g in range(8):
                                mm(scv[:, 0:ncol], Wdb.t[:, g, :], Rg.t[:, g, 0:ncol], g == 0, g == 7, [Wdb.tr, Rg.k(g)], sct, g == 0)
                            if b == 2 and kc == 0:
                                STOP("P3b2")
                            last = (k0 + ncol == N)
                            nplain = ncol - 128 if last else ncol
                            if nplain > 0:
                                kb.op("act", lambda e: e.copy(out=SC.t[:, offs[b] + k0:offs[b] + k0 + nplain], in_=scv[:, 0:nplain]),
                                      R=[sct], Wa=[SC.k(b)])
                            if b == 2 and kc == 0:
                                STOP("P3b2a")
                            if last:
                                kb.op("act", lambda e: e.copy(out=SC.t[:, offs[b] + N - 128:offs[b] + N], in_=scv[:, ncol - 128:ncol]),
                                      R=[sct], Wa=[SC.k(b)])
                                kb.op("pool", lambda e: e.tensor_tensor(out=SC.t[:, offs[b] + N - 128:offs[b] + N],
                                                                       in0=SC.t[:, offs[b] + N - 128:offs[b] + N],
                                                                       in1=tribias.t[:], op=ALU.add), R=[SC.k(b), tribias.tr], Wa=[SC.k(b)])
                        if b == 2:
                            STOP("P3b3")
                        if b == 4:
                            STOP("P3b4")
                    kb.barrier()
                STOP("P3b")
                junkb = kb.sbuf("junkb", [128, S], BF16, es)
                tmpb = kb.sbuf("bs_tmp", [128, 14], F32, es)
                sct_all = [SC.k(b) for b in range(2, NT)]
                for i_, b in enumerate(range(2, NT)):
                    N = 128 * (b + 1)
                    kb.op("dve", lambda e: e.reduce_max(out=hi.t[:, i_:i_ + 1], in_=SC.t[:, offs[b]:offs[b] + N], axis=AX.X),
                          R=[SC.k(b)], Wa=[hi.tr])
                    kb.op("dve", lambda e: e.tensor_reduce(out=lo.t[:, i_:i_ + 1], in_=SC.t[:, offs[b]:offs[b] + N - 128], axis=AX.X, op=ALU.min),
                          R=[SC.k(b)], Wa=[lo.tr])
                kb.op("dve", lambda e: e.tensor_scalar(out=lo.t[:], in0=lo.t[:], scalar1=-1.0, scalar2=None, op0=ALU.add), R=[lo.tr], W=[lo.tr])
                kb.op("dve", lambda e: e.tensor_scalar(out=hi.t[:], in0=hi.t[:], scalar1=1.0, scalar2=None, op0=ALU.add), R=[hi.tr], W=[hi.tr])
                kb.op("dve", lambda e: e.tensor_tensor(out=wv.t[:], in0=hi.t[:], in1=lo.t[:], op=ALU.subtract), R=[hi.tr, lo.tr], W=[wv.tr])
                junka = kb.sbuf("junka", [128, S], BF16, es)
                nmid = kb.sbuf("bs_nmid", [128, 14], F32, es)
                thrv = kb.sbuf("bs_thr", [128, 14], F32, es)
                for i_, b in enumerate(range(2, NT)):
                    kb.op("dve", lambda e: e.memset(thrv.t[:, i_:i_ + 1], 255.5 if i_ % 2 == 0 else float(511 - 128 * (b + 1))),
                          W=[thrv.tr] if i_ == 0 else (), Wa=[thrv.tr] if i_ else ())
                for itb in range(NBIS):
                    kb.op("dve", lambda e: e.tensor_scalar(out=wv.t[:], in0=wv.t[:], scalar1=0.5, scalar2=None, op0=ALU.mult), R=[wv.tr], W=[wv.tr])
                    kb.op("dve", lambda e: e.tensor_tensor(out=mid.t[:], in0=lo.t[:], in1=wv.t[:], op=ALU.add), R=[lo.tr, wv.tr], W=[mid.tr])
                    kb.op("dve", lambda e: e.tensor_scalar(out=nmid.t[:], in0=mid.t[:], scalar1=-1.0, scalar2=None, op0=ALU.mult),
                          R=[mid.tr], W=[nmid.tr])
                    for i_, b in enumerate(range(2, NT)):
                        N = 128 * (b + 1)
                        if i_ % 2 == 0:
                            kb.op("dve", lambda e: e.tensor_scalar(out=junkb.t[:, 0:N], in0=SC.t[:, offs[b]:offs[b] + N], scalar1=mid.t[:, i_:i_ + 1],
                                                                   scalar2=None, op0=ALU.is_ge, op1=ALU.add, accum_out=cnt.t[:, i_:i_ + 1]),
                                  R=[SC.k(b), mid.tr], W=[junkb.tr], Wa=[cnt.k(i_)])
                        else:
                            kb.op("act", lambda e: e.activation(out=junka.t[:, 0:N], in_=SC.t[:, offs[b]:offs[b] + N], func=AF.Sign,
                                                                bias=nmid.t[:, i_:i_ + 1], scale=1.0, accum_out=cnt.t[:, i_:i_ + 1]),
                                  R=[SC.k(b), nmid.tr], W=[junka.tr], Wa=[cnt.k(i_)])
                    kb.op("dve", lambda e: e.tensor_tensor(out=gef.t[:], in0=cnt.t[:], in1=thrv.t[:], op=ALU.is_ge),
                          R=[cnt.k(i__) for i__ in range(14)] + [thrv.tr], W=[gef.tr])
                    kb.op("dve", lambda e: e.tensor_tensor(out=tmpb.t[:], in0=gef.t[:], in1=wv.t[:], op=ALU.mult), R=[gef.tr, wv.tr], W=[tmpb.tr])
                    kb.op("dve", lambda e: e.tensor_tensor(out=lo.t[:], in0=lo.t[:], in1=tmpb.t[:], op=ALU.add), R=[lo.tr, tmpb.tr], W=[lo.tr])
                STOP("P3c")
                mq = kb.sbuf("mq", [128, S], BF16, es)
                maskT = kb.sbuf("maskT", [128, NT, 512], BF16, es)
                Pb = [kb.sbuf("Pbd%d" % i_, [128, 512], BF16, es) for i_ in range(2)]
                rden = kb.sbuf("rdend", [128, 4], F32, es)
                tq = 0
                for c in range(4):
                    for j in range(4):
                        b = 4 * c + j
                        N = 128 * (b + 1)
                        if b >= 2:
                            kb.op("dve", lambda e: e.tensor_scalar(out=mq.t[:, 0:N], in0=SC.t[:, offs[b]:offs[b] + N], scalar1=lo.t[:, b - 2:b - 1],
                                                                   scalar2=None, op0=ALU.is_ge), R=[SC.k(b), lo.tr], W=[mq.tr])
                            for kb0 in range(0, b + 1, 8):
                                n_ = min(8, b + 1 - kb0)
                                pm_, pmt_ = psbf(2, tq % 2)
                                tq += 1
                                for i_ in range(n_):
                                    tp(pm_[:, i_ * 128:(i_ + 1) * 128], mq.t[:, (kb0 + i_) * 128:(kb0 + i_ + 1) * 128], [mq.tr], pmt_, i_ == 0)
                                kb.op("act", lambda e: e.copy(out=maskT.t[:, kb0:kb0 + n_, j * 128:(j + 1) * 128],
                                                              in_=pm_[:, 0:n_ * 128].rearrange("p (k q) -> p k q", k=n_)), R=[pmt_], Wa=[maskT.tr])
                        else:
                            for kb_ in range(b + 1):
                                kb.op("dve", lambda e: e.tensor_copy(out=maskT.t[:, kb_, j * 128:(j + 1) * 128],
                                                                     in_=dmask.t[:, 0 if kb_ == b else 7, :]), R=[dmask.tr], Wa=[maskT.tr])

                    def d_mask(c_, kb_, j0):
                        return maskT.t[:, kb_, 128 * j0:512], [maskT.tr], 512 - 128 * j0
                    attention((Pb, rden), 4, [c], lambda h, kb_: kTd.t[0:64, kb_ * 128:(kb_ + 1) * 128],
                              lambda h, q0, q1: qTd.t[0:64, h, q0:q1], lambda h, kb_: VAd.t[:, kb_, :],
                              0.125, d_mask, Osd, 0, [qTd.tr, kTd.tr, VAd.tr])
                for t in range(NT):
                    kb.dma("sp", O_d.t[t * 128:(t + 1) * 128, 512:768], Osd.t[:, t, :], R=[Osd.tr], Wa=[O_d.tr])
                kb.barrier()
            if li == 0 and s == 0:
                tap("O_dsa", O_d.t[:, 512:768], [O_d.tr], [S, 256], BF16)
            STOP("P3")

            es_wo = contextlib.ExitStack()
            w_o = kb.sbuf("w_o", [128, 8, D], BF16, es_wo)
            for kc in range(8):
                kb.dma("pool", w_o.t[:, kc, :], W["w_o"][kc * 128:(kc + 1) * 128, :], R=[Wtr["w_o"]], Wa=[w_o.tr],
                       max_dma_last_dim=4096)
            with contextlib.ExitStack() as es:
                qTc = kb.sbuf("qTc", [64, 12, S], BF16, es)
                kTc = kb.sbuf("kTc", [64, 12, S], BF16, es)
                VAc = kb.sbuf("VAc", [128, NT, 12, 65], BF16, es)
                Osc = kb.sbuf("Osc", [128, NT, 256], BF16, es)
                kb.op("dve", lambda e: e.memset(VAc.t[:].rearrange("p a b c -> p (a b c)"), 1.0), W=[VAc.tr])
                ucs = [kb.sbuf("uc%d" % i_, [128, 2304], F32, es) for i_ in range(2)]
                cbfs = [kb.sbuf("cbf%d" % i_, [128, 1536], BF16, es) for i_ in range(2)]
                tm1 = kb.sbuf("tm1c", [128, 256], F32, es)
                tm2 = kb.sbuf("tm2c", [128, 256], F32, es)
                for t in range(NT):
                    uc, cbf = ucs[t % 2], cbfs[t % 2]
                    ch, sh_ = cosT.t[:, tb + t, 16:24], sinT.t[:, tb + t, 16:24]
                    kb.dma("sp", uc.t[:], u_d.t[t * 128:(t + 1) * 128, 1096:3400], R=[u_d.tr], W=[uc.tr])
                    kb.op("dve", lambda e: e.tensor_copy(out=cbf.t[:], in_=uc.t[:, 0:1536]), R=[uc.tr], W=[cbf.tr])
                    rope2("dve", uc.t[:, 0:1536].rearrange("p (h d) -> p h d", h=24), cbf.t[:].rearrange("p (h d) -> p h d", h=24),
                          24, 8, ch, sh_, tm1, tm2, [uc.tr], [cbf.tr])
                    if t == 0:
                        STOP("P4a1")
                    kb.op("act", lambda e: e.copy(out=VAc.t[:, t, :, 0:64], in_=uc.t[:, 1536:2304].rearrange("p (h d) -> p h d", h=12)),
                          R=[uc.tr], Wa=[VAc.tr])
                    for (src0, dstb, bk) in ((0, qTc, 0), (768, kTc, 2)):
                        pA, ptA = psbf(bk, 0)
                        pB, ptB = psbf(bk, 1)
                        for h in range(12):
                            if h < 8:
                                tp(pA[0:64, h * 128:(h + 1) * 128], cbf.t[:, src0 + h * 64:src0 + (h + 1) * 64], [cbf.tr], ptA, h == 0)
                            else:
                                tp(pB[0:64, (h - 8) * 128:(h - 7) * 128], cbf.t[:, src0 + h * 64:src0 + (h + 1) * 64], [cbf.tr], ptB, h == 8)
                        kb.op("act", lambda e: e.copy(out=dstb.t[0:64, 0:8, t * 128:(t + 1) * 128],
                                                      in_=pA[0:64, 0:1024].rearrange("p (k t) -> p k t", k=8)), R=[ptA], Wa=[dstb.tr])
                        kb.op("act", lambda e: e.copy(out=dstb.t[0:64, 8:12, t * 128:(t + 1) * 128],
                                                      in_=pB[0:64, 0:512].rearrange("p (k t) -> p k t", k=4)), R=[ptB], Wa=[dstb.tr])
                STOP("P4a")
                Pc = [kb.sbuf("Pc%d" % i_, [128, 512], BF16, es) for i_ in range(2)]
                rdc = kb.sbuf("rdc", [128, 4], F32, es)
                Rops = [qTc.tr, kTc.tr, VAc.tr]
                dit = []
                for b in range(NT):
                    blocks = []
                    for kb_ in range(max(0, b - 1), b + 1):
                        blocks.append((0, kb_, 0 if kb_ == b else 1))
                    for kb_ in range(max(0, b - 4), b + 1):
                        d_ = b - kb_
                        blocks.append((1, kb_, 2 if d_ == 0 else (4 if d_ == 4 else 3)))
                    for kb_ in range(0, b + 1):
                        blocks.append((2, kb_, 5 if kb_ == b else 6))
                    for bi, (g, kb_, midx) in enumerate(blocks):
                        dit.append((b, bi, len(blocks), g, kb_, midx))

                def d_qk(i):
                    b, bi, nb, g, kb_, midx = dit[i]
                    sv_, st_ = psb(2, i % 2)
                    for jj in range(4):
                        hh = 4 * g + jj
                        mm(sv_[:, jj * 128:(jj + 1) * 128], kTc.t[0:64, hh, kb_ * 128:(kb_ + 1) * 128],
                           qTc.t[0:64, hh, b * 128:(b + 1) * 128], True, True, Rops, st_, jj == 0)

                d_qk(0)
                for i, (b, bi, nb, g, kb_, midx) in enumerate(dit):
                    if i + 1 < len(dit):
                        d_qk(i + 1)
                    accv, acct = psb(1, b % 2)
                    acc3 = accv[:, 0:260].rearrange("p (j d) -> p j d", j=4)
                    sv_, st_ = psb(2, i % 2)
                    P = Pc[i % 2]
                    kb.op("act", lambda e: e.activation(out=P.t[:], in_=sv_[:, 0:512], func=AF.Exp, scale=0.125), R=[st_], W=[P.tr])
                    kb.op("dve", lambda e: e.tensor_tensor(out=P.t[:].rearrange("p (j q) -> p j q", j=4),
                                                           in0=P.t[:].rearrange("p (j q) -> p j q", j=4),
                                                           in1=dmask.t[:, midx, :].unsqueeze(1).to_broadcast([128, 4, 128]), op=ALU.mult),
                          R=[P.tr, dmask.tr], W=[P.tr])
                    for jj in range(4):
                        mm(acc3[:, jj, :], P.t[:, jj * 128:(jj + 1) * 128], VAc.t[:, kb_, 4 * g + jj, :],
                           (bi == 0 and jj == 0), (bi == nb - 1 and jj == 3), [P.tr] + Rops, acct, (bi == 0 and jj == 0))
                    if bi == nb - 1:
                        kb.op("dve", lambda e: e.reciprocal(out=rdc.t[:, 0:4], in_=acc3[:, :, 64]), R=[acct], W=[rdc.tr])
                        kb.op("dve", lambda e: e.tensor_tensor(out=Osc.t[:, b, :].rearrange("p (j d) -> p j d", j=4), in0=acc3[:, :, 0:64],
                                                               in1=rdc.t[:, 0:4].unsqueeze(2).to_broadcast([128, 4, 64]), op=ALU.mult),
                              R=[acct, rdc.tr], Wa=[Osc.tr])
                for t in range(NT):
                    kb.dma("sp", O_d.t[t * 128:(t + 1) * 128, 768:1024], Osc.t[:, t, :], R=[Osc.tr], Wa=[O_d.tr])
                kb.barrier()
            if li == 0 and s == 0:
                tap("O_dil", O_d.t[:, 768:1024], [O_d.tr], [S, 256], BF16)
            STOP("P4")

            with contextlib.ExitStack() as es:
                yacc = kb.sbuf("yacc", [128, NT, D], F32, es)
                x1T = kb.sbuf("x1T", [128, 8, S], BF16, es)
                gate = kb.sbuf("gate", [128, NT, 32], F32, es)
                lng = kb.sbuf("lng", [128, 4, D], F32, es)
                for i_, n_ in enumerate(("ln1_g", "ln1_b", "ln2_g", "ln2_b")):
                    kb.dma("sp", lng.t[:, i_, :], W[n_].rearrange("(o d) -> o d", o=1).partition_broadcast(128), R=[Wtr[n_]], Wa=[lng.tr])
                zb = kb.sbuf("zb", [128, D], F32, es)
                st6 = kb.sbuf("st6", [128, 2, 6], F32, es)
                mv = kb.sbuf("mv", [128, 2], F32, es)
                r1 = kb.sbuf("r1", [128, 1], F32, es)
                r2 = kb.sbuf("r2", [128, 1], F32, es)
                r3 = kb.sbuf("r3", [128, 1], F32, es)

                def layer_norm(src_ap, src_tr, gi, out_ap, out_tr):
                    for hh in range(2):
                        kb.op("dve", lambda e: e.bn_stats(out=st6.t[:, hh, :], in_=src_ap[:, hh * 512:(hh + 1) * 512]),
                              R=src_tr, W=[st6.tr] if hh == 0 else (), Wa=[st6.tr] if hh else ())
                    kb.op("dve", lambda e: e.bn_aggr(out=mv.t[:], in_=st6.t[:].rearrange("p a b -> p (a b)")), R=[st6.tr], W=[mv.tr])
                    kb.op("dve", lambda e: e.tensor_scalar(out=r1.t[:], in0=mv.t[:, 1:2], scalar1=LN_EPS, scalar2=None, op0=ALU.add),
                          R=[mv.tr], W=[r1.tr])
                    kb.op("act", lambda e: e.activation(out=r2.t[:], in_=r1.t[:], func=AF.Sqrt), R=[r1.tr], W=[r2.tr])
                    kb.op("dve", lambda e: e.reciprocal(out=r3.t[:], in_=r2.t[:]), R=[r2.tr], W=[r3.tr])
                    kb.op("dve", lambda e: e.tensor_scalar(out=zb.t[:], in0=src_ap, scalar1=mv.t[:, 0:1], scalar2=r3.t[:, 0:1],
                                                           op0=ALU.subtract, op1=ALU.mult), R=list(src_tr) + [mv.tr, r3.tr], W=[zb.tr])
                    kb.op("dve", lambda e: e.tensor_tensor(out=zb.t[:], in0=zb.t[:], in1=lng.t[:, gi, :], op=ALU.mult),
                          R=[zb.tr, lng.tr], W=[zb.tr])
                    kb.op("dve", lambda e: e.tensor_tensor(out=out_ap, in0=zb.t[:], in1=lng.t[:, gi + 1, :], op=ALU.add),
                          R=[zb.tr, lng.tr], W=out_tr)

                with contextlib.ExitStack() as es5:
                    rwf = kb.sbuf("rwf", [128, 8, 36], F32, es5)
                    rw = kb.sbuf("rw", [128, 8, 36], BF16, es5)
                    rbias = kb.sbuf("rbias", [128, 36], F32, es5)
                    kb.dma("sp", rwf.t[:, :, 0:4], W["router_group_w"].rearrange("(k p) g -> p k g", p=128), R=[Wtr["router_group_w"]],
                           Wa=[rwf.tr])
                    for g_ in range(4):
                        kb.dma("sp", rwf.t[:, :, 4 + 8 * g_:12 + 8 * g_], W["router_expert_w"][g_].rearrange("(k p) e -> p k e", p=128),
                               R=[Wtr["router_expert_w"]], Wa=[rwf.tr])
                    kb.op("dve", lambda e: e.tensor_copy(out=rw.t[:], in_=rwf.t[:]), R=[rwf.tr], W=[rw.tr])
                    kb.dma("sp", rbias.t[:, 0:4], W["router_group_b"].rearrange("(o g) -> o g", o=1).partition_broadcast(128),
                           R=[Wtr["router_group_b"]], Wa=[rbias.tr])
                    kb.dma("sp", rbias.t[:, 4:36], W["router_expert_b"].rearrange("(o g) e -> o (g e)", o=1).partition_broadcast(128),
                           R=[Wtr["router_expert_b"]], Wa=[rbias.tr])
                    Ots = [kb.sbuf("Ot%d" % i, [128, D], BF16, es5) for i in range(2)]
                    OTs = [kb.sbuf("OT%d" % i, [128, 8, 128], BF16, es5) for i in range(2)]
                    xts = [kb.sbuf("xq%d" % i, [128, D], F32, es5) for i in range(2)]
                    zs = kb.sbuf("zs", [128, D], F32, es5)
                    x1 = kb.sbuf("x1", [128, D], F32, es5)
                    x1b = kb.sbuf("x1b", [128, D], BF16, es5)
                    lgt = kb.sbuf("lgt", [128, 36], F32, es5)
                    sm = {n_: kb.sbuf("rt_" + n_, [128, w_], F32, es5) for n_, w_ in
                          (("gm", 1), ("ghot", 4), ("ng", 1), ("eg", 4), ("sg", 1), ("ptop", 1), ("tmp", 32), ("le", 8),
                           ("m8", 8), ("nm1", 1), ("ex", 8), ("exm", 8), ("den", 1), ("rd", 1), ("fac", 1), ("ge", 8))}
                    for t in range(NT):
                        Ot, OT, xt = Ots[t % 2], OTs[t % 2], xts[t % 2]
                        kb.dma("sp", Ot.t[:], O_d.t[t * 128:(t + 1) * 128, :], R=[O_d.tr], W=[Ot.tr])
                        kb.dma("sp", xt.t[:], x_src.t[s, t * 128:(t + 1) * 128, :], R=[x_src.k(s)], W=[xt.tr])
                        pa, pt = psbf(0, t % 2)
                        for kc in range(8):
                            tp(pa[:, kc * 128:(kc + 1) * 128], Ot.t[:, kc * 128:(kc + 1) * 128], [Ot.tr], pt, kc == 0)
                        kb.op("act", lambda e: e.copy(out=OT.t[:].rearrange("p k t -> p (k t)"), in_=pa), R=[pt], W=[OT.tr])
                        for half in range(2):
                            pm, pmt = psb(1, half)
                            for kc in range(8):
                                mm(pm[:, 0:512], OT.t[:, kc, :], w_o.t[:, kc, half * 512:(half + 1) * 512], kc == 0, kc == 7,
                                   [OT.tr, w_o.tr], pmt, kc == 0)
                            kb.op("dve", lambda e: e.scalar_tensor_tensor(out=zs.t[:, half * 512:(half + 1) * 512],
                                                                          in0=xt.t[:, half * 512:(half + 1) * 512], scalar=ALPHA,
                                                                          in1=pm[:, 0:512], op0=ALU.mult, op1=ALU.add),
                                  R=[xt.tr, pmt], W=[zs.tr] if half == 0 else (), Wa=[zs.tr] if half else ())
                        layer_norm(zs.t[:], [zs.tr], 0, x1.t[:], [x1.tr])
                        kb.op("act", lambda e: e.mul(out=yacc.t[:, t, :], in_=x1.t[:], mul=ALPHA), R=[x1.tr], W=[yacc.k(t)])
                        kb.op("dve", lambda e: e.tensor_copy(out=x1b.t[:], in_=x1.t[:]), R=[x1.tr], W=[x1b.tr])
                        pa2, pt2 = psbf(2, t % 2)
                        for kc in range(8):
                            tp(pa2[:, kc * 128:(kc + 1) * 128], x1b.t[:, kc * 128:(kc + 1) * 128], [x1b.tr], pt2, kc == 0)
                        kb.op("act", lambda e: e.copy(out=x1T.t[:, :, t * 128:(t + 1) * 128],
                                                      in_=pa2.rearrange("p (k t) -> p k t", k=8)), R=[pt2], Wa=[x1T.k(t)])
                        pr, prt = psb(3, t % 2)
                        for kc in range(8):
                            mm(pr[:, 0:36], x1T.t[:, kc, t * 128:(t + 1) * 128], rw.t[:, kc, :], kc == 0, kc == 7, [x1T.k(t), rw.tr], prt, kc == 0)
                        def so(eng, fn, R, Wn):
                            kb.op(eng, fn, R=[sm[r_].tr if isinstance(r_, str) else r_ for r_ in R], W=[sm[Wn].tr])
                        kb.op("dve", lambda e: e.tensor_tensor(out=lgt.t[:], in0=pr[:, 0:36], in1=rbias.t[:], op=ALU.add),
                              R=[prt, rbias.tr], W=[lgt.tr])
                        so("dve", lambda e: e.reduce_max(out=sm["gm"].t[:], in_=lgt.t[:, 0:4], axis=AX.X), [lgt.tr], "gm")
                        so("dve", lambda e: e.tensor_scalar(out=sm["ghot"].t[:], in0=lgt.t[:, 0:4], scalar1=sm["gm"].t[:, 0:1], scalar2=None,
                                                            op0=ALU.is_ge), [lgt.tr, "gm"], "ghot")
                        so("dve", lambda e: e.tensor_scalar(out=sm["ng"].t[:], in0=sm["gm"].t[:], scalar1=-1.0, scalar2=None, op0=ALU.mult),
                           ["gm"], "ng")
                        so("act", lambda e: e.activation(out=sm["eg"].t[:], in_=lgt.t[:, 0:4], func=AF.Exp, bias=sm["ng"].t[:, 0:1]),
                           [lgt.tr, "ng"], "eg")
                        so("dve", lambda e: e.reduce_sum(out=sm["sg"].t[:], in_=sm["eg"].t[:], axis=AX.X), ["eg"], "sg")
                        so("dve", lambda e: e.reciprocal(out=sm["ptop"].t[:], in_=sm["sg"].t[:]), ["sg"], "ptop")
                        so("dve", lambda e: e.tensor_tensor(out=sm["tmp"].t[:].rearrange("p (g e) -> p g e", g=4),
                                                            in0=lgt.t[:, 4:36].rearrange("p (g e) -> p g e", g=4),
                                                            in1=sm["ghot"].t[:].unsqueeze(2).to_broadcast([128, 4, 8]), op=ALU.mult),
                           [lgt.tr, "ghot"], "tmp")
                        so("dve", lambda e: e.tensor_reduce(out=sm["le"].t[:], in_=sm["tmp"].t[:].rearrange("p (g e) -> p e g", g=4),
                                                            axis=AX.X, op=ALU.add), ["tmp"], "le")
                        so("dve", lambda e: e.max(out=sm["m8"].t[:], in_=sm["le"].t[:]), ["le"], "m8")
                        so("dve", lambda e: e.tensor_scalar(out=sm["nm1"].t[:], in0=sm["m8"].t[:, 0:1], scalar1=-1.0, scalar2=None,
                                                            op0=ALU.mult), ["m8"], "nm1")
                        so("act", lambda e: e.activation(out=sm["ex"].t[:], in_=sm["le"].t[:], func=AF.Exp, bias=sm["nm1"].t[:, 0:1]),
                           ["le", "nm1"], "ex")
                        so("dve", lambda e: e.scalar_tensor_tensor(out=sm["exm"].t[:], in0=sm["le"].t[:], scalar=sm["m8"].t[:, 1:2],
                                                                   in1=sm["ex"].t[:], op0=ALU.is_ge, op1=ALU.mult), ["le", "m8", "ex"], "exm")
                        so("dve", lambda e: e.reduce_sum(out=sm["den"].t[:], in_=sm["exm"].t[:], axis=AX.X), ["exm"], "den")
                        so("dve", lambda e: e.reciprocal(out=sm["rd"].t[:], in_=sm["den"].t[:]), ["den"], "rd")
                        so("dve", lambda e: e.tensor_tensor(out=sm["fac"].t[:], in0=sm["rd"].t[:], in1=sm["ptop"].t[:], op=ALU.mult),
                           ["rd", "ptop"], "fac")
                        so("dve", lambda e: e.tensor_scalar(out=sm["ge"].t[:], in0=sm["exm"].t[:], scalar1=sm["fac"].t[:, 0:1], scalar2=None,
                                                            op0=ALU.mult), ["exm", "fac"], "ge")
                        kb.op("dve", lambda e: e.tensor_tensor(out=gate.t[:, t, :].rearrange("p (g e) -> p g e", g=4),
                                                               in0=sm["ghot"].t[:].unsqueeze(2).to_broadcast([128, 4, 8]),
                                                               in1=sm["ge"].t[:].unsqueeze(1).to_broadcast([128, 4, 8]), op=ALU.mult),
                              R=[sm["ghot"].tr, sm["ge"].tr], W=[gate.k(t)])
                    kb.barrier()
                STOP("P5")
                with contextlib.ExitStack() as es6:
                    wgs = [kb.sbuf("wg%d" % i, [128, 8, 256], BF16, es6) for i in range(2)]
                    wus = [kb.sbuf("wu%d" % i, [128, 8, 256], BF16, es6) for i in range(2)]
                    wds = [kb.sbuf("wd%d" % i, [128, 2, D], BF16, es6) for i in range(2)]
                    sgs = [kb.sbuf("sg%d" % i, [128, 512], F32, es6) for i in range(2)]
                    aTs = [kb.sbuf("aT%d" % i, [128, 512], BF16, es6) for i in range(4)]
                    for e_ in range(32):
                        g_, ee = divmod(e_, 8)
                        wg, wu, wd = wgs[e_ % 2], wus[e_ % 2], wds[e_ % 2]
                        kb.dma("pool", wg.t[:], W["expert_w_gate"][g_, ee].rearrange("(k p) f -> p k f", p=128), R=[Wtr["expert_w_gate"]], W=[wg.tr])
                        kb.dma("pool", wu.t[:], W["expert_w_up"][g_, ee].rearrange("(k p) f -> p k f", p=128), R=[Wtr["expert_w_up"]], W=[wu.tr])
                        kb.dma("pool", wd.t[:], W["expert_w_down"][g_, ee].rearrange("(k p) d -> p k d", p=128), R=[Wtr["expert_w_down"]], W=[wd.tr])
                        for c in range(4):
                            x1tr = [x1T.k(4 * c + j) for j in range(4)]
                            for fc in range(2):
                                hg, hgt = psb(0, fc)
                                hu, hut = psb(1, fc)
                                for kc in range(8):
                                    mm(hg[:, 0:512], wg.t[:, kc, fc * 128:(fc + 1) * 128], x1T.t[:, kc, c * 512:(c + 1) * 512], kc == 0, kc == 7,
                                       [wg.tr] + x1tr, hgt, kc == 0)
                                for kc in range(8):
                                    mm(hu[:, 0:512], wu.t[:, kc, fc * 128:(fc + 1) * 128], x1T.t[:, kc, c * 512:(c + 1) * 512], kc == 0, kc == 7,
                                       [wu.tr] + x1tr, hut, kc == 0)
                                sgb = sgs[fc]
                                aT = aTs[(c % 2) * 2 + fc]
                                kb.op("act", lambda e: e.activation(out=sgb.t[:], in_=hg[:, 0:512], func=AF.Silu), R=[hgt], W=[sgb.tr])
                                kb.op("dve", lambda e: e.tensor_tensor(out=aT.t[:], in0=sgb.t[:], in1=hu[:, 0:512], op=ALU.mult),
                                      R=[sgb.tr, hut], W=[aT.tr])
                            for j in range(4):
                                t = 4 * c + j
                                for half in range(2):
                                    py, pyt = psb(2 + j % 2, half)
                                    for fc in range(2):
                                        aT = aTs[(c % 2) * 2 + fc]
                                        mm(py[:, 0:512], aT.t[:, j * 128:(j + 1) * 128], wd.t[:, fc, half * 512:(half + 1) * 512], fc == 0, fc == 1,
                                           [aT.tr, wd.tr], pyt, fc == 0)
                                    kb.op("dve", lambda e: e.scalar_tensor_tensor(
                                        out=yacc.t[:, t, half * 512:(half + 1) * 512], in0=py[:, 0:512], scalar=gate.t[:, t, e_:e_ + 1],
                                        in1=yacc.t[:, t, half * 512:(half + 1) * 512], op0=ALU.mult, op1=ALU.add),
                                        R=[pyt, gate.k(t), yacc.k(t)], W=[yacc.k(t)])
                    outs = [kb.sbuf("ob%d" % i, [128, D], F32, es6) for i in range(2)]
                    for t in range(NT):
                        ob = outs[t % 2]
                        layer_norm(yacc.t[:, t, :], [yacc.k(t)], 2, ob.t[:], [ob.tr])
                        kb.dma("sp", x_dst.t[s, t * 128:(t + 1) * 128, :], ob.t[:], R=[ob.tr], Wa=[x_dst.k(s)])
                    kb.barrier()
            es_wo.close()
            if li == nl - 1:
                out_tracks.append(y_out.k(s))

    try:
        body()
    except _Stop:
        pass
    kb.finish(out_tracks)
    return kb, tapd


def _wshapes(inputs):
    return {n: list(inputs[n].shape) for n in WNAMES}


def kernel(**inputs):
    x = np.ascontiguousarray(inputs["x"], dtype=np.float32)
    pos = np.ascontiguousarray(inputs["positions"], dtype=np.int32)
    ncores = 8
    nseq = x.shape[0] // ncores
    kb, _ = build(nseq, list(range(DEPTH)), _wshapes(inputs))
    hc = host_consts()
    in_maps = []
    for c in range(ncores):
        m = {"x": x[c * nseq:(c + 1) * nseq], "positions": pos[c * nseq:(c + 1) * nseq]}
        for n in WNAMES:
            m[n] = np.ascontiguousarray(inputs[n], dtype=np.float32)
        for n, v in hc.items():
            m["c_" + n] = v
        in_maps.append(m)
    res = run_bass_kernel_spmd(kb.nc, in_maps, core_ids=list(range(ncores)))
    return np.concatenate([r["y"] for r in res.results], axis=0)
```

```python
import contextlib
import numpy as np
import ml_dtypes
import concourse.bass as bass
import concourse.mybir as mybir
from concourse.bass_utils import run_bass_kernel_spmd

F32 = mybir.dt.float32
BF16 = mybir.dt.bfloat16
I32 = mybir.dt.int32
ALU = mybir.AluOpType
AF = mybir.ActivationFunctionType
AX = mybir.AxisListType

D = 1024
S = 2048
NT = S // 128
DEPTH = 4
INC = 3400
ALPHA = float((2.0 * DEPTH) ** 0.25)
LN_EPS = 1e-5
RMS_EPS = 1e-6
TWO_PI = 6.283185307179586
C1 = 6.28125
C2 = TWO_PI - C1
MAGIC = 12582912.0
NBIS = 22


class Track:
    __slots__ = ("w", "r", "dsem", "dcnt")

    def __init__(self):
        self.w = {}
        self.r = {}
        self.dsem = None
        self.dcnt = 0


class Eng:
    def __init__(self, name, e, sem):
        self.name = name
        self.e = e
        self.sem = sem
        self.n = 0
        self.seen = {}


class Buf:
    def __init__(self, t):
        self.t = t
        self.tr = Track()
        self.sub = {}

    def k(self, key):
        if key not in self.sub:
            self.sub[key] = Track()
        return self.sub[key]

    def all(self):
        return [self.tr] + list(self.sub.values())

    def __getitem__(self, idx):
        return self.t[idx]


class KB:
    def __init__(self):
        self.nc = bass.Bass("TRN2", target_bir_lowering=False)
        self.es = contextlib.ExitStack()
        nc = self.nc
        self.E = {}
        for name, e in (("pe", nc.tensor), ("act", nc.scalar), ("dve", nc.vector),
                        ("pool", nc.gpsimd), ("sp", nc.sync)):
            sem = self.es.enter_context(nc.semaphore("sem_" + name))
            self.E[name] = Eng(name, e, sem)
        self.dsems = []
        self.ninst = 0
        self.pool_ds = []
        self.pool_i = 0

    def sbuf(self, name, shape, dtype, es=None):
        self.uid = getattr(self, "uid", 0) + 1
        return Buf((es or self.es).enter_context(self.nc.sbuf_tensor("sb%d_%s" % (self.uid, name), list(shape), dtype)))

    def psum(self, name, shape, dtype):
        return Buf(self.es.enter_context(self.nc.psum_tensor(name, list(shape), dtype)))

    def dram(self, name, shape, dtype, kind="Internal"):
        return Buf(self.nc.dram_tensor(name, list(shape), dtype, kind=kind))

    def _need(self, E, sem, val, owner, raw):
        if owner is E and (not raw or E.name == "pe"):
            return
        if E.seen.get(sem, 0) >= val:
            return
        E.e.wait_ge(sem, val)
        E.seen[sem] = val

    def _deps(self, E, R, W, Wa):
        for t in R:
            for sem, (val, owner) in t.w.items():
                self._need(E, sem, val, owner, True)
        for t in W:
            for sem, (val, owner) in t.w.items():
                self._need(E, sem, val, owner, False)
            for sem, (val, owner) in t.r.items():
                self._need(E, sem, val, owner, False)
        for t in Wa:
            for sem, (val, owner) in t.r.items():
                self._need(E, sem, val, owner, False)

    def _post(self, sem, val, owner, R, W, Wa):
        for t in R:
            t.r[sem] = (val, owner)
        for t in W:
            t.w = {sem: (val, owner)}
            t.r = {}
        for t in Wa:
            t.w[sem] = (val, owner)

    def op(self, eng, fn, R=(), W=(), Wa=(), inc=True):
        E = self.E[eng]
        self._deps(E, R, W, Wa)
        inst = fn(E.e)
        if inc:
            E.n += 1
            inst.then_inc(E.sem, 1)
            self._post(E.sem, E.n, E, R, W, Wa)
        else:
            self._post(E.sem, E.n + 1, E, R, W, Wa)
        self.ninst += 1
        return inst

    def dma(self, q, out_ap, in_ap, R=(), W=(), Wa=(), **kw):
        E = self.E[q]
        self._deps(E, R, W, Wa)
        tgt = (list(W) + list(Wa))[0]
        if tgt.dsem is None:
            if len(self.pool_ds) < 40:
                ds = [self.es.enter_context(self.nc.semaphore("ds%d" % len(self.dsems))), 0]
                self.pool_ds.append(ds)
                self.dsems.append(ds)
            else:
                ds = self.pool_ds[self.pool_i % 40]
                self.pool_i += 1
            tgt.dsem = ds
        ds = tgt.dsem
        inst = E.e.dma_start(out=out_ap, in_=in_ap, **kw)
        ds[1] += 16
        inst.then_inc(ds[0], 16)
        self._post(ds[0], ds[1], None, R, W, Wa)
        self.ninst += 1
        return inst

    def dedicate(self, track):
        ds = [self.es.enter_context(self.nc.semaphore("dd%d" % len(self.dsems))), 0]
        self.dsems.append(ds)
        track.dsem = ds

    def barrier(self):
        for E in self.E.values():
            for E2 in self.E.values():
                if E2 is not E and E2.n > 0:
                    self._need(E, E2.sem, E2.n, E2, True)
            for ds in self.dsems:
                if ds[1] > 0:
                    self._need(E, ds[0], ds[1], None, True)

    def finish(self, out_tracks):
        E = self.E["sp"]
        for t in out_tracks:
            for sem, (val, owner) in t.w.items():
                self._need(E, sem, val, owner, True)
        self.barrier()


def host_consts():
    c = {}
    c["ident"] = np.eye(128, dtype=np.float32).astype(ml_dtypes.bfloat16)
    k = np.arange(128)[:, None]
    q = np.arange(128)[None, :]
    dm = np.zeros((128, 8, 128), np.float32)
    dm[:, 0, :] = (k <= q)
    dm[:, 1, :] = (q <= k)
    dm[:, 2, :] = ((q - k) % 4 == 0) & (q >= k)
    dm[:, 3, :] = ((q - k) % 4 == 0)
    dm[:, 4, :] = ((q - k) % 4 == 0) & (q <= k)
    dm[:, 5, :] = ((q - k) % 16 == 0) & (q >= k)
    dm[:, 6, :] = ((q - k) % 16 == 0)
    dm[:, 7, :] = 1.0
    c["dmask"] = dm.astype(ml_dtypes.bfloat16)
    qq = np.arange(128)[:, None]
    kk = np.arange(128)[None, :]
    c["tribias"] = np.where(kk <= qq, 0.0, -1e30).astype(np.float32)
    inv = []
    for rot in (32, 16, 8):
        inv.append((500000.0 ** (-np.arange(0, rot, 2, dtype=np.float32) / np.float32(rot))).astype(np.float32))
    inv = np.concatenate(inv).astype(np.float32)
    c["inv"] = np.tile(inv[None, :], (128, 1)).astype(np.float32)
    p = np.arange(128)
    sel = np.zeros((128, 8, 128), np.float32)
    for g in range(8):
        sel[p, g, 16 * g + (p % 16)] = 1.0
    c["selmask"] = sel.astype(ml_dtypes.bfloat16)
    rt = np.zeros((32, 128), np.float32)
    rt[p // 16, p] = 1.0
    c["rt"] = rt.astype(ml_dtypes.bfloat16)
    return c


WNAMES = ["w_in", "mla_q_norm", "mla_kv_norm", "mla_w_uq", "mla_w_ukv", "w_o", "ln1_g", "ln1_b",
          "router_group_w", "router_group_b", "router_expert_w", "router_expert_b",
          "expert_w_gate", "expert_w_up", "expert_w_down", "ln2_g", "ln2_b"]


class _Stop(Exception):
    pass


def build(nseq, layers, wshapes, taps=None, stop_after=None):
    kb = KB()
    nc = kb.nc
    taps = taps or []
    x_in = kb.dram("x", [nseq, S, D], F32, kind="ExternalInput")
    pos_in = kb.dram("positions", [nseq, S], I32, kind="ExternalInput")
    y_out = kb.dram("y", [nseq, S, D], F32, kind="ExternalOutput")
    Wd_ = {n: kb.dram(n, wshapes[n], F32, kind="ExternalInput") for n in WNAMES}
    hc = host_consts()
    Cd = {n: kb.dram("c_" + n, list(v.shape), BF16 if v.dtype != np.float32 else F32, kind="ExternalInput")
          for n, v in hc.items()}
    tapd = {}
    nl = len(layers)
    xs = [kb.dram("xs%d" % i, [nseq, S, D], F32) for i in range(2)] if nl > 1 else []
    u_d = kb.dram("u_d", [S, INC], F32)

    ident = kb.sbuf("ident", [128, 128], BF16)
    dmask = kb.sbuf("dmask", [128, 8, 128], BF16)
    tribias = kb.sbuf("tribias", [128, 128], F32)
    inv = kb.sbuf("inv", [128, 28], F32)
    selmask = kb.sbuf("selmask", [128, 8, 128], BF16)
    rt = kb.sbuf("rt", [32, 128], BF16)
    cosT = kb.sbuf("cosT", [128, nseq * NT, 28], F32)
    sinT = kb.sbuf("sinT", [128, nseq * NT, 28], F32)
    for b_, n in ((ident, "ident"), (dmask, "dmask"), (tribias, "tribias"), (inv, "inv"),
                  (selmask, "selmask"), (rt, "rt")):
        kb.dma("sp", b_.t[:], Cd[n].t[:], R=[Cd[n].tr], W=[b_.tr])

    PS = [kb.psum("ps%d" % i, [128, 1024], F32) for i in range(4)]

    def psb(i, h):
        return PS[i].t[:, h * 512:(h + 1) * 512], PS[i].k(h)

    def psbf(i, h):
        return PS[i].t.bitcast(BF16)[:, h * 1024:(h + 1) * 1024], PS[i].k(h)

    with contextlib.ExitStack() as es:
        posi = kb.sbuf("posi", [128, nseq * NT], I32, es)
        posf = kb.sbuf("posf", [128, nseq * NT], F32, es)
        ang = kb.sbuf("ang", [128, nseq * NT, 28], F32, es)
        tt = kb.sbuf("rp_t", [128, nseq * NT, 28], F32, es)
        kf = kb.sbuf("rp_k", [128, nseq * NT, 28], F32, es)
        rr = kb.sbuf("rp_r", [128, nseq * NT, 28], F32, es)
        kb.dma("sp", posi.t[:].rearrange("p (s t) -> p s t", s=nseq),
               pos_in.t[:].rearrange("s (t p) -> p s t", p=128), R=[pos_in.tr], W=[posi.tr],
               allow_slow_non_contiguous=True)
        kb.op("dve", lambda e: e.tensor_copy(out=posf.t[:], in_=posi.t[:]), R=[posi.tr], W=[posf.tr])
        nt_all = nseq * NT
        kb.op("dve", lambda e: e.tensor_tensor(
            out=ang.t[:], in0=posf.t[:].unsqueeze(2).to_broadcast([128, nt_all, 28]),
            in1=inv.t[:].unsqueeze(1).to_broadcast([128, nt_all, 28]), op=ALU.mult),
            R=[posf.tr, inv.tr], W=[ang.tr])
        for dst, shift in ((sinT, 0.0), (cosT, float(np.pi / 2))):
            kb.op("dve", lambda e: e.tensor_scalar(out=tt.t[:], in0=ang.t[:], scalar1=shift, scalar2=float(1.0 / TWO_PI),
                                                   op0=ALU.add, op1=ALU.mult), R=[ang.tr], W=[tt.tr])
            kb.op("dve", lambda e: e.tensor_scalar(out=kf.t[:], in0=tt.t[:], scalar1=MAGIC, scalar2=-MAGIC,
                                                   op0=ALU.add, op1=ALU.add), R=[tt.tr], W=[kf.tr])
            kb.op("dve", lambda e: e.scalar_tensor_tensor(out=rr.t[:], in0=kf.t[:], scalar=-C1, in1=ang.t[:],
                                                          op0=ALU.mult, op1=ALU.add), R=[kf.tr, ang.tr], W=[rr.tr])
            kb.op("dve", lambda e: e.scalar_tensor_tensor(out=tt.t[:], in0=kf.t[:], scalar=-C2, in1=rr.t[:],
                                                          op0=ALU.mult, op1=ALU.add), R=[kf.tr, rr.tr], W=[tt.tr])
            kb.op("dve", lambda e: e.tensor_scalar(out=rr.t[:], in0=tt.t[:], scalar1=shift, scalar2=None,
                                                   op0=ALU.add), R=[tt.tr], W=[rr.tr])
            kb.op("dve", lambda e: e.tensor_scalar(out=tt.t[:], in0=rr.t[:], scalar1=3.1415925, scalar2=-3.1415925,
                                                   op0=ALU.min, op1=ALU.max), R=[rr.tr], W=[tt.tr])
            kb.op("act", lambda e: e.activation(out=dst.t[:], in_=tt.t[:], func=AF.Sin), R=[tt.tr], W=[dst.tr])
        kb.barrier()

    def rope(eng, src, dst, col0, nh, hd, half, cs, sn, tmp1, tmp2, R, Wt):
        def v(buf, off):
            return buf.t[:, col0:col0 + nh * hd].rearrange("p (h d) -> p h d", h=nh)[:, :, off:off + half]
        cb = cs.unsqueeze(1).to_broadcast([128, nh, half])
        sb = sn.unsqueeze(1).to_broadcast([128, nh, half])
        t1 = tmp1.t[:, 0:nh * half].rearrange("p (h d) -> p h d", h=nh)
        t2 = tmp2.t[:, 0:nh * half].rearrange("p (h d) -> p h d", h=nh)
        x1, x2 = v(src, 0), v(src, half)
        o1, o2 = v(dst, 0), v(dst, half)
        kb.op(eng, lambda e: e.tensor_tensor(out=t1, in0=x1, in1=cb, op=ALU.mult), R=R, W=[tmp1.tr])
        kb.op(eng, lambda e: e.tensor_tensor(out=t2, in0=x2, in1=sb, op=ALU.mult), R=R, W=[tmp2.tr])
        kb.op(eng, lambda e: e.tensor_tensor(out=o1, in0=t1, in1=t2, op=ALU.subtract), R=[tmp1.tr, tmp2.tr], Wa=Wt)
        kb.op(eng, lambda e: e.tensor_tensor(out=t1, in0=x2, in1=cb, op=ALU.mult), R=R, W=[tmp1.tr])
        kb.op(eng, lambda e: e.tensor_tensor(out=t2, in0=x1, in1=sb, op=ALU.mult), R=R, W=[tmp2.tr])
        kb.op(eng, lambda e: e.tensor_tensor(out=o2, in0=t1, in1=t2, op=ALU.add), R=[tmp1.tr, tmp2.tr], Wa=Wt)

    def rope2(eng, sv, dv, nh, half, cs, sn, tmp1, tmp2, R, Wt):
        cb_ = cs.unsqueeze(1).to_broadcast([128, nh, half])
        sb_ = sn.unsqueeze(1).to_broadcast([128, nh, half])
        t1 = tmp1.t[:, 0:nh * half].rearrange("p (h d) -> p h d", h=nh)
        t2 = tmp2.t[:, 0:nh * half].rearrange("p (h d) -> p h d", h=nh)
        x1, x2 = sv[:, :, 0:half], sv[:, :, half:2 * half]
        o1, o2 = dv[:, :, 0:half], dv[:, :, half:2 * half]
        kb.op(eng, lambda e: e.tensor_tensor(out=t1, in0=x1, in1=cb_, op=ALU.mult), R=R, W=[tmp1.tr])
        kb.op(eng, lambda e: e.tensor_tensor(out=t2, in0=x2, in1=sb_, op=ALU.mult), R=R, W=[tmp2.tr])
        kb.op(eng, lambda e: e.tensor_tensor(out=o1, in0=t1, in1=t2, op=ALU.subtract), R=[tmp1.tr, tmp2.tr], Wa=Wt)
        kb.op(eng, lambda e: e.tensor_tensor(out=t1, in0=x2, in1=cb_, op=ALU.mult), R=R, W=[tmp1.tr])
        kb.op(eng, lambda e: e.tensor_tensor(out=t2, in0=x1, in1=sb_, op=ALU.mult), R=R, W=[tmp2.tr])
        kb.op(eng, lambda e: e.tensor_tensor(out=o2, in0=t1, in1=t2, op=ALU.add), R=[tmp1.tr, tmp2.tr], Wa=Wt)

    def tap(name, ap, tracks, shape, dtype):
        if name in taps:
            d = kb.dram("tap_" + name, list(shape), dtype, kind="ExternalOutput")
            tapd[name] = d
            n0 = shape[0]
            for r0 in range(0, n0, 128):
                r1 = min(n0, r0 + 128)
                kb.dma("sp", d.t[r0:r1], ap[r0:r1], R=tracks, Wa=[d.tr])

    out_tracks = []
    kb.dedicate(u_d.tr)
    O_d = kb.dram("O_d", [S, D], BF16)
    kb.dedicate(O_d.tr)
    for xx in xs:
        for s_ in range(nseq):
            kb.dedicate(xx.k(s_))
    for s_ in range(nseq):
        kb.dedicate(y_out.k(s_))

    def mm(out, lhsT, rhs, start, stop, R, pt, first, inc=True):
        kb.op("pe", lambda e: e.matmul(out, lhsT=lhsT, rhs=rhs, start=start, stop=stop),
              R=R, W=[pt] if first else (), Wa=() if first else [pt], inc=inc)

    def tp(out, in_, R, pt, first):
        kb.op("pe", lambda e: e.transpose(out, in_, ident.t[:]),
              R=list(R) + [ident.tr], W=[pt] if first else (), Wa=() if first else [pt])

    def attention(es, nheads, nch_fn, lhs_fn, rhs_fn, v_fn, scale, mask_fn, Ost, ocol0, Rops):
        if isinstance(es, tuple):
            Pb, rden = es
        else:
            Pb = [kb.sbuf("Pb%d" % i, [128, 512], BF16, es) for i in range(2)]
            rden = kb.sbuf("rden", [128, 4], F32, es)
        chunks_ = list(nch_fn) if nch_fn is not None else list(range(4))
        iters = [(h, c, kb_) for h in range(nheads) for c in chunks_ for kb_ in range(4 * c + 4)]

        def emit_qk(i):
            h, c, kb_ = iters[i]
            j0 = max(0, kb_ - 4 * c)
            ncol = 512 - 128 * j0
            sv_, st_ = psb(0, i % 2)
            mm(sv_[:, 0:ncol], lhs_fn(h, kb_), rhs_fn(h, c * 512 + 128 * j0, (c + 1) * 512), True, True, Rops, st_, True)

        emit_qk(0)
        for i, (h, c, kb_) in enumerate(iters):
            if i + 1 < len(iters):
                emit_qk(i + 1)
            hc_i = (h * len(chunks_) + chunks_.index(c))
            accv, acct = psb(1, hc_i % 2)
            acc3 = accv[:, 0:260].rearrange("p (j d) -> p j d", j=4)
            j0 = max(0, kb_ - 4 * c)
            ncol = 512 - 128 * j0
            sv_, st_ = psb(0, i % 2)
            P = Pb[i % 2]
            kb.op("act", lambda e: e.activation(out=P.t[:, 0:ncol], in_=sv_[:, 0:ncol], func=AF.Exp, scale=scale),
                  R=[st_], W=[P.tr])
            mk = mask_fn(c, kb_, j0)
            if mk is not None:
                map_, mtr, mw = mk
                kb.op("dve", lambda e: e.tensor_tensor(out=P.t[:, 0:mw], in0=P.t[:, 0:mw], in1=map_, op=ALU.mult),
                      R=[P.tr] + mtr, W=[P.tr])
            for j in range(j0, 4):
                mm(acc3[:, j, :], P.t[:, (j - j0) * 128:(j - j0 + 1) * 128], v_fn(h, kb_), (kb_ == 0 and j == 0), (kb_ == 4 * c + 3 and j == 3),
                   [P.tr] + Rops, acct, (kb_ == 0 and j == 0))
            if kb_ == 4 * c + 3:
                kb.op("dve", lambda e: e.reciprocal(out=rden.t[:, 0:4], in_=acc3[:, :, 64]), R=[acct], W=[rden.tr])
                kb.op("dve", lambda e: e.tensor_tensor(
                    out=Ost.t[:, 4 * c:4 * c + 4, ocol0 + h * 64:ocol0 + (h + 1) * 64], in0=acc3[:, :, 0:64],
                    in1=rden.t[:, 0:4].unsqueeze(2).to_broadcast([128, 4, 64]), op=ALU.mult),
                    R=[acct, rden.tr], Wa=[Ost.tr])

    def STOP(name):
        if stop_after == name:
            kb.barrier()
            raise _Stop()

    def body():
      for li, l in enumerate(layers):
        x_src = x_in if li == 0 else xs[(li - 1) % 2]
        x_dst = y_out if li == nl - 1 else xs[li % 2]
        W = {n: Wd_[n].t[l] for n in WNAMES}
        Wtr = {n: Wd_[n].tr for n in WNAMES}
        for s in range(nseq):
            tb = s * NT
            with contextlib.ExitStack() as es:
                xTa = kb.sbuf("xTa", [128, 8, S], BF16, es)
                wgr = [kb.sbuf("wgr%d" % i, [128, 8, 512], BF16, es) for i in range(2)]
                xts = [kb.sbuf("xt%d" % i, [128, D], F32, es) for i in range(2)]
                xbs = [kb.sbuf("xb%d" % i, [128, D], BF16, es) for i in range(2)]
                ugs = [kb.sbuf("ug%d" % i, [128, 512], F32, es) for i in range(4)]
                groups = []
                c0 = 0
                while c0 < INC:
                    groups.append((c0, min(512, INC - c0)))
                    c0 += 512

                def w_load(gi):
                    c0_, cw_ = groups[gi]
                    kb.dma("pool", wgr[gi % 2].t[:, :, 0:cw_], W["w_in"][:, c0_:c0_ + cw_].rearrange("(k p) c -> p k c", p=128),
                           R=[Wtr["w_in"]], W=[wgr[gi % 2].tr])
                w_load(0)
                w_load(1)
                for t in range(NT):
                    xt, xb = xts[t % 2], xbs[t % 2]
                    kb.dma("sp", xt.t[:], x_src.t[s, t * 128:(t + 1) * 128, :], R=[x_src.k(s)], W=[xt.tr])
                    kb.op("dve", lambda e: e.tensor_copy(out=xb.t[:], in_=xt.t[:]), R=[xt.tr], W=[xb.tr])
                    pa, pt = psbf(0, t % 2)
                    for kc in range(8):
                        tp(pa[:, kc * 128:(kc + 1) * 128], xb.t[:, kc * 128:(kc + 1) * 128], [xb.tr], pt, kc == 0)
                    kb.op("act", lambda e: e.copy(out=xTa.t[:, :, t * 128:(t + 1) * 128], in_=pa.rearrange("p (k t) -> p k t", k=8)),
                          R=[pt], Wa=[xTa.k(t)])
                it_ = 0
                for gi, (c0, cw) in enumerate(groups):
                    wg_ = wgr[gi % 2]
                    for t in range(NT):
                        pm, pmt = psb(1 + it_ % 2, (it_ // 2) % 2)
                        ug = ugs[it_ % 4]
                        for kc in range(8):
                            mm(pm[:, 0:cw], xTa.t[:, kc, t * 128:(t + 1) * 128], wg_.t[:, kc, 0:cw], kc == 0, kc == 7,
                               [xTa.k(t), wg_.tr], pmt, kc == 0, inc=(kc == 7))
                        if it_ % 2 == 0:
                            kb.op("act", lambda e: e.copy(out=ug.t[:, 0:cw], in_=pm[:, 0:cw]), R=[pmt], W=[ug.tr])
                        else:
                            kb.op("dve", lambda e: e.tensor_copy(out=ug.t[:, 0:cw], in_=pm[:, 0:cw]), R=[pmt], W=[ug.tr])
                        kb.dma("sp", u_d.t[t * 128:(t + 1) * 128, c0:c0 + cw], ug.t[:, 0:cw], R=[ug.tr], Wa=[u_d.tr])
                        it_ += 1
                    if gi + 2 < len(groups):
                        w_load(gi + 2)
                kb.barrier()
            if li == 0 and s == 0:
                tap("u", u_d.t[:], [u_d.tr], [S, INC], F32)
            STOP("P1")

            with contextlib.ExitStack() as es:
                w_uq = kb.sbuf("w_uq", [128, 2, 768], BF16, es)
                w_ukv = kb.sbuf("w_ukv", [128, 1024], BF16, es)
                stg = kb.sbuf("stg", [128, 1024], F32, es)
                qn = kb.sbuf("qn", [128, 3], F32, es)
                kb.dma("sp", qn.t[:, 0:2], W["mla_q_norm"].rearrange("(k p) -> p k", p=128), R=[Wtr["mla_q_norm"]], W=[qn.tr],
                       allow_slow_non_contiguous=True)
                kb.dma("sp", qn.t[:, 2:3], W["mla_kv_norm"].rearrange("(p o) -> p o", o=1), R=[Wtr["mla_kv_norm"]], Wa=[qn.tr])
                STOP("P2a0")
                for kc in range(2):
                    kb.dma("sp", stg.t[:, 0:768], W["mla_w_uq"][kc * 128:(kc + 1) * 128, :], R=[Wtr["mla_w_uq"]], W=[stg.tr])
                    kb.op("dve", lambda e: e.tensor_scalar(out=w_uq.t[:, kc, :], in0=stg.t[:, 0:768], scalar1=qn.t[:, kc:kc + 1],
                                                           scalar2=None, op0=ALU.mult), R=[stg.tr, qn.tr], Wa=[w_uq.tr])
                kb.dma("sp", stg.t[:, :], W["mla_w_ukv"][:, :], R=[Wtr["mla_w_ukv"]], W=[stg.tr])
                kb.op("dve", lambda e: e.tensor_scalar(out=w_ukv.t[:], in0=stg.t[:], scalar1=qn.t[:, 2:3], scalar2=None,
                                                       op0=ALU.mult), R=[stg.tr, qn.tr], W=[w_ukv.tr])
                STOP("P2a")
                QT = kb.sbuf("QT", [96, 8, S], BF16, es)
                KT = kb.sbuf("KT", [96, 8, S], BF16, es)
                VA = kb.sbuf("VA", [128, NT, 8, 65], BF16, es)
                Ost = kb.sbuf("Ost", [128, NT, 512], BF16, es)
                kb.op("dve", lambda e: e.memset(VA.t[:].rearrange("p a b c -> p (a b c)"), 1.0), W=[VA.tr])
                uus = [kb.sbuf("uu%d" % i, [128, 416], F32, es) for i in range(2)]
                cbs = [kb.sbuf("cb%d" % i, [128, 480], BF16, es) for i in range(2)]
                for cb in cbs:
                    kb.op("dve", lambda e: e.memset(cb.t[:], 0.0), W=[cb.tr])
                junk = kb.sbuf("junk", [128, 384], F32, es)
                ssq = kb.sbuf("ssq", [128, 2], F32, es)
                rs = kb.sbuf("rs", [128, 2], F32, es)
                rs2 = kb.sbuf("rs2", [128, 2], F32, es)
                rstd = kb.sbuf("rstd", [128, 2], F32, es)
                tm1 = kb.sbuf("tm1", [128, 256], F32, es)
                tm2 = kb.sbuf("tm2", [128, 256], F32, es)
                cT = kb.sbuf("cT", [128, 384], BF16, es)
                kp = kb.sbuf("kp", [128, 128], BF16, es)
                q32 = kb.sbuf("q32", [128, 768], F32, es)
                qb = kb.sbuf("qb", [128, 768], BF16, es)
                for t in range(NT):
                    uu, cb = uus[t % 2], cbs[t % 2]
                    cm, sm = cosT.t[:, tb + t, 0:16], sinT.t[:, tb + t, 0:16]
                    kb.dma("sp", uu.t[:], u_d.t[t * 128:(t + 1) * 128, 0:416], R=[u_d.tr], W=[uu.tr])
                    kb.op("act", lambda e: e.activation(out=junk.t[:, 0:384], in_=uu.t[:, 0:384], func=AF.Square),
                          R=[uu.tr], W=[junk.tr])
                    kb.op("dve", lambda e: e.reduce_sum(out=ssq.t[:, 0:1], in_=junk.t[:, 0:256], axis=AX.X), R=[junk.tr], W=[ssq.tr])
                    kb.op("dve", lambda e: e.reduce_sum(out=ssq.t[:, 1:2], in_=junk.t[:, 256:384], axis=AX.X), R=[junk.tr], Wa=[ssq.tr])
                    kb.op("dve", lambda e: e.tensor_scalar(out=rs.t[:, 0:1], in0=ssq.t[:, 0:1], scalar1=1.0 / 256, scalar2=RMS_EPS,
                                                           op0=ALU.mult, op1=ALU.add), R=[ssq.tr], W=[rs.tr])
                    kb.op("dve", lambda e: e.tensor_scalar(out=rs.t[:, 1:2], in0=ssq.t[:, 1:2], scalar1=1.0 / 128, scalar2=RMS_EPS,
                                                           op0=ALU.mult, op1=ALU.add), R=[ssq.tr], Wa=[rs.tr])
                    if t == 0:
                        STOP("P2b1")
                    kb.op("act", lambda e: e.activation(out=rs2.t[:], in_=rs.t[:], func=AF.Sqrt), R=[rs.tr], W=[rs2.tr])
                    kb.op("dve", lambda e: e.reciprocal(out=rstd.t[:], in_=rs2.t[:]), R=[rs2.tr], W=[rstd.tr])
                    kb.op("dve", lambda e: e.tensor_scalar(out=cb.t[:, 0:256], in0=uu.t[:, 0:256], scalar1=rstd.t[:, 0:1], scalar2=None,
                                                           op0=ALU.mult), R=[uu.tr, rstd.tr], W=[cb.tr])
                    kb.op("dve", lambda e: e.tensor_scalar(out=cb.t[:, 256:384], in0=uu.t[:, 256:384], scalar1=rstd.t[:, 1:2], scalar2=None,
                                                           op0=ALU.mult), R=[uu.tr, rstd.tr], Wa=[cb.tr])
                    if t == 0:
                        STOP("P2b3")
                    rope2("dve", uu.t[:, 384:416].rearrange("p (h d) -> p h d", h=1), cb.t[:, 448:480].rearrange("p (h d) -> p h d", h=1),
                          1, 16, cm, sm, tm1, tm2, [uu.tr], [cb.tr])
                    if t == 0:
                        STOP("P2b4")
                    pa, pt = psbf(0, 0)
                    tp(pa[:, 0:128], cb.t[:, 0:128], [cb.tr], pt, True)
                    tp(pa[:, 128:256], cb.t[:, 128:256], [cb.tr], pt, False)
                    tp(pa[:, 256:384], cb.t[:, 256:384], [cb.tr], pt, False)
                    tp(pa[0:96, 384:512], cb.t[:, 384:480], [cb.tr], pt, False)
                    kb.op("act", lambda e: e.copy(out=cT.t[:], in_=pa[:, 0:384]), R=[pt], W=[cT.tr])
                    if t == 0:
                        STOP("P2b6")
                    kb.op("act", lambda e: e.copy(out=kp.t[64:96, :], in_=pa[64:96, 384:512]), R=[pt], W=[kp.tr])
                    for h in range(8):
                        kb.op("dve", lambda e: e.tensor_copy(out=KT.t[64:96, h, t * 128:(t + 1) * 128], in_=kp.t[64:96, :]),
                              R=[kp.tr], Wa=[KT.tr])
                    if t == 0:
                        STOP("P2b")
                    for half, cw in ((0, 512), (1, 256)):
                        pq, pqt = psb(1, half)
                        for kc in range(2):
                            mm(pq[:, 0:cw], cT.t[:, kc * 128:(kc + 1) * 128], w_uq.t[:, kc, half * 512:half * 512 + cw], kc == 0, kc == 1,
                               [cT.tr, w_uq.tr], pqt, kc == 0)
                        kb.op("act", lambda e: e.copy(out=q32.t[:, half * 512:half * 512 + cw], in_=pq[:, 0:cw]), R=[pqt],
                              W=[q32.tr] if half == 0 else (), Wa=[q32.tr] if half else ())
                    kb.op("dve", lambda e: e.tensor_copy(out=qb.t[:], in_=q32.t[:]), R=[q32.tr], W=[qb.tr])
                    rope2("dve", q32.t[:].rearrange("p (h d) -> p h d", h=8)[:, :, 64:96],
                          qb.t[:].rearrange("p (h d) -> p h d", h=8)[:, :, 64:96], 8, 16, cm, sm, tm1, tm2, [q32.tr], [qb.tr])
                    pa2, pt2 = psbf(0, 1)
                    for h in range(8):
                        tp(pa2[0:96, h * 128:(h + 1) * 128], qb.t[:, h * 96:(h + 1) * 96], [qb.tr], pt2, h == 0)
                    kb.op("act", lambda e: e.copy(out=QT.t[0:96, :, t * 128:(t + 1) * 128],
                                                  in_=pa2[0:96, :].rearrange("p (h t) -> p h t", h=8)), R=[pt2], Wa=[QT.tr])
                    if t == 0:
                        STOP("P2c")
                    for hb in range(2):
                        pk, pkt = psb(2, hb)
                        for hh in range(4):
                            h = hb * 4 + hh
                            mm(pk[0:64, hh * 128:(hh + 1) * 128], w_ukv.t[:, h * 128:h * 128 + 64], cT.t[:, 256:384], True, True,
                               [cT.tr, w_ukv.tr], pkt, hh == 0)
                        kb.op("dve", lambda e: e.tensor_copy(out=KT.t[0:64, hb * 4:hb * 4 + 4, t * 128:(t + 1) * 128],
                                                             in_=pk[0:64, :].rearrange("p (h t) -> p h t", h=4)), R=[pkt], Wa=[KT.tr])
                    pv, pvt = psb(3, 0)
                    for h in range(8):
                        mm(pv[:, h * 64:(h + 1) * 64], cT.t[:, 256:384], w_ukv.t[:, h * 128 + 64:h * 128 + 128], True, True,
                           [cT.tr, w_ukv.tr], pvt, h == 0)
                    kb.op("act", lambda e: e.copy(out=VA.t[:, t, :, 0:64], in_=pv[:, 0:512].rearrange("p (h d) -> p h d", h=8)),
                          R=[pvt], Wa=[VA.tr])
                STOP("P2d")

                def m_mask(c, kb_, j0):
                    if kb_ >= 4 * c:
                        return dmask.t[:, 0, :], [dmask.tr], 128
                    return None
                attention(es, 8, None, lambda h, kb_: KT.t[0:96, h, kb_ * 128:(kb_ + 1) * 128],
                          lambda h, q0, q1: QT.t[0:96, h, q0:q1], lambda h, kb_: VA.t[:, kb_, h, :],
                          float(96 ** -0.5), m_mask, Ost, 0, [QT.tr, KT.tr, VA.tr])
                for t in range(NT):
                    kb.dma("sp", O_d.t[t * 128:(t + 1) * 128, 0:512], Ost.t[:, t, :], R=[Ost.tr], Wa=[O_d.tr])
                kb.barrier()
            if li == 0 and s == 0:
                tap("O_mla", O_d.t[:, 0:512], [O_d.tr], [S, 512], BF16)
            STOP("P2")

            with contextlib.ExitStack() as es:
                qTd = kb.sbuf("qTd", [64, 4, S], BF16, es)
                kTd = kb.sbuf("kTd", [64, S], BF16, es)
                VAd = kb.sbuf("VAd", [128, NT, 65], BF16, es)
                Osd = kb.sbuf("Osd", [128, NT, 256], BF16, es)
                offs = {}
                o_ = 0
                for b in range(2, NT):
                    offs[b] = o_
                    o_ += 128 * (b + 1)
                SC = kb.sbuf("SC", [128, o_], F32, es)
                lo = kb.sbuf("bs_lo", [128, 14], F32, es)
                hi = kb.sbuf("bs_hi", [128, 14], F32, es)
                wv = kb.sbuf("bs_w", [128, 14], F32, es)
                mid = kb.sbuf("bs_mid", [128, 14], F32, es)
                cnt = kb.sbuf("bs_cnt", [128, 14], F32, es)
                gef = kb.sbuf("bs_ge", [128, 14], F32, es)
                kb.op("dve", lambda e: e.memset(VAd.t[:].rearrange("p a b -> p (a b)"), 1.0), W=[VAd.tr])
                with contextlib.ExitStack() as es2:
                    QIT = kb.sbuf("QIT", [32, 8, S], BF16, es2)
                    kiT = kb.sbuf("kiT", [32, S], BF16, es2)
                    wiT = kb.sbuf("wiT", [32, NT, 128], BF16, es2)
                    kb.op("dve", lambda e: e.memset(wiT.t[:].rearrange("p a b -> p (a b)"), 0.0), W=[wiT.tr])
                    uds = [kb.sbuf("ud%d" % i_, [128, 680], F32, es2) for i_ in range(2)]
                    dbs = [kb.sbuf("db%d" % i_, [128, 680], BF16, es2) for i_ in range(2)]
                    tm1 = kb.sbuf("tm1d", [128, 64], F32, es2)
                    tm2 = kb.sbuf("tm2d", [128, 64], F32, es2)
                    Rg = kb.sbuf("Rg", [128, 8, 512], BF16, es2)
                    Wdb = kb.sbuf("Wdb", [128, 8, 128], BF16, es2)
                    wrep = kb.sbuf("wrep", [128, 128], BF16, es2)
                    ZLb = kb.sbuf("ZLb", [32, 8, 128], BF16, es2)
                    for t in range(NT):
                        ud, db = uds[t % 2], dbs[t % 2]
                        kb.dma("sp", ud.t[:], u_d.t[t * 128:(t + 1) * 128, 416:1096], R=[u_d.tr], W=[ud.tr])
                        kb.op("dve", lambda e: e.tensor_copy(out=db.t[:], in_=ud.t[:]), R=[ud.tr], W=[db.tr])
                        rope2("dve", ud.t[:, 0:320].rearrange("p (h d) -> p h d", h=5), db.t[:, 0:320].rearrange("p (h d) -> p h d", h=5),
                              5, 8, cosT.t[:, tb + t, 16:24], sinT.t[:, tb + t, 16:24], tm1, tm2, [ud.tr], [db.tr])
                        rope2("dve", ud.t[:, 384:672].rearrange("p (h d) -> p h d", h=9), db.t[:, 384:672].rearrange("p (h d) -> p h d", h=9),
                              9, 4, cosT.t[:, tb + t, 24:28], sinT.t[:, tb + t, 24:28], tm1, tm2, [ud.tr], [db.tr])
                        kb.op("act", lambda e: e.copy(out=VAd.t[:, t, 0:64], in_=ud.t[:, 320:384]), R=[ud.tr], Wa=[VAd.tr])
                        pA, ptA = psbf(0, 0)
                        pB, ptB = psbf(0, 1)
                        for h in range(4):
                            tp(pA[0:64, h * 128:(h + 1) * 128], db.t[:, h * 64:(h + 1) * 64], [db.tr], ptA, h == 0)
                        tp(pA[0:64, 512:640], db.t[:, 256:320], [db.tr], ptA, False)
                        tp(pA[0:32, 640:768], db.t[:, 640:672], [db.tr], ptA, False)
                        tp(pA[0:8, 768:896], db.t[:, 672:680], [db.tr], ptA, False)
                        for h in range(8):
                            tp(pB[0:32, h * 128:(h + 1) * 128], db.t[:, 384 + 32 * h:416 + 32 * h], [db.tr], ptB, h == 0)
                        kb.op("act", lambda e: e.copy(out=qTd.t[0:64, :, t * 128:(t + 1) * 128],
                                                      in_=pA[0:64, 0:512].rearrange("p (k t) -> p k t", k=4)), R=[ptA], Wa=[qTd.tr])
                        kb.op("act", lambda e: e.copy(out=kTd.t[0:64, t * 128:(t + 1) * 128], in_=pA[0:64, 512:640]), R=[ptA], Wa=[kTd.tr])
                        kb.op("act", lambda e: e.copy(out=kiT.t[0:32, t * 128:(t + 1) * 128], in_=pA[0:32, 640:768]), R=[ptA], Wa=[kiT.tr])
                        kb.op("act", lambda e: e.copy(out=wiT.t[0:8, t, :], in_=pA[0:8, 768:896]), R=[ptA], Wa=[wiT.tr])
                        kb.op("act", lambda e: e.copy(out=QIT.t[0:32, :, t * 128:(t + 1) * 128],
                                                      in_=pB[0:32, 0:1024].rearrange("p (k t) -> p k t", k=8)), R=[ptB], Wa=[QIT.tr])
                    STOP("P3a")
                    ev = 0
                    for b in range(2, NT):
                        N = 128 * (b + 1)
                        pw, pwt = psb(3, 0)
                        mm(pw[:, 0:128], rt.t[0:32, :], wiT.t[0:32, b, :], True, True, [rt.tr, wiT.tr], pwt, True)
                        kb.op("act", lambda e: e.copy(out=wrep.t[:], in_=pw[:, 0:128]), R=[pwt], W=[wrep.tr])
                        kb.op("dve", lambda e: e.tensor_tensor(out=Wdb.t[:], in0=wrep.t[:].unsqueeze(1).to_broadcast([128, 8, 128]),
                                                               in1=selmask.t[:], op=ALU.mult), R=[wrep.tr, selmask.tr], W=[Wdb.tr])
                        for g in range(8):
                            kb.op("dve", lambda e: e.tensor_copy(
                                out=ZLb.t[0:32, g, :].rearrange("p (h q) -> p h q", h=8),
                                in_=QIT.t[0:32, :, b * 128 + 16 * g:b * 128 + 16 * g + 16]), R=[QIT.tr],
                                W=[ZLb.tr] if g == 0 else (), Wa=[ZLb.tr] if g else ())
                        if b == 2:
                            STOP("P3b0")
                        for kc in range((N + 511) // 512):
                            k0 = kc * 512
                            ncol = min(512, N - k0)
                            for g in range(8):
                                zv, zt_ = psb(0, g % 2)
                                mm(zv[:, 0:ncol], ZLb.t[0:32, g, :], kiT.t[0:32, k0:k0 + ncol], True, True, [ZLb.tr, kiT.tr], zt_, True)
                                if g % 2 == 0:
                                    kb.op("act", lambda e: e.activation(out=Rg.t[:, g, 0:ncol], in_=zv[:, 0:ncol], func=AF.Relu),
                                          R=[zt_], Wa=[Rg.k(g)])
                                else:
                                    kb.op("dve", lambda e: e.tensor_scalar(out=Rg.t[:, g, 0:ncol], in0=zv[:, 0:ncol], scalar1=0.0, scalar2=None,
                                                                           op0=ALU.max), R=[zt_], Wa=[Rg.k(g)])
                            if b == 2 and kc == 0:
                                STOP("P3b1")
                            scv, sct = psb(1, ev % 2)
                            ev += 1
                            for g in range(8):
                                mm(scv[:, 0:ncol], Wdb.t[:, g, :], Rg.t[:, g, 0:ncol], g == 0, g == 7, [Wdb.tr, Rg.k(g)], sct, g == 0)
                            if b == 2 and kc == 0:
                                STOP("P3b2")
                            last = (k0 + ncol == N)
                            nplain = ncol - 128 if last else ncol
                            if nplain > 0:
                                kb.op("act", lambda e: e.copy(out=SC.t[:, offs[b] + k0:offs[b] + k0 + nplain], in_=scv[:, 0:nplain]),
                                      R=[sct], Wa=[SC.k(b)])
                            if b == 2 and kc == 0:
                                STOP("P3b2a")
                            if last:
                                kb.op("act", lambda e: e.copy(out=SC.t[:, offs[b] + N - 128:offs[b] + N], in_=scv[:, ncol - 128:ncol]),
                                      R=[sct], Wa=[SC.k(b)])
                                kb.op("pool", lambda e: e.tensor_tensor(out=SC.t[:, offs[b] + N - 128:offs[b] + N],
                                                                       in0=SC.t[:, offs[b] + N - 128:offs[b] + N],
                                                                       in1=tribias.t[:], op=ALU.add), R=[SC.k(b), tribias.tr], Wa=[SC.k(b)])
                        if b == 2:
                            STOP("P3b3")
                        if b == 4:
                            STOP("P3b4")
                    kb.barrier()
                STOP("P3b")
                junkb = kb.sbuf("junkb", [128, S], BF16, es)
                tmpb = kb.sbuf("bs_tmp", [128, 14], F32, es)
                sct_all = [SC.k(b) for b in range(2, NT)]
                for i_, b in enumerate(range(2, NT)):
                    N = 128 * (b + 1)
                    kb.op("dve", lambda e: e.reduce_max(out=hi.t[:, i_:i_ + 1], in_=SC.t[:, offs[b]:offs[b] + N], axis=AX.X),
                          R=[SC.k(b)], Wa=[hi.tr])
                    kb.op("dve", lambda e: e.tensor_reduce(out=lo.t[:, i_:i_ + 1], in_=SC.t[:, offs[b]:offs[b] + N - 128], axis=AX.X, op=ALU.min),
                          R=[SC.k(b)], Wa=[lo.tr])
                kb.op("dve", lambda e: e.tensor_scalar(out=lo.t[:], in0=lo.t[:], scalar1=-1.0, scalar2=None, op0=ALU.add), R=[lo.tr], W=[lo.tr])
                kb.op("dve", lambda e: e.tensor_scalar(out=hi.t[:], in0=hi.t[:], scalar1=1.0, scalar2=None, op0=ALU.add), R=[hi.tr], W=[hi.tr])
                kb.op("dve", lambda e: e.tensor_tensor(out=wv.t[:], in0=hi.t[:], in1=lo.t[:], op=ALU.subtract), R=[hi.tr, lo.tr], W=[wv.tr])
                junka = kb.sbuf("junka", [128, S], BF16, es)
                nmid = kb.sbuf("bs_nmid", [128, 14], F32, es)
                thrv = kb.sbuf("bs_thr", [128, 14], F32, es)
                for i_, b in enumerate(range(2, NT)):
                    kb.op("dve", lambda e: e.memset(thrv.t[:, i_:i_ + 1], 255.5 if i_ % 2 == 0 else float(511 - 128 * (b + 1))),
                          W=[thrv.tr] if i_ == 0 else (), Wa=[thrv.tr] if i_ else ())
                for itb in range(NBIS):
                    kb.op("dve", lambda e: e.tensor_scalar(out=wv.t[:], in0=wv.t[:], scalar1=0.5, scalar2=None, op0=ALU.mult), R=[wv.tr], W=[wv.tr])
                    kb.op("dve", lambda e: e.tensor_tensor(out=mid.t[:], in0=lo.t[:], in1=wv.t[:], op=ALU.add), R=[lo.tr, wv.tr], W=[mid.tr])
                    kb.op("dve", lambda e: e.tensor_scalar(out=nmid.t[:], in0=mid.t[:], scalar1=-1.0, scalar2=None, op0=ALU.mult),
                          R=[mid.tr], W=[nmid.tr])
                    for i_, b in enumerate(range(2, NT)):
                        N = 128 * (b + 1)
                        if i_ % 2 == 0:
                            kb.op("dve", lambda e: e.tensor_scalar(out=junkb.t[:, 0:N], in0=SC.t[:, offs[b]:offs[b] + N], scalar1=mid.t[:, i_:i_ + 1],
                                                                   scalar2=None, op0=ALU.is_ge, op1=ALU.add, accum_out=cnt.t[:, i_:i_ + 1]),
                                  R=[SC.k(b), mid.tr], W=[junkb.tr], Wa=[cnt.k(i_)])
                        else:
                            kb.op("act", lambda e: e.activation(out=junka.t[:, 0:N], in_=SC.t[:, offs[b]:offs[b] + N], func=AF.Sign,
                                                                bias=nmid.t[:, i_:i_ + 1], scale=1.0, accum_out=cnt.t[:, i_:i_ + 1]),
                                  R=[SC.k(b), nmid.tr], W=[junka.tr], Wa=[cnt.k(i_)])
                    kb.op("dve", lambda e: e.tensor_tensor(out=gef.t[:], in0=cnt.t[:], in1=thrv.t[:], op=ALU.is_ge),
                          R=[cnt.k(i__) for i__ in range(14)] + [thrv.tr], W=[gef.tr])
                    kb.op("dve", lambda e: e.tensor_tensor(out=tmpb.t[:], in0=gef.t[:], in1=wv.t[:], op=ALU.mult), R=[gef.tr, wv.tr], W=[tmpb.tr])
                    kb.op("dve", lambda e: e.tensor_tensor(out=lo.t[:], in0=lo.t[:], in1=tmpb.t[:], op=ALU.add), R=[lo.tr, tmpb.tr], W=[lo.tr])
                STOP("P3c")
                mq = kb.sbuf("mq", [128, S], BF16, es)
                maskT = kb.sbuf("maskT", [128, NT, 512], BF16, es)
                Pb = [kb.sbuf("Pbd%d" % i_, [128, 512], BF16, es) for i_ in range(2)]
                rden = kb.sbuf("rdend", [128, 4], F32, es)
                tq = 0
                for c in range(4):
                    for j in range(4):
                        b = 4 * c + j
                        N = 128 * (b + 1)
                        if b >= 2:
                            kb.op("dve", lambda e: e.tensor_scalar(out=mq.t[:, 0:N], in0=SC.t[:, offs[b]:offs[b] + N], scalar1=lo.t[:, b - 2:b - 1],
                                                                   scalar2=None, op0=ALU.is_ge), R=[SC.k(b), lo.tr], W=[mq.tr])
                            for kb0 in range(0, b + 1, 8):
                                n_ = min(8, b + 1 - kb0)
                                pm_, pmt_ = psbf(2, tq % 2)
                                tq += 1
                                for i_ in range(n_):
                                    tp(pm_[:, i_ * 128:(i_ + 1) * 128], mq.t[:, (kb0 + i_) * 128:(kb0 + i_ + 1) * 128], [mq.tr], pmt_, i_ == 0)
                                kb.op("act", lambda e: e.copy(out=maskT.t[:, kb0:kb0 + n_, j * 128:(j + 1) * 128],
                                                              in_=pm_[:, 0:n_ * 128].rearrange("p (k q) -> p k q", k=n_)), R=[pmt_], Wa=[maskT.tr])
                        else:
                            for kb_ in range(b + 1):
                                kb.op("dve", lambda e: e.tensor_copy(out=maskT.t[:, kb_, j * 128:(j + 1) * 128],
                                                                     in_=dmask.t[:, 0 if kb_ == b else 7, :]), R=[dmask.tr], Wa=[maskT.tr])

                    def d_mask(c_, kb_, j0):
                        return maskT.t[:, kb_, 128 * j0:512], [maskT.tr], 512 - 128 * j0
                    attention((Pb, rden), 4, [c], lambda h, kb_: kTd.t[0:64, kb_ * 128:(kb_ + 1) * 128],
                              lambda h, q0, q1: qTd.t[0:64, h, q0:q1], lambda h, kb_: VAd.t[:, kb_, :],
                              0.125, d_mask, Osd, 0, [qTd.tr, kTd.tr, VAd.tr])
                for t in range(NT):
                    kb.dma("sp", O_d.t[t * 128:(t + 1) * 128, 512:768], Osd.t[:, t, :], R=[Osd.tr], Wa=[O_d.tr])
                kb.barrier()
            if li == 0 and s == 0:
                tap("O_dsa", O_d.t[:, 512:768], [O_d.tr], [S, 256], BF16)
            STOP("P3")

            es_wo = contextlib.ExitStack()
            w_o = kb.sbuf("w_o", [128, 8, D], BF16, es_wo)
            for kc in range(8):
                kb.dma("pool", w_o.t[:, kc, :], W["w_o"][kc * 128:(kc + 1) * 128, :], R=[Wtr["w_o"]], Wa=[w_o.tr],
                       max_dma_last_dim=4096)
            with contextlib.ExitStack() as es:
                qTc = kb.sbuf("qTc", [64, 12, S], BF16, es)
                kTc = kb.sbuf("kTc", [64, 12, S], BF16, es)
                VAc = kb.sbuf("VAc", [128, NT, 12, 65], BF16, es)
                Osc = kb.sbuf("Osc", [128, NT, 256], BF16, es)
                kb.op("dve", lambda e: e.memset(VAc.t[:].rearrange("p a b c -> p (a b c)"), 1.0), W=[VAc.tr])
                ucs = [kb.sbuf("uc%d" % i_, [128, 2304], F32, es) for i_ in range(2)]
                cbfs = [kb.sbuf("cbf%d" % i_, [128, 1536], BF16, es) for i_ in range(2)]
                tm1 = kb.sbuf("tm1c", [128, 256], F32, es)
                tm2 = kb.sbuf("tm2c", [128, 256], F32, es)
                for t in range(NT):
                    uc, cbf = ucs[t % 2], cbfs[t % 2]
                    ch, sh_ = cosT.t[:, tb + t, 16:24], sinT.t[:, tb + t, 16:24]
                    kb.dma("sp", uc.t[:], u_d.t[t * 128:(t + 1) * 128, 1096:3400], R=[u_d.tr], W=[uc.tr])
                    kb.op("dve", lambda e: e.tensor_copy(out=cbf.t[:], in_=uc.t[:, 0:1536]), R=[uc.tr], W=[cbf.tr])
                    rope2("dve", uc.t[:, 0:1536].rearrange("p (h d) -> p h d", h=24), cbf.t[:].rearrange("p (h d) -> p h d", h=24),
                          24, 8, ch, sh_, tm1, tm2, [uc.tr], [cbf.tr])
                    if t == 0:
                        STOP("P4a1")
                    kb.op("act", lambda e: e.copy(out=VAc.t[:, t, :, 0:64], in_=uc.t[:, 1536:2304].rearrange("p (h d) -> p h d", h=12)),
                          R=[uc.tr], Wa=[VAc.tr])
                    for (src0, dstb, bk) in ((0, qTc, 0), (768, kTc, 2)):
                        pA, ptA = psbf(bk, 0)
                        pB, ptB = psbf(bk, 1)
                        for h in range(12):
                            if h < 8:
                                tp(pA[0:64, h * 128:(h + 1) * 128], cbf.t[:, src0 + h * 64:src0 + (h + 1) * 64], [cbf.tr], ptA, h == 0)
                            else:
                                tp(pB[0:64, (h - 8) * 128:(h - 7) * 128], cbf.t[:, src0 + h * 64:src0 + (h + 1) * 64], [cbf.tr], ptB, h == 8)
                        kb.op("act", lambda e: e.copy(out=dstb.t[0:64, 0:8, t * 128:(t + 1) * 128],
                                                      in_=pA[0:64, 0:1024].rearrange("p (k t) -> p k t", k=8)), R=[ptA], Wa=[dstb.tr])
                        kb.op("act", lambda e: e.copy(out=dstb.t[0:64, 8:12, t * 128:(t + 1) * 128],
                                                      in_=pB[0:64, 0:512].rearrange("p (k t) -> p k t", k=4)), R=[ptB], Wa=[dstb.tr])
                STOP("P4a")
                Pc = [kb.sbuf("Pc%d" % i_, [128, 512], BF16, es) for i_ in range(2)]
                rdc = kb.sbuf("rdc", [128, 4], F32, es)
                Rops = [qTc.tr, kTc.tr, VAc.tr]
                dit = []
                for b in range(NT):
                    blocks = []
                    for kb_ in range(max(0, b - 1), b + 1):
                        blocks.append((0, kb_, 0 if kb_ == b else 1))
                    for kb_ in range(max(0, b - 4), b + 1):
                        d_ = b - kb_
                        blocks.append((1, kb_, 2 if d_ == 0 else (4 if d_ == 4 else 3)))
                    for kb_ in range(0, b + 1):
                        blocks.append((2, kb_, 5 if kb_ == b else 6))
                    for bi, (g, kb_, midx) in enumerate(blocks):
                        dit.append((b, bi, len(blocks), g, kb_, midx))

                def d_qk(i):
                    b, bi, nb, g, kb_, midx = dit[i]
                    sv_, st_ = psb(2, i % 2)
                    for jj in range(4):
                        hh = 4 * g + jj
                        mm(sv_[:, jj * 128:(jj + 1) * 128], kTc.t[0:64, hh, kb_ * 128:(kb_ + 1) * 128],
                           qTc.t[0:64, hh, b * 128:(b + 1) * 128], True, True, Rops, st_, jj == 0)

                d_qk(0)
                for i, (b, bi, nb, g, kb_, midx) in enumerate(dit):
                    if i + 1 < len(dit):
                        d_qk(i + 1)
                    accv, acct = psb(1, b % 2)
                    acc3 = accv[:, 0:260].rearrange("p (j d) -> p j d", j=4)
                    sv_, st_ = psb(2, i % 2)
                    P = Pc[i % 2]
                    kb.op("act", lambda e: e.activation(out=P.t[:], in_=sv_[:, 0:512], func=AF.Exp, scale=0.125), R=[st_], W=[P.tr])
                    kb.op("dve", lambda e: e.tensor_tensor(out=P.t[:].rearrange("p (j q) -> p j q", j=4),
                                                           in0=P.t[:].rearrange("p (j q) -> p j q", j=4),
                                                           in1=dmask.t[:, midx, :].unsqueeze(1).to_broadcast([128, 4, 128]), op=ALU.mult),
                          R=[P.tr, dmask.tr], W=[P.tr])
                    for jj in range(4):
                        mm(acc3[:, jj, :], P.t[:, jj * 128:(jj + 1) * 128], VAc.t[:, kb_, 4 * g + jj, :],
                           (bi == 0 and jj == 0), (bi == nb - 1 and jj == 3), [P.tr] + Rops, acct, (bi == 0 and jj == 0))
                    if bi == nb - 1:
                        kb.op("dve", lambda e: e.reciprocal(out=rdc.t[:, 0:4], in_=acc3[:, :, 64]), R=[acct], W=[rdc.tr])
                        kb.op("dve", lambda e: e.tensor_tensor(out=Osc.t[:, b, :].rearrange("p (j d) -> p j d", j=4), in0=acc3[:, :, 0:64],
                                                               in1=rdc.t[:, 0:4].unsqueeze(2).to_broadcast([128, 4, 64]), op=ALU.mult),
                              R=[acct, rdc.tr], Wa=[Osc.tr])
                for t in range(NT):
                    kb.dma("sp", O_d.t[t * 128:(t + 1) * 128, 768:1024], Osc.t[:, t, :], R=[Osc.tr], Wa=[O_d.tr])
                kb.barrier()
            if li == 0 and s == 0:
                tap("O_dil", O_d.t[:, 768:1024], [O_d.tr], [S, 256], BF16)
            STOP("P4")

            with contextlib.ExitStack() as es:
                yacc = kb.sbuf("yacc", [128, NT, D], F32, es)
                x1T = kb.sbuf("x1T", [128, 8, S], BF16, es)
                gate = kb.sbuf("gate", [128, NT, 32], F32, es)
                lng = kb.sbuf("lng", [128, 4, D], F32, es)
                for i_, n_ in enumerate(("ln1_g", "ln1_b", "ln2_g", "ln2_b")):
                    kb.dma("sp", lng.t[:, i_, :], W[n_].rearrange("(o d) -> o d", o=1).partition_broadcast(128), R=[Wtr[n_]], Wa=[lng.tr])
                zb = kb.sbuf("zb", [128, D], F32, es)
                st6 = kb.sbuf("st6", [128, 2, 6], F32, es)
                mv = kb.sbuf("mv", [128, 2], F32, es)
                r1 = kb.sbuf("r1", [128, 1], F32, es)
                r2 = kb.sbuf("r2", [128, 1], F32, es)
                r3 = kb.sbuf("r3", [128, 1], F32, es)

                def layer_norm(src_ap, src_tr, gi, out_ap, out_tr):
                    for hh in range(2):
                        kb.op("dve", lambda e: e.bn_stats(out=st6.t[:, hh, :], in_=src_ap[:, hh * 512:(hh + 1) * 512]),
                              R=src_tr, W=[st6.tr] if hh == 0 else (), Wa=[st6.tr] if hh else ())
                    kb.op("dve", lambda e: e.bn_aggr(out=mv.t[:], in_=st6.t[:].rearrange("p a b -> p (a b)")), R=[st6.tr], W=[mv.tr])
                    kb.op("dve", lambda e: e.tensor_scalar(out=r1.t[:], in0=mv.t[:, 1:2], scalar1=LN_EPS, scalar2=None, op0=ALU.add),
                          R=[mv.tr], W=[r1.tr])
                    kb.op("act", lambda e: e.activation(out=r2.t[:], in_=r1.t[:], func=AF.Sqrt), R=[r1.tr], W=[r2.tr])
                    kb.op("dve", lambda e: e.reciprocal(out=r3.t[:], in_=r2.t[:]), R=[r2.tr], W=[r3.tr])
                    kb.op("dve", lambda e: e.tensor_scalar(out=zb.t[:], in0=src_ap, scalar1=mv.t[:, 0:1], scalar2=r3.t[:, 0:1],
                                                           op0=ALU.subtract, op1=ALU.mult), R=list(src_tr) + [mv.tr, r3.tr], W=[zb.tr])
                    kb.op("dve", lambda e: e.tensor_tensor(out=zb.t[:], in0=zb.t[:], in1=lng.t[:, gi, :], op=ALU.mult),
                          R=[zb.tr, lng.tr], W=[zb.tr])
                    kb.op("dve", lambda e: e.tensor_tensor(out=out_ap, in0=zb.t[:], in1=lng.t[:, gi + 1, :], op=ALU.add),
                          R=[zb.tr, lng.tr], W=out_tr)

                with contextlib.ExitStack() as es5:
                    rwf = kb.sbuf("rwf", [128, 8, 36], F32, es5)
                    rw = kb.sbuf("rw", [128, 8, 36], BF16, es5)
                    rbias = kb.sbuf("rbias", [128, 36], F32, es5)
                    kb.dma("sp", rwf.t[:, :, 0:4], W["router_group_w"].rearrange("(k p) g -> p k g", p=128), R=[Wtr["router_group_w"]],
                           Wa=[rwf.tr])
                    for g_ in range(4):
                        kb.dma("sp", rwf.t[:, :, 4 + 8 * g_:12 + 8 * g_], W["router_expert_w"][g_].rearrange("(k p) e -> p k e", p=128),
                               R=[Wtr["router_expert_w"]], Wa=[rwf.tr])
                    kb.op("dve", lambda e: e.tensor_copy(out=rw.t[:], in_=rwf.t[:]), R=[rwf.tr], W=[rw.tr])
                    kb.dma("sp", rbias.t[:, 0:4], W["router_group_b"].rearrange("(o g) -> o g", o=1).partition_broadcast(128),
                           R=[Wtr["router_group_b"]], Wa=[rbias.tr])
                    kb.dma("sp", rbias.t[:, 4:36], W["router_expert_b"].rearrange("(o g) e -> o (g e)", o=1).partition_broadcast(128),
                           R=[Wtr["router_expert_b"]], Wa=[rbias.tr])
                    Ots = [kb.sbuf("Ot%d" % i, [128, D], BF16, es5) for i in range(2)]
                    OTs = [kb.sbuf("OT%d" % i, [128, 8, 128], BF16, es5) for i in range(2)]
                    xts = [kb.sbuf("xq%d" % i, [128, D], F32, es5) for i in range(2)]
                    zs = kb.sbuf("zs", [128, D], F32, es5)
                    x1 = kb.sbuf("x1", [128, D], F32, es5)
                    x1b = kb.sbuf("x1b", [128, D], BF16, es5)
                    lgt = kb.sbuf("lgt", [128, 36], F32, es5)
                    sm = {n_: kb.sbuf("rt_" + n_, [128, w_], F32, es5) for n_, w_ in
                          (("gm", 1), ("ghot", 4), ("ng", 1), ("eg", 4), ("sg", 1), ("ptop", 1), ("tmp", 32), ("le", 8),
                           ("m8", 8), ("nm1", 1), ("ex", 8), ("exm", 8), ("den", 1), ("rd", 1), ("fac", 1), ("ge", 8))}
                    for t in range(NT):
                        Ot, OT, xt = Ots[t % 2], OTs[t % 2], xts[t % 2]
                        kb.dma("sp", Ot.t[:], O_d.t[t * 128:(t + 1) * 128, :], R=[O_d.tr], W=[Ot.tr])
                        kb.dma("sp", xt.t[:], x_src.t[s, t * 128:(t + 1) * 128, :], R=[x_src.k(s)], W=[xt.tr])
                        pa, pt = psbf(0, t % 2)
                        for kc in range(8):
                            tp(pa[:, kc * 128:(kc + 1) * 128], Ot.t[:, kc * 128:(kc + 1) * 128], [Ot.tr], pt, kc == 0)
                        kb.op("act", lambda e: e.copy(out=OT.t[:].rearrange("p k t -> p (k t)"), in_=pa), R=[pt], W=[OT.tr])
                        for half in range(2):
                            pm, pmt = psb(1, half)
                            for kc in range(8):
                                mm(pm[:, 0:512], OT.t[:, kc, :], w_o.t[:, kc, half * 512:(half + 1) * 512], kc == 0, kc == 7,
                                   [OT.tr, w_o.tr], pmt, kc == 0)
                            kb.op("dve", lambda e: e.scalar_tensor_tensor(out=zs.t[:, half * 512:(half + 1) * 512],
                                                                          in0=xt.t[:, half * 512:(half + 1) * 512], scalar=ALPHA,
                                                                          in1=pm[:, 0:512], op0=ALU.mult, op1=ALU.add),
                                  R=[xt.tr, pmt], W=[zs.tr] if half == 0 else (), Wa=[zs.tr] if half else ())
                        layer_norm(zs.t[:], [zs.tr], 0, x1.t[:], [x1.tr])
                        kb.op("act", lambda e: e.mul(out=yacc.t[:, t, :], in_=x1.t[:], mul=ALPHA), R=[x1.tr], W=[yacc.k(t)])
                        kb.op("dve", lambda e: e.tensor_copy(out=x1b.t[:], in_=x1.t[:]), R=[x1.tr], W=[x1b.tr])
                        pa2, pt2 = psbf(2, t % 2)
                        for kc in range(8):
                            tp(pa2[:, kc * 128:(kc + 1) * 128], x1b.t[:, kc * 128:(kc + 1) * 128], [x1b.tr], pt2, kc == 0)
                        kb.op("act", lambda e: e.copy(out=x1T.t[:, :, t * 128:(t + 1) * 128],
                                                      in_=pa2.rearrange("p (k t) -> p k t", k=8)), R=[pt2], Wa=[x1T.k(t)])
                        pr, prt = psb(3, t % 2)
                        for kc in range(8):
                            mm(pr[:, 0:36], x1T.t[:, kc, t * 128:(t + 1) * 128], rw.t[:, kc, :], kc == 0, kc == 7, [x1T.k(t), rw.tr], prt, kc == 0)
                        def so(eng, fn, R, Wn):
                            kb.op(eng, fn, R=[sm[r_].tr if isinstance(r_, str) else r_ for r_ in R], W=[sm[Wn].tr])
                        kb.op("dve", lambda e: e.tensor_tensor(out=lgt.t[:], in0=pr[:, 0:36], in1=rbias.t[:], op=ALU.add),
                              R=[prt, rbias.tr], W=[lgt.tr])
                        so("dve", lambda e: e.reduce_max(out=sm["gm"].t[:], in_=lgt.t[:, 0:4], axis=AX.X), [lgt.tr], "gm")
                        so("dve", lambda e: e.tensor_scalar(out=sm["ghot"].t[:], in0=lgt.t[:, 0:4], scalar1=sm["gm"].t[:, 0:1], scalar2=None,
                                                            op0=ALU.is_ge), [lgt.tr, "gm"], "ghot")
                        so("dve", lambda e: e.tensor_scalar(out=sm["ng"].t[:], in0=sm["gm"].t[:], scalar1=-1.0, scalar2=None, op0=ALU.mult),
                           ["gm"], "ng")
                        so("act", lambda e: e.activation(out=sm["eg"].t[:], in_=lgt.t[:, 0:4], func=AF.Exp, bias=sm["ng"].t[:, 0:1]),
                           [lgt.tr, "ng"], "eg")
                        so("dve", lambda e: e.reduce_sum(out=sm["sg"].t[:], in_=sm["eg"].t[:], axis=AX.X), ["eg"], "sg")
                        so("dve", lambda e: e.reciprocal(out=sm["ptop"].t[:], in_=sm["sg"].t[:]), ["sg"], "ptop")
                        so("dve", lambda e: e.tensor_tensor(out=sm["tmp"].t[:].rearrange("p (g e) -> p g e", g=4),
                                                            in0=lgt.t[:, 4:36].rearrange("p (g e) -> p g e", g=4),
                                                            in1=sm["ghot"].t[:].unsqueeze(2).to_broadcast([128, 4, 8]), op=ALU.mult),
                           [lgt.tr, "ghot"], "tmp")
                        so("dve", lambda e: e.tensor_reduce(out=sm["le"].t[:], in_=sm["tmp"].t[:].rearrange("p (g e) -> p e g", g=4),
                                                            axis=AX.X, op=ALU.add), ["tmp"], "le")
                        so("dve", lambda e: e.max(out=sm["m8"].t[:], in_=sm["le"].t[:]), ["le"], "m8")
                        so("dve", lambda e: e.tensor_scalar(out=sm["nm1"].t[:], in0=sm["m8"].t[:, 0:1], scalar1=-1.0, scalar2=None,
                                                            op0=ALU.mult), ["m8"], "nm1")
                        so("act", lambda e: e.activation(out=sm["ex"].t[:], in_=sm["le"].t[:], func=AF.Exp, bias=sm["nm1"].t[:, 0:1]),
                           ["le", "nm1"], "ex")
                        so("dve", lambda e: e.scalar_tensor_tensor(out=sm["exm"].t[:], in0=sm["le"].t[:], scalar=sm["m8"].t[:, 1:2],
                                                                   in1=sm["ex"].t[:], op0=ALU.is_ge, op1=ALU.mult), ["le", "m8", "ex"], "exm")
                        so("dve", lambda e: e.reduce_sum(out=sm["den"].t[:], in_=sm["exm"].t[:], axis=AX.X), ["exm"], "den")
                        so("dve", lambda e: e.reciprocal(out=sm["rd"].t[:], in_=sm["den"].t[:]), ["den"], "rd")
                        so("dve", lambda e: e.tensor_tensor(out=sm["fac"].t[:], in0=sm["rd"].t[:], in1=sm["ptop"].t[:], op=ALU.mult),
                           ["rd", "ptop"], "fac")
                        so("dve", lambda e: e.tensor_scalar(out=sm["ge"].t[:], in0=sm["exm"].t[:], scalar1=sm["fac"].t[:, 0:1], scalar2=None,
                                                            op0=ALU.mult), ["exm", "fac"], "ge")
                        kb.op("dve", lambda e: e.tensor_tensor(out=gate.t[:, t, :].rearrange("p (g e) -> p g e", g=4),
                                                               in0=sm["ghot"].t[:].unsqueeze(2).to_broadcast([128, 4, 8]),
                                                               in1=sm["ge"].t[:].unsqueeze(1).to_broadcast([128, 4, 8]), op=ALU.mult),
                              R=[sm["ghot"].tr, sm["ge"].tr], W=[gate.k(t)])
                    kb.barrier()
                STOP("P5")
                with contextlib.ExitStack() as es6:
                    wgs = [kb.sbuf("wg%d" % i, [128, 8, 256], BF16, es6) for i in range(2)]
                    wus = [kb.sbuf("wu%d" % i, [128, 8, 256], BF16, es6) for i in range(2)]
                    wds = [kb.sbuf("wd%d" % i, [128, 2, D], BF16, es6) for i in range(2)]
                    sgs = [kb.sbuf("sg%d" % i, [128, 512], F32, es6) for i in range(2)]
                    aTs = [kb.sbuf("aT%d" % i, [128, 512], BF16, es6) for i in range(4)]
                    def w_issue(e_):
                        g_, ee = divmod(e_, 8)
                        wg, wu, wd = wgs[e_ % 2], wus[e_ % 2], wds[e_ % 2]
                        kb.dma("pool", wg.t[:], W["expert_w_gate"][g_, ee].rearrange("(k p) f -> p k f", p=128), R=[Wtr["expert_w_gate"]], W=[wg.tr])
                        kb.dma("pool", wu.t[:], W["expert_w_up"][g_, ee].rearrange("(k p) f -> p k f", p=128), R=[Wtr["expert_w_up"]], W=[wu.tr])
                        kb.dma("pool", wd.t[:], W["expert_w_down"][g_, ee].rearrange("(k p) d -> p k d", p=128), R=[Wtr["expert_w_down"]], W=[wd.tr])

                    mits = [(e_, c) for e_ in range(32) for c in range(4)]

                    def gate_up(i):
                        e_, c = mits[i]
                        wg, wu = wgs[e_ % 2], wus[e_ % 2]
                        x1tr = [x1T.k(4 * c + j) for j in range(4)]
                        for fc in range(2):
                            hg, hgt = psb(0, fc)
                            hu, hut = psb(1, fc)
                            for kc in range(8):
                                mm(hg[:, 0:512], wg.t[:, kc, fc * 128:(fc + 1) * 128], x1T.t[:, kc, c * 512:(c + 1) * 512], kc == 0, kc == 7,
                                   [wg.tr] + x1tr, hgt, kc == 0, inc=(kc == 7))
                            for kc in range(8):
                                mm(hu[:, 0:512], wu.t[:, kc, fc * 128:(fc + 1) * 128], x1T.t[:, kc, c * 512:(c + 1) * 512], kc == 0, kc == 7,
                                   [wu.tr] + x1tr, hut, kc == 0, inc=(kc == 7))
                            sgb = sgs[fc]
                            aT = aTs[(i % 2) * 2 + fc]
                            kb.op("act", lambda e: e.activation(out=sgb.t[:], in_=hg[:, 0:512], func=AF.Silu), R=[hgt], W=[sgb.tr])
                            kb.op("dve", lambda e: e.tensor_tensor(out=aT.t[:], in0=sgb.t[:], in1=hu[:, 0:512], op=ALU.mult),
                                  R=[sgb.tr, hut], W=[aT.tr])

                    def down(i):
                        e_, c = mits[i]
                        wd = wds[e_ % 2]
                        for j in range(4):
                            t = 4 * c + j
                            for half in range(2):
                                py, pyt = psb(2 + j % 2, half)
                                for fc in range(2):
                                    aT = aTs[(i % 2) * 2 + fc]
                                    mm(py[:, 0:512], aT.t[:, j * 128:(j + 1) * 128], wd.t[:, fc, half * 512:(half + 1) * 512], fc == 0, fc == 1,
                                       [aT.tr, wd.tr], pyt, fc == 0, inc=(fc == 1))
                                kb.op("dve", lambda e: e.scalar_tensor_tensor(
                                    out=yacc.t[:, t, half * 512:(half + 1) * 512], in0=py[:, 0:512], scalar=gate.t[:, t, e_:e_ + 1],
                                    in1=yacc.t[:, t, half * 512:(half + 1) * 512], op0=ALU.mult, op1=ALU.add),
                                    R=[pyt, gate.k(t), yacc.k(t)], W=[yacc.k(t)])

                    w_issue(0)
                    w_issue(1)
                    gate_up(0)
                    for i in range(len(mits)):
                        if i + 1 < len(mits):
                            gate_up(i + 1)
                        down(i)
                        e_, c = mits[i]
                        if c == 3 and e_ + 2 < 32:
                            w_issue(e_ + 2)
                    outs = [kb.sbuf("ob%d" % i, [128, D], F32, es6) for i in range(2)]
                    for t in range(NT):
                        ob = outs[t % 2]
                        layer_norm(yacc.t[:, t, :], [yacc.k(t)], 2, ob.t[:], [ob.tr])
                        kb.dma("sp", x_dst.t[s, t * 128:(t + 1) * 128, :], ob.t[:], R=[ob.tr], Wa=[x_dst.k(s)])
                    kb.barrier()
            es_wo.close()
            if li == nl - 1:
                out_tracks.append(y_out.k(s))

    try:
        body()
    except _Stop:
        pass
    kb.finish(out_tracks)
    return kb, tapd


def _wshapes(inputs):
    return {n: list(inputs[n].shape) for n in WNAMES}


def kernel(**inputs):
    x = np.ascontiguousarray(inputs["x"], dtype=np.float32)
    pos = np.ascontiguousarray(inputs["positions"], dtype=np.int32)
    ncores = 8
    nseq = x.shape[0] // ncores
    kb, _ = build(nseq, list(range(DEPTH)), _wshapes(inputs))
    hc = host_consts()
    in_maps = []
    for c in range(ncores):
        m = {"x": x[c * nseq:(c + 1) * nseq], "positions": pos[c * nseq:(c + 1) * nseq]}
        for n in WNAMES:
            m[n] = np.ascontiguousarray(inputs[n], dtype=np.float32)
        for n, v in hc.items():
            m["c_" + n] = v
        in_maps.append(m)
    res = run_bass_kernel_spmd(kb.nc, in_maps, core_ids=list(range(ncores)))
    return np.concatenate([r["y"] for r in res.results], axis=0)
```

```python
import contextlib
import numpy as np
import ml_dtypes
import concourse.bass as bass
import concourse.mybir as mybir
from concourse.bass_utils import run_bass_kernel_spmd

F32 = mybir.dt.float32
BF16 = mybir.dt.bfloat16
I32 = mybir.dt.int32
ALU = mybir.AluOpType
AF = mybir.ActivationFunctionType
AX = mybir.AxisListType

D = 1024
S = 2048
NT = S // 128
DEPTH = 4
INC = 3400
ALPHA = float((2.0 * DEPTH) ** 0.25)
LN_EPS = 1e-5
RMS_EPS = 1e-6
TWO_PI = 6.283185307179586
C1 = 6.28125
C2 = TWO_PI - C1
MAGIC = 12582912.0
NBIS = 22


class Track:
    __slots__ = ("w", "r", "dsem", "dcnt")

    def __init__(self):
        self.w = {}
        self.r = {}
        self.dsem = None
        self.dcnt = 0


class Eng:
    def __init__(self, name, e, sem):
        self.name = name
        self.e = e
        self.sem = sem
        self.n = 0
        self.seen = {}


class Buf:
    def __init__(self, t):
        self.t = t
        self.tr = Track()
        self.sub = {}

    def k(self, key):
        if key not in self.sub:
            self.sub[key] = Track()
        return self.sub[key]

    def all(self):
        return [self.tr] + list(self.sub.values())

    def __getitem__(self, idx):
        return self.t[idx]


class KB:
    def __init__(self):
        self.nc = bass.Bass("TRN2", target_bir_lowering=False)
        self.es = contextlib.ExitStack()
        nc = self.nc
        self.E = {}
        for name, e in (("pe", nc.tensor), ("act", nc.scalar), ("dve", nc.vector),
                        ("pool", nc.gpsimd), ("sp", nc.sync)):
            sem = self.es.enter_context(nc.semaphore("sem_" + name))
            self.E[name] = Eng(name, e, sem)
        self.dsems = []
        self.ninst = 0
        self.pool_ds = []
        self.pool_i = 0

    def sbuf(self, name, shape, dtype, es=None):
        self.uid = getattr(self, "uid", 0) + 1
        return Buf((es or self.es).enter_context(self.nc.sbuf_tensor("sb%d_%s" % (self.uid, name), list(shape), dtype)))

    def psum(self, name, shape, dtype):
        return Buf(self.es.enter_context(self.nc.psum_tensor(name, list(shape), dtype)))

    def dram(self, name, shape, dtype, kind="Internal"):
        return Buf(self.nc.dram_tensor(name, list(shape), dtype, kind=kind))

    def _need(self, E, sem, val, owner, raw):
        if owner is E and (not raw or E.name == "pe"):
            return
        if E.seen.get(sem, 0) >= val:
            return
        E.e.wait_ge(sem, val)
        E.seen[sem] = val

    def _deps(self, E, R, W, Wa):
        for t in R:
            for sem, (val, owner) in t.w.items():
                self._need(E, sem, val, owner, True)
        for t in W:
            for sem, (val, owner) in t.w.items():
                self._need(E, sem, val, owner, False)
            for sem, (val, owner) in t.r.items():
                self._need(E, sem, val, owner, False)
        for t in Wa:
            for sem, (val, owner) in t.r.items():
                self._need(E, sem, val, owner, False)

    def _post(self, sem, val, owner, R, W, Wa):
        for t in R:
            t.r[sem] = (val, owner)
        for t in W:
            t.w = {sem: (val, owner)}
            t.r = {}
        for t in Wa:
            t.w[sem] = (val, owner)

    def op(self, eng, fn, R=(), W=(), Wa=(), inc=True):
        E = self.E[eng]
        self._deps(E, R, W, Wa)
        inst = fn(E.e)
        if inc:
            E.n += 1
            inst.then_inc(E.sem, 1)
            self._post(E.sem, E.n, E, R, W, Wa)
        else:
            self._post(E.sem, E.n + 1, E, R, W, Wa)
        self.ninst += 1
        return inst

    def dma(self, q, out_ap, in_ap, R=(), W=(), Wa=(), **kw):
        E = self.E[q]
        self._deps(E, R, W, Wa)
        tgt = (list(W) + list(Wa))[0]
        if tgt.dsem is None:
            if len(self.pool_ds) < 40:
                ds = [self.es.enter_context(self.nc.semaphore("ds%d" % len(self.dsems))), 0]
                self.pool_ds.append(ds)
                self.dsems.append(ds)
            else:
                ds = self.pool_ds[self.pool_i % 40]
                self.pool_i += 1
            tgt.dsem = ds
        ds = tgt.dsem
        inst = E.e.dma_start(out=out_ap, in_=in_ap, **kw)
        ds[1] += 16
        inst.then_inc(ds[0], 16)
        self._post(ds[0], ds[1], None, R, W, Wa)
        self.ninst += 1
        return inst

    def dedicate(self, track):
        ds = [self.es.enter_context(self.nc.semaphore("dd%d" % len(self.dsems))), 0]
        self.dsems.append(ds)
        track.dsem = ds

    def barrier(self):
        for E in self.E.values():
            for E2 in self.E.values():
                if E2 is not E and E2.n > 0:
                    self._need(E, E2.sem, E2.n, E2, True)
            for ds in self.dsems:
                if ds[1] > 0:
                    self._need(E, ds[0], ds[1], None, True)

    def finish(self, out_tracks):
        E = self.E["sp"]
        for t in out_tracks:
            for sem, (val, owner) in t.w.items():
                self._need(E, sem, val, owner, True)
        self.barrier()


def host_consts():
    c = {}
    c["ident"] = np.eye(128, dtype=np.float32).astype(ml_dtypes.bfloat16)
    k = np.arange(128)[:, None]
    q = np.arange(128)[None, :]
    dm = np.zeros((128, 8, 128), np.float32)
    dm[:, 0, :] = (k <= q)
    dm[:, 1, :] = (q <= k)
    dm[:, 2, :] = ((q - k) % 4 == 0) & (q >= k)
    dm[:, 3, :] = ((q - k) % 4 == 0)
    dm[:, 4, :] = ((q - k) % 4 == 0) & (q <= k)
    dm[:, 5, :] = ((q - k) % 16 == 0) & (q >= k)
    dm[:, 6, :] = ((q - k) % 16 == 0)
    dm[:, 7, :] = 1.0
    c["dmask"] = dm.astype(ml_dtypes.bfloat16)
    qq = np.arange(128)[:, None]
    kk = np.arange(128)[None, :]
    c["tribias"] = np.where(kk <= qq, 0.0, -1e30).astype(np.float32)
    inv = []
    for rot in (32, 16, 8):
        inv.append((500000.0 ** (-np.arange(0, rot, 2, dtype=np.float32) / np.float32(rot))).astype(np.float32))
    inv = np.concatenate(inv).astype(np.float32)
    c["inv"] = np.tile(inv[None, :], (128, 1)).astype(np.float32)
    p = np.arange(128)
    sel = np.zeros((128, 8, 128), np.float32)
    for g in range(8):
        sel[p, g, 16 * g + (p % 16)] = 1.0
    c["selmask"] = sel.astype(ml_dtypes.bfloat16)
    rt = np.zeros((32, 128), np.float32)
    rt[p // 16, p] = 1.0
    c["rt"] = rt.astype(ml_dtypes.bfloat16)
    return c


WNAMES = ["w_in", "mla_q_norm", "mla_kv_norm", "mla_w_uq", "mla_w_ukv", "w_o", "ln1_g", "ln1_b",
          "router_group_w", "router_group_b", "router_expert_w", "router_expert_b",
          "expert_w_gate", "expert_w_up", "expert_w_down", "ln2_g", "ln2_b"]


class _Stop(Exception):
    pass


def build(nseq, layers, wshapes, taps=None, stop_after=None):
    kb = KB()
    nc = kb.nc
    taps = taps or []
    x_in = kb.dram("x", [nseq, S, D], F32, kind="ExternalInput")
    pos_in = kb.dram("positions", [nseq, S], I32, kind="ExternalInput")
    y_out = kb.dram("y", [nseq, S, D], F32, kind="ExternalOutput")
    Wd_ = {n: kb.dram(n, wshapes[n], F32, kind="ExternalInput") for n in WNAMES}
    hc = host_consts()
    Cd = {n: kb.dram("c_" + n, list(v.shape), BF16 if v.dtype != np.float32 else F32, kind="ExternalInput")
          for n, v in hc.items()}
    tapd = {}
    nl = len(layers)
    xs = [kb.dram("xs%d" % i, [nseq, S, D], F32) for i in range(2)] if nl > 1 else []
    u_d = kb.dram("u_d", [S, INC], F32)

    ident = kb.sbuf("ident", [128, 128], BF16)
    dmask = kb.sbuf("dmask", [128, 8, 128], BF16)
    tribias = kb.sbuf("tribias", [128, 128], F32)
    inv = kb.sbuf("inv", [128, 28], F32)
    selmask = kb.sbuf("selmask", [128, 8, 128], BF16)
    rt = kb.sbuf("rt", [32, 128], BF16)
    cosT = kb.sbuf("cosT", [128, nseq * NT, 28], F32)
    sinT = kb.sbuf("sinT", [128, nseq * NT, 28], F32)
    for b_, n in ((ident, "ident"), (dmask, "dmask"), (tribias, "tribias"), (inv, "inv"),
                  (selmask, "selmask"), (rt, "rt")):
        kb.dma("sp", b_.t[:], Cd[n].t[:], R=[Cd[n].tr], W=[b_.tr])

    PS = [kb.psum("ps%d" % i, [128, 1024], F32) for i in range(4)]

    def psb(i, h):
        return PS[i].t[:, h * 512:(h + 1) * 512], PS[i].k(h)

    def psbf(i, h):
        return PS[i].t.bitcast(BF16)[:, h * 1024:(h + 1) * 1024], PS[i].k(h)

    with contextlib.ExitStack() as es:
        posi = kb.sbuf("posi", [128, nseq * NT], I32, es)
        posf = kb.sbuf("posf", [128, nseq * NT], F32, es)
        ang = kb.sbuf("ang", [128, nseq * NT, 28], F32, es)
        tt = kb.sbuf("rp_t", [128, nseq * NT, 28], F32, es)
        kf = kb.sbuf("rp_k", [128, nseq * NT, 28], F32, es)
        rr = kb.sbuf("rp_r", [128, nseq * NT, 28], F32, es)
        kb.dma("sp", posi.t[:].rearrange("p (s t) -> p s t", s=nseq),
               pos_in.t[:].rearrange("s (t p) -> p s t", p=128), R=[pos_in.tr], W=[posi.tr],
               allow_slow_non_contiguous=True)
        kb.op("dve", lambda e: e.tensor_copy(out=posf.t[:], in_=posi.t[:]), R=[posi.tr], W=[posf.tr])
        nt_all = nseq * NT
        kb.op("dve", lambda e: e.tensor_tensor(
            out=ang.t[:], in0=posf.t[:].unsqueeze(2).to_broadcast([128, nt_all, 28]),
            in1=inv.t[:].unsqueeze(1).to_broadcast([128, nt_all, 28]), op=ALU.mult),
            R=[posf.tr, inv.tr], W=[ang.tr])
        for dst, shift in ((sinT, 0.0), (cosT, float(np.pi / 2))):
            kb.op("dve", lambda e: e.tensor_scalar(out=tt.t[:], in0=ang.t[:], scalar1=shift, scalar2=float(1.0 / TWO_PI),
                                                   op0=ALU.add, op1=ALU.mult), R=[ang.tr], W=[tt.tr])
            kb.op("dve", lambda e: e.tensor_scalar(out=kf.t[:], in0=tt.t[:], scalar1=MAGIC, scalar2=-MAGIC,
                                                   op0=ALU.add, op1=ALU.add), R=[tt.tr], W=[kf.tr])
            kb.op("dve", lambda e: e.scalar_tensor_tensor(out=rr.t[:], in0=kf.t[:], scalar=-C1, in1=ang.t[:],
                                                          op0=ALU.mult, op1=ALU.add), R=[kf.tr, ang.tr], W=[rr.tr])
            kb.op("dve", lambda e: e.scalar_tensor_tensor(out=tt.t[:], in0=kf.t[:], scalar=-C2, in1=rr.t[:],
                                                          op0=ALU.mult, op1=ALU.add), R=[kf.tr, rr.tr], W=[tt.tr])
            kb.op("dve", lambda e: e.tensor_scalar(out=rr.t[:], in0=tt.t[:], scalar1=shift, scalar2=None,
                                                   op0=ALU.add), R=[tt.tr], W=[rr.tr])
            kb.op("dve", lambda e: e.tensor_scalar(out=tt.t[:], in0=rr.t[:], scalar1=3.1415925, scalar2=-3.1415925,
                                                   op0=ALU.min, op1=ALU.max), R=[rr.tr], W=[tt.tr])
            kb.op("act", lambda e: e.activation(out=dst.t[:], in_=tt.t[:], func=AF.Sin), R=[tt.tr], W=[dst.tr])
        kb.barrier()

    def rope(eng, src, dst, col0, nh, hd, half, cs, sn, tmp1, tmp2, R, Wt):
        def v(buf, off):
            return buf.t[:, col0:col0 + nh * hd].rearrange("p (h d) -> p h d", h=nh)[:, :, off:off + half]
        cb = cs.unsqueeze(1).to_broadcast([128, nh, half])
        sb = sn.unsqueeze(1).to_broadcast([128, nh, half])
        t1 = tmp1.t[:, 0:nh * half].rearrange("p (h d) -> p h d", h=nh)
        t2 = tmp2.t[:, 0:nh * half].rearrange("p (h d) -> p h d", h=nh)
        x1, x2 = v(src, 0), v(src, half)
        o1, o2 = v(dst, 0), v(dst, half)
        kb.op(eng, lambda e: e.tensor_tensor(out=t1, in0=x1, in1=cb, op=ALU.mult), R=R, W=[tmp1.tr])
        kb.op(eng, lambda e: e.tensor_tensor(out=t2, in0=x2, in1=sb, op=ALU.mult), R=R, W=[tmp2.tr])
        kb.op(eng, lambda e: e.tensor_tensor(out=o1, in0=t1, in1=t2, op=ALU.subtract), R=[tmp1.tr, tmp2.tr], Wa=Wt)
        kb.op(eng, lambda e: e.tensor_tensor(out=t1, in0=x2, in1=cb, op=ALU.mult), R=R, W=[tmp1.tr])
        kb.op(eng, lambda e: e.tensor_tensor(out=t2, in0=x1, in1=sb, op=ALU.mult), R=R, W=[tmp2.tr])
        kb.op(eng, lambda e: e.tensor_tensor(out=o2, in0=t1, in1=t2, op=ALU.add), R=[tmp1.tr, tmp2.tr], Wa=Wt)

    def rope2(eng, sv, dv, nh, half, cs, sn, tmp1, tmp2, R, Wt):
        cb_ = cs.unsqueeze(1).to_broadcast([128, nh, half])
        sb_ = sn.unsqueeze(1).to_broadcast([128, nh, half])
        t1 = tmp1.t[:, 0:nh * half].rearrange("p (h d) -> p h d", h=nh)
        t2 = tmp2.t[:, 0:nh * half].rearrange("p (h d) -> p h d", h=nh)
        x1, x2 = sv[:, :, 0:half], sv[:, :, half:2 * half]
        o1, o2 = dv[:, :, 0:half], dv[:, :, half:2 * half]
        kb.op(eng, lambda e: e.tensor_tensor(out=t1, in0=x1, in1=cb_, op=ALU.mult), R=R, W=[tmp1.tr])
        kb.op(eng, lambda e: e.tensor_tensor(out=t2, in0=x2, in1=sb_, op=ALU.mult), R=R, W=[tmp2.tr])
        kb.op(eng, lambda e: e.tensor_tensor(out=o1, in0=t1, in1=t2, op=ALU.subtract), R=[tmp1.tr, tmp2.tr], Wa=Wt)
        kb.op(eng, lambda e: e.tensor_tensor(out=t1, in0=x2, in1=cb_, op=ALU.mult), R=R, W=[tmp1.tr])
        kb.op(eng, lambda e: e.tensor_tensor(out=t2, in0=x1, in1=sb_, op=ALU.mult), R=R, W=[tmp2.tr])
        kb.op(eng, lambda e: e.tensor_tensor(out=o2, in0=t1, in1=t2, op=ALU.add), R=[tmp1.tr, tmp2.tr], Wa=Wt)

    def tap(name, ap, tracks, shape, dtype):
        if name in taps:
            d = kb.dram("tap_" + name, list(shape), dtype, kind="ExternalOutput")
            tapd[name] = d
            n0 = shape[0]
            for r0 in range(0, n0, 128):
                r1 = min(n0, r0 + 128)
                kb.dma("sp", d.t[r0:r1], ap[r0:r1], R=tracks, Wa=[d.tr])

    out_tracks = []
    kb.dedicate(u_d.tr)
    O_d = kb.dram("O_d", [S, D], BF16)
    kb.dedicate(O_d.tr)
    for xx in xs:
        for s_ in range(nseq):
            kb.dedicate(xx.k(s_))
    for s_ in range(nseq):
        kb.dedicate(y_out.k(s_))

    def mm(out, lhsT, rhs, start, stop, R, pt, first, inc=True):
        kb.op("pe", lambda e: e.matmul(out, lhsT=lhsT, rhs=rhs, start=start, stop=stop),
              R=R, W=[pt] if first else (), Wa=() if first else [pt], inc=inc)

    def tp(out, in_, R, pt, first):
        kb.op("pe", lambda e: e.transpose(out, in_, ident.t[:]),
              R=list(R) + [ident.tr], W=[pt] if first else (), Wa=() if first else [pt])

    def attention(es, nheads, nch_fn, lhs_fn, rhs_fn, v_fn, scale, mask_fn, Ost, ocol0, Rops):
        if isinstance(es, tuple):
            Pb, rden = es
        else:
            Pb = [kb.sbuf("Pb%d" % i, [128, 512], BF16, es) for i in range(2)]
            rden = kb.sbuf("rden", [128, 4], F32, es)
        chunks_ = list(nch_fn) if nch_fn is not None else list(range(4))
        iters = [(h, c, kb_) for h in range(nheads) for c in chunks_ for kb_ in range(4 * c + 4)]

        def emit_qk(i):
            h, c, kb_ = iters[i]
            j0 = max(0, kb_ - 4 * c)
            ncol = 512 - 128 * j0
            sv_, st_ = psb(0, i % 2)
            mm(sv_[:, 0:ncol], lhs_fn(h, kb_), rhs_fn(h, c * 512 + 128 * j0, (c + 1) * 512), True, True, Rops, st_, True)

        emit_qk(0)
        for i, (h, c, kb_) in enumerate(iters):
            if i + 1 < len(iters):
                emit_qk(i + 1)
            hc_i = (h * len(chunks_) + chunks_.index(c))
            accv, acct = psb(1, hc_i % 2)
            acc3 = accv[:, 0:260].rearrange("p (j d) -> p j d", j=4)
            j0 = max(0, kb_ - 4 * c)
            ncol = 512 - 128 * j0
            sv_, st_ = psb(0, i % 2)
            P = Pb[i % 2]
            kb.op("act", lambda e: e.activation(out=P.t[:, 0:ncol], in_=sv_[:, 0:ncol], func=AF.Exp, scale=scale),
                  R=[st_], W=[P.tr])
            mk = mask_fn(c, kb_, j0)
            if mk is not None:
                map_, mtr, mw = mk
                kb.op("dve", lambda e: e.tensor_tensor(out=P.t[:, 0:mw], in0=P.t[:, 0:mw], in1=map_, op=ALU.mult),
                      R=[P.tr] + mtr, W=[P.tr])
            for j in range(j0, 4):
                mm(acc3[:, j, :], P.t[:, (j - j0) * 128:(j - j0 + 1) * 128], v_fn(h, kb_), (kb_ == 0 and j == 0), (kb_ == 4 * c + 3 and j == 3),
                   [P.tr] + Rops, acct, (kb_ == 0 and j == 0))
            if kb_ == 4 * c + 3:
                kb.op("dve", lambda e: e.reciprocal(out=rden.t[:, 0:4], in_=acc3[:, :, 64]), R=[acct], W=[rden.tr])
                kb.op("dve", lambda e: e.tensor_tensor(
                    out=Ost.t[:, 4 * c:4 * c + 4, ocol0 + h * 64:ocol0 + (h + 1) * 64], in0=acc3[:, :, 0:64],
                    in1=rden.t[:, 0:4].unsqueeze(2).to_broadcast([128, 4, 64]), op=ALU.mult),
                    R=[acct, rden.tr], Wa=[Ost.tr])

    def STOP(name):
        if stop_after == name:
            kb.barrier()
            raise _Stop()

    def body():
      for li, l in enumerate(layers):
        x_src = x_in if li == 0 else xs[(li - 1) % 2]
        x_dst = y_out if li == nl - 1 else xs[li % 2]
        W = {n: Wd_[n].t[l] for n in WNAMES}
        Wtr = {n: Wd_[n].tr for n in WNAMES}
        for s in range(nseq):
            tb = s * NT
            with contextlib.ExitStack() as es:
                xTa = kb.sbuf("xTa", [128, 8, S], BF16, es)
                wgr = [kb.sbuf("wgr%d" % i, [128, 8, 512], BF16, es) for i in range(2)]
                xts = [kb.sbuf("xt%d" % i, [128, D], F32, es) for i in range(2)]
                xbs = [kb.sbuf("xb%d" % i, [128, D], BF16, es) for i in range(2)]
                ugs = [kb.sbuf("ug%d" % i, [128, 512], F32, es) for i in range(4)]
                groups = []
                c0 = 0
                while c0 < INC:
                    groups.append((c0, min(512, INC - c0)))
                    c0 += 512

                def w_load(gi):
                    c0_, cw_ = groups[gi]
                    kb.dma("pool", wgr[gi % 2].t[:, :, 0:cw_], W["w_in"][:, c0_:c0_ + cw_].rearrange("(k p) c -> p k c", p=128),
                           R=[Wtr["w_in"]], W=[wgr[gi % 2].tr])
                w_load(0)
                w_load(1)
                for t in range(NT):
                    xt, xb = xts[t % 2], xbs[t % 2]
                    kb.dma("sp", xt.t[:], x_src.t[s, t * 128:(t + 1) * 128, :], R=[x_src.k(s)], W=[xt.tr])
                    kb.op("dve", lambda e: e.tensor_copy(out=xb.t[:], in_=xt.t[:]), R=[xt.tr], W=[xb.tr])
                    pa, pt = psbf(0, t % 2)
                    for kc in range(8):
                        tp(pa[:, kc * 128:(kc + 1) * 128], xb.t[:, kc * 128:(kc + 1) * 128], [xb.tr], pt, kc == 0)
                    kb.op("act", lambda e: e.copy(out=xTa.t[:, :, t * 128:(t + 1) * 128], in_=pa.rearrange("p (k t) -> p k t", k=8)),
                          R=[pt], Wa=[xTa.k(t)])
                it_ = 0
                for gi, (c0, cw) in enumerate(groups):
                    wg_ = wgr[gi % 2]
                    for t in range(NT):
                        pm, pmt = psb(1 + it_ % 2, (it_ // 2) % 2)
                        ug = ugs[it_ % 4]
                        for kc in range(8):
                            mm(pm[:, 0:cw], xTa.t[:, kc, t * 128:(t + 1) * 128], wg_.t[:, kc, 0:cw], kc == 0, kc == 7,
                               [xTa.k(t), wg_.tr], pmt, kc == 0, inc=(kc == 7))
                        if it_ % 2 == 0:
                            kb.op("act", lambda e: e.copy(out=ug.t[:, 0:cw], in_=pm[:, 0:cw]), R=[pmt], W=[ug.tr])
                        else:
                            kb.op("dve", lambda e: e.tensor_copy(out=ug.t[:, 0:cw], in_=pm[:, 0:cw]), R=[pmt], W=[ug.tr])
                        kb.dma("sp", u_d.t[t * 128:(t + 1) * 128, c0:c0 + cw], ug.t[:, 0:cw], R=[ug.tr], Wa=[u_d.tr])
                        it_ += 1
                    if gi + 2 < len(groups):
                        w_load(gi + 2)
                kb.barrier()
            if li == 0 and s == 0:
                tap("u", u_d.t[:], [u_d.tr], [S, INC], F32)
            STOP("P1")

            with contextlib.ExitStack() as es:
                w_uq = kb.sbuf("w_uq", [128, 2, 768], BF16, es)
                w_ukv = kb.sbuf("w_ukv", [128, 1024], BF16, es)
                stg = kb.sbuf("stg", [128, 1024], F32, es)
                qn = kb.sbuf("qn", [128, 3], F32, es)
                kb.dma("sp", qn.t[:, 0:2], W["mla_q_norm"].rearrange("(k p) -> p k", p=128), R=[Wtr["mla_q_norm"]], W=[qn.tr],
                       allow_slow_non_contiguous=True)
                kb.dma("sp", qn.t[:, 2:3], W["mla_kv_norm"].rearrange("(p o) -> p o", o=1), R=[Wtr["mla_kv_norm"]], Wa=[qn.tr])
                STOP("P2a0")
                for kc in range(2):
                    kb.dma("sp", stg.t[:, 0:768], W["mla_w_uq"][kc * 128:(kc + 1) * 128, :], R=[Wtr["mla_w_uq"]], W=[stg.tr])
                    kb.op("dve", lambda e: e.tensor_scalar(out=w_uq.t[:, kc, :], in0=stg.t[:, 0:768], scalar1=qn.t[:, kc:kc + 1],
                                                           scalar2=None, op0=ALU.mult), R=[stg.tr, qn.tr], Wa=[w_uq.tr])
                kb.dma("sp", stg.t[:, :], W["mla_w_ukv"][:, :], R=[Wtr["mla_w_ukv"]], W=[stg.tr])
                kb.op("dve", lambda e: e.tensor_scalar(out=w_ukv.t[:], in0=stg.t[:], scalar1=qn.t[:, 2:3], scalar2=None,
                                                       op0=ALU.mult), R=[stg.tr, qn.tr], W=[w_ukv.tr])
                STOP("P2a")
                QT = kb.sbuf("QT", [96, 8, S], BF16, es)
                KT = kb.sbuf("KT", [96, 8, S], BF16, es)
                VA = kb.sbuf("VA", [128, NT, 8, 65], BF16, es)
                Ost = kb.sbuf("Ost", [128, NT, 512], BF16, es)
                kb.op("dve", lambda e: e.memset(VA.t[:].rearrange("p a b c -> p (a b c)"), 1.0), W=[VA.tr])
                uus = [kb.sbuf("uu%d" % i, [128, 416], F32, es) for i in range(2)]
                cbs = [kb.sbuf("cb%d" % i, [128, 480], BF16, es) for i in range(2)]
                for cb in cbs:
                    kb.op("dve", lambda e: e.memset(cb.t[:], 0.0), W=[cb.tr])
                junk = kb.sbuf("junk", [128, 384], F32, es)
                ssq = kb.sbuf("ssq", [128, 2], F32, es)
                rs = kb.sbuf("rs", [128, 2], F32, es)
                rs2 = kb.sbuf("rs2", [128, 2], F32, es)
                rstd = kb.sbuf("rstd", [128, 2], F32, es)
                tm1 = kb.sbuf("tm1", [128, 256], F32, es)
                tm2 = kb.sbuf("tm2", [128, 256], F32, es)
                cT = kb.sbuf("cT", [128, 384], BF16, es)
                kp = kb.sbuf("kp", [128, 128], BF16, es)
                q32 = kb.sbuf("q32", [128, 768], F32, es)
                qb = kb.sbuf("qb", [128, 768], BF16, es)
                for t in range(NT):
                    uu, cb = uus[t % 2], cbs[t % 2]
                    cm, sm = cosT.t[:, tb + t, 0:16], sinT.t[:, tb + t, 0:16]
                    kb.dma("sp", uu.t[:], u_d.t[t * 128:(t + 1) * 128, 0:416], R=[u_d.tr], W=[uu.tr])
                    kb.op("act", lambda e: e.activation(out=junk.t[:, 0:384], in_=uu.t[:, 0:384], func=AF.Square),
                          R=[uu.tr], W=[junk.tr])
                    kb.op("dve", lambda e: e.reduce_sum(out=ssq.t[:, 0:1], in_=junk.t[:, 0:256], axis=AX.X), R=[junk.tr], W=[ssq.tr])
                    kb.op("dve", lambda e: e.reduce_sum(out=ssq.t[:, 1:2], in_=junk.t[:, 256:384], axis=AX.X), R=[junk.tr], Wa=[ssq.tr])
                    kb.op("dve", lambda e: e.tensor_scalar(out=rs.t[:, 0:1], in0=ssq.t[:, 0:1], scalar1=1.0 / 256, scalar2=RMS_EPS,
                                                           op0=ALU.mult, op1=ALU.add), R=[ssq.tr], W=[rs.tr])
                    kb.op("dve", lambda e: e.tensor_scalar(out=rs.t[:, 1:2], in0=ssq.t[:, 1:2], scalar1=1.0 / 128, scalar2=RMS_EPS,
                                                           op0=ALU.mult, op1=ALU.add), R=[ssq.tr], Wa=[rs.tr])
                    if t == 0:
                        STOP("P2b1")
                    kb.op("act", lambda e: e.activation(out=rs2.t[:], in_=rs.t[:], func=AF.Sqrt), R=[rs.tr], W=[rs2.tr])
                    kb.op("dve", lambda e: e.reciprocal(out=rstd.t[:], in_=rs2.t[:]), R=[rs2.tr], W=[rstd.tr])
                    kb.op("dve", lambda e: e.tensor_scalar(out=cb.t[:, 0:256], in0=uu.t[:, 0:256], scalar1=rstd.t[:, 0:1], scalar2=None,
                                                           op0=ALU.mult), R=[uu.tr, rstd.tr], W=[cb.tr])
                    kb.op("dve", lambda e: e.tensor_scalar(out=cb.t[:, 256:384], in0=uu.t[:, 256:384], scalar1=rstd.t[:, 1:2], scalar2=None,
                                                           op0=ALU.mult), R=[uu.tr, rstd.tr], Wa=[cb.tr])
                    if t == 0:
                        STOP("P2b3")
                    rope2("dve", uu.t[:, 384:416].rearrange("p (h d) -> p h d", h=1), cb.t[:, 448:480].rearrange("p (h d) -> p h d", h=1),
                          1, 16, cm, sm, tm1, tm2, [uu.tr], [cb.tr])
                    if t == 0:
                        STOP("P2b4")
                    pa, pt = psbf(0, 0)
                    tp(pa[:, 0:128], cb.t[:, 0:128], [cb.tr], pt, True)
                    tp(pa[:, 128:256], cb.t[:, 128:256], [cb.tr], pt, False)
                    tp(pa[:, 256:384], cb.t[:, 256:384], [cb.tr], pt, False)
                    tp(pa[0:96, 384:512], cb.t[:, 384:480], [cb.tr], pt, False)
                    kb.op("act", lambda e: e.copy(out=cT.t[:], in_=pa[:, 0:384]), R=[pt], W=[cT.tr])
                    if t == 0:
                        STOP("P2b6")
                    kb.op("act", lambda e: e.copy(out=kp.t[64:96, :], in_=pa[64:96, 384:512]), R=[pt], W=[kp.tr])
                    for h in range(8):
                        kb.op("dve", lambda e: e.tensor_copy(out=KT.t[64:96, h, t * 128:(t + 1) * 128], in_=kp.t[64:96, :]),
                              R=[kp.tr], Wa=[KT.tr])
                    if t == 0:
                        STOP("P2b")
                    for half, cw in ((0, 512), (1, 256)):
                        pq, pqt = psb(1, half)
                        for kc in range(2):
                            mm(pq[:, 0:cw], cT.t[:, kc * 128:(kc + 1) * 128], w_uq.t[:, kc, half * 512:half * 512 + cw], kc == 0, kc == 1,
                               [cT.tr, w_uq.tr], pqt, kc == 0)
                        kb.op("act", lambda e: e.copy(out=q32.t[:, half * 512:half * 512 + cw], in_=pq[:, 0:cw]), R=[pqt],
                              W=[q32.tr] if half == 0 else (), Wa=[q32.tr] if half else ())
                    kb.op("dve", lambda e: e.tensor_copy(out=qb.t[:], in_=q32.t[:]), R=[q32.tr], W=[qb.tr])
                    rope2("dve", q32.t[:].rearrange("p (h d) -> p h d", h=8)[:, :, 64:96],
                          qb.t[:].rearrange("p (h d) -> p h d", h=8)[:, :, 64:96], 8, 16, cm, sm, tm1, tm2, [q32.tr], [qb.tr])
                    pa2, pt2 = psbf(0, 1)
                    for h in range(8):
                        tp(pa2[0:96, h * 128:(h + 1) * 128], qb.t[:, h * 96:(h + 1) * 96], [qb.tr], pt2, h == 0)
                    kb.op("act", lambda e: e.copy(out=QT.t[0:96, :, t * 128:(t + 1) * 128],
                                                  in_=pa2[0:96, :].rearrange("p (h t) -> p h t", h=8)), R=[pt2], Wa=[QT.tr])
                    if t == 0:
                        STOP("P2c")
                    for hb in range(2):
                        pk, pkt = psb(2, hb)
                        for hh in range(4):
                            h = hb * 4 + hh
                            mm(pk[0:64, hh * 128:(hh + 1) * 128], w_ukv.t[:, h * 128:h * 128 + 64], cT.t[:, 256:384], True, True,
                               [cT.tr, w_ukv.tr], pkt, hh == 0)
                        kb.op("dve", lambda e: e.tensor_copy(out=KT.t[0:64, hb * 4:hb * 4 + 4, t * 128:(t + 1) * 128],
                                                             in_=pk[0:64, :].rearrange("p (h t) -> p h t", h=4)), R=[pkt], Wa=[KT.tr])
                    pv, pvt = psb(3, 0)
                    for h in range(8):
                        mm(pv[:, h * 64:(h + 1) * 64], cT.t[:, 256:384], w_ukv.t[:, h * 128 + 64:h * 128 + 128], True, True,
                           [cT.tr, w_ukv.tr], pvt, h == 0)
                    kb.op("act", lambda e: e.copy(out=VA.t[:, t, :, 0:64], in_=pv[:, 0:512].rearrange("p (h d) -> p h d", h=8)),
                          R=[pvt], Wa=[VA.tr])
                STOP("P2d")

                def m_mask(c, kb_, j0):
                    if kb_ >= 4 * c:
                        return dmask.t[:, 0, :], [dmask.tr], 128
                    return None
                attention(es, 8, None, lambda h, kb_: KT.t[0:96, h, kb_ * 128:(kb_ + 1) * 128],
                          lambda h, q0, q1: QT.t[0:96, h, q0:q1], lambda h, kb_: VA.t[:, kb_, h, :],
                          float(96 ** -0.5), m_mask, Ost, 0, [QT.tr, KT.tr, VA.tr])
                for t in range(NT):
                    kb.dma("sp", O_d.t[t * 128:(t + 1) * 128, 0:512], Ost.t[:, t, :], R=[Ost.tr], Wa=[O_d.tr])
                kb.barrier()
            if li == 0 and s == 0:
                tap("O_mla", O_d.t[:, 0:512], [O_d.tr], [S, 512], BF16)
            STOP("P2")

            with contextlib.ExitStack() as es:
                qTd = kb.sbuf("qTd", [64, 4, S], BF16, es)
                kTd = kb.sbuf("kTd", [64, S], BF16, es)
                VAd = kb.sbuf("VAd", [128, NT, 65], BF16, es)
                Osd = kb.sbuf("Osd", [128, NT, 256], BF16, es)
                offs = {}
                o_ = 0
                for b in range(2, NT):
                    offs[b] = o_
                    o_ += 128 * (b + 1)
                SC = kb.sbuf("SC", [128, o_], F32, es)
                lo = kb.sbuf("bs_lo", [128, 14], F32, es)
                hi = kb.sbuf("bs_hi", [128, 14], F32, es)
                wv = kb.sbuf("bs_w", [128, 14], F32, es)
                mid = kb.sbuf("bs_mid", [128, 14], F32, es)
                cnt = kb.sbuf("bs_cnt", [128, 14], F32, es)
                gef = kb.sbuf("bs_ge", [128, 14], F32, es)
                kb.op("dve", lambda e: e.memset(VAd.t[:].rearrange("p a b -> p (a b)"), 1.0), W=[VAd.tr])
                with contextlib.ExitStack() as es2:
                    QIT = kb.sbuf("QIT", [32, 8, S], BF16, es2)
                    kiT = kb.sbuf("kiT", [32, S], BF16, es2)
                    wiT = kb.sbuf("wiT", [32, NT, 128], BF16, es2)
                    kb.op("dve", lambda e: e.memset(wiT.t[:].rearrange("p a b -> p (a b)"), 0.0), W=[wiT.tr])
                    uds = [kb.sbuf("ud%d" % i_, [128, 680], F32, es2) for i_ in range(2)]
                    dbs = [kb.sbuf("db%d" % i_, [128, 680], BF16, es2) for i_ in range(2)]
                    tm1 = kb.sbuf("tm1d", [128, 64], F32, es2)
                    tm2 = kb.sbuf("tm2d", [128, 64], F32, es2)
                    Rg = kb.sbuf("Rg", [128, 8, 512], BF16, es2)
                    Wdb = kb.sbuf("Wdb", [128, 8, 128], BF16, es2)
                    wrep = kb.sbuf("wrep", [128, 128], BF16, es2)
                    ZLb = kb.sbuf("ZLb", [32, 8, 128], BF16, es2)
                    for t in range(NT):
                        ud, db = uds[t % 2], dbs[t % 2]
                        kb.dma("sp", ud.t[:], u_d.t[t * 128:(t + 1) * 128, 416:1096], R=[u_d.tr], W=[ud.tr])
                        kb.op("dve", lambda e: e.tensor_copy(out=db.t[:], in_=ud.t[:]), R=[ud.tr], W=[db.tr])
                        rope2("dve", ud.t[:, 0:320].rearrange("p (h d) -> p h d", h=5), db.t[:, 0:320].rearrange("p (h d) -> p h d", h=5),
                              5, 8, cosT.t[:, tb + t, 16:24], sinT.t[:, tb + t, 16:24], tm1, tm2, [ud.tr], [db.tr])
                        rope2("dve", ud.t[:, 384:672].rearrange("p (h d) -> p h d", h=9), db.t[:, 384:672].rearrange("p (h d) -> p h d", h=9),
                              9, 4, cosT.t[:, tb + t, 24:28], sinT.t[:, tb + t, 24:28], tm1, tm2, [ud.tr], [db.tr])
                        kb.op("act", lambda e: e.copy(out=VAd.t[:, t, 0:64], in_=ud.t[:, 320:384]), R=[ud.tr], Wa=[VAd.tr])
                        pA, ptA = psbf(0, 0)
                        pB, ptB = psbf(0, 1)
                        for h in range(4):
                            tp(pA[0:64, h * 128:(h + 1) * 128], db.t[:, h * 64:(h + 1) * 64], [db.tr], ptA, h == 0)
                        tp(pA[0:64, 512:640], db.t[:, 256:320], [db.tr], ptA, False)
                        tp(pA[0:32, 640:768], db.t[:, 640:672], [db.tr], ptA, False)
                        tp(pA[0:8, 768:896], db.t[:, 672:680], [db.tr], ptA, False)
                        for h in range(8):
                            tp(pB[0:32, h * 128:(h + 1) * 128], db.t[:, 384 + 32 * h:416 + 32 * h], [db.tr], ptB, h == 0)
                        kb.op("act", lambda e: e.copy(out=qTd.t[0:64, :, t * 128:(t + 1) * 128],
                                                      in_=pA[0:64, 0:512].rearrange("p (k t) -> p k t", k=4)), R=[ptA], Wa=[qTd.tr])
                        kb.op("act", lambda e: e.copy(out=kTd.t[0:64, t * 128:(t + 1) * 128], in_=pA[0:64, 512:640]), R=[ptA], Wa=[kTd.tr])
                        kb.op("act", lambda e: e.copy(out=kiT.t[0:32, t * 128:(t + 1) * 128], in_=pA[0:32, 640:768]), R=[ptA], Wa=[kiT.tr])
                        kb.op("act", lambda e: e.copy(out=wiT.t[0:8, t, :], in_=pA[0:8, 768:896]), R=[ptA], Wa=[wiT.tr])
                        kb.op("act", lambda e: e.copy(out=QIT.t[0:32, :, t * 128:(t + 1) * 128],
                                                      in_=pB[0:32, 0:1024].rearrange("p (k t) -> p k t", k=8)), R=[ptB], Wa=[QIT.tr])
                    STOP("P3a")
                    Rg2 = [Rg, kb.sbuf("Rgb", [128, 8, 512], BF16, es2)]
                    Wd2 = [Wdb, kb.sbuf("Wdbb", [128, 8, 128], BF16, es2)]
                    units = []
                    for b in range(2, NT):
                        N = 128 * (b + 1)
                        for kc in range((N + 511) // 512):
                            units.append((b, kc, N, kc * 512, min(512, N - kc * 512)))
                    zbanks = [psb(0, 0), psb(0, 1), psb(2, 0), psb(2, 1)]
                    zi = [0]

                    def zphase(u):
                        b, kc, N, k0, ncol = units[u]
                        Wdb_ = Wd2[b % 2]
                        Rg_ = Rg2[u % 2]
                        if kc == 0:
                            pw, pwt = psb(3, 0)
                            mm(pw[:, 0:128], rt.t[0:32, :], wiT.t[0:32, b, :], True, True, [rt.tr, wiT.tr], pwt, True)
                            kb.op("act", lambda e: e.copy(out=wrep.t[:], in_=pw[:, 0:128]), R=[pwt], W=[wrep.tr])
                            kb.op("dve", lambda e: e.tensor_tensor(out=Wdb_.t[:], in0=wrep.t[:].unsqueeze(1).to_broadcast([128, 8, 128]),
                                                                   in1=selmask.t[:], op=ALU.mult), R=[wrep.tr, selmask.tr], W=[Wdb_.tr])
                            for g in range(8):
                                kb.op("dve", lambda e: e.tensor_copy(
                                    out=ZLb.t[0:32, g, :].rearrange("p (h q) -> p h q", h=8),
                                    in_=QIT.t[0:32, :, b * 128 + 16 * g:b * 128 + 16 * g + 16]), R=[QIT.tr],
                                    W=[ZLb.tr] if g == 0 else (), Wa=[ZLb.tr] if g else ())
                        for g in range(8):
                            zv, zt_ = zbanks[zi[0] % 4]
                            zi[0] += 1
                            mm(zv[:, 0:ncol], ZLb.t[0:32, g, :], kiT.t[0:32, k0:k0 + ncol], True, True, [ZLb.tr, kiT.tr], zt_, True)
                            if g % 2 == 0:
                                kb.op("act", lambda e: e.activation(out=Rg_.t[:, g, 0:ncol], in_=zv[:, 0:ncol], func=AF.Relu),
                                      R=[zt_], Wa=[Rg_.k(g)])
                            else:
                                kb.op("dve", lambda e: e.tensor_scalar(out=Rg_.t[:, g, 0:ncol], in0=zv[:, 0:ncol], scalar1=0.0, scalar2=None,
                                                                       op0=ALU.max), R=[zt_], Wa=[Rg_.k(g)])

                    def sphase(u):
                        b, kc, N, k0, ncol = units[u]
                        Wdb_ = Wd2[b % 2]
                        Rg_ = Rg2[u % 2]
                        scv, sct = psb(1, u % 2)
                        for g in range(8):
                            mm(scv[:, 0:ncol], Wdb_.t[:, g, :], Rg_.t[:, g, 0:ncol], g == 0, g == 7, [Wdb_.tr, Rg_.k(g)], sct, g == 0, inc=(g == 7))
                        last = (k0 + ncol == N)
                        nplain = ncol - 128 if last else ncol
                        if nplain > 0:
                            kb.op("act", lambda e: e.copy(out=SC.t[:, offs[b] + k0:offs[b] + k0 + nplain], in_=scv[:, 0:nplain]),
                                  R=[sct], Wa=[SC.k(b)])
                        if last:
                            kb.op("act", lambda e: e.copy(out=SC.t[:, offs[b] + N - 128:offs[b] + N], in_=scv[:, ncol - 128:ncol]),
                                  R=[sct], Wa=[SC.k(b)])
                            kb.op("pool", lambda e: e.tensor_tensor(out=SC.t[:, offs[b] + N - 128:offs[b] + N],
                                                                   in0=SC.t[:, offs[b] + N - 128:offs[b] + N],
                                                                   in1=tribias.t[:], op=ALU.add), R=[SC.k(b), tribias.tr], Wa=[SC.k(b)])

                    zphase(0)
                    for u in range(len(units)):
                        if u + 1 < len(units):
                            zphase(u + 1)
                        sphase(u)
                    kb.barrier()
                STOP("P3b")
                junkb = kb.sbuf("junkb", [128, S], BF16, es)
                tmpb = kb.sbuf("bs_tmp", [128, 14], F32, es)
                sct_all = [SC.k(b) for b in range(2, NT)]
                for i_, b in enumerate(range(2, NT)):
                    N = 128 * (b + 1)
                    kb.op("dve", lambda e: e.reduce_max(out=hi.t[:, i_:i_ + 1], in_=SC.t[:, offs[b]:offs[b] + N], axis=AX.X),
                          R=[SC.k(b)], Wa=[hi.tr])
                    kb.op("dve", lambda e: e.tensor_reduce(out=lo.t[:, i_:i_ + 1], in_=SC.t[:, offs[b]:offs[b] + N - 128], axis=AX.X, op=ALU.min),
                          R=[SC.k(b)], Wa=[lo.tr])
                kb.op("dve", lambda e: e.tensor_scalar(out=lo.t[:], in0=lo.t[:], scalar1=-1.0, scalar2=None, op0=ALU.add), R=[lo.tr], W=[lo.tr])
                kb.op("dve", lambda e: e.tensor_scalar(out=hi.t[:], in0=hi.t[:], scalar1=1.0, scalar2=None, op0=ALU.add), R=[hi.tr], W=[hi.tr])
                kb.op("dve", lambda e: e.tensor_tensor(out=wv.t[:], in0=hi.t[:], in1=lo.t[:], op=ALU.subtract), R=[hi.tr, lo.tr], W=[wv.tr])
                junka = kb.sbuf("junka", [128, S], BF16, es)
                nmid = kb.sbuf("bs_nmid", [128, 14], F32, es)
                thrv = kb.sbuf("bs_thr", [128, 14], F32, es)
                for i_, b in enumerate(range(2, NT)):
                    kb.op("dve", lambda e: e.memset(thrv.t[:, i_:i_ + 1], 255.5 if i_ % 2 == 0 else float(511 - 128 * (b + 1))),
                          W=[thrv.tr] if i_ == 0 else (), Wa=[thrv.tr] if i_ else ())
                for itb in range(NBIS):
                    kb.op("dve", lambda e: e.tensor_scalar(out=wv.t[:], in0=wv.t[:], scalar1=0.5, scalar2=None, op0=ALU.mult), R=[wv.tr], W=[wv.tr])
                    kb.op("dve", lambda e: e.tensor_tensor(out=mid.t[:], in0=lo.t[:], in1=wv.t[:], op=ALU.add), R=[lo.tr, wv.tr], W=[mid.tr])
                    kb.op("dve", lambda e: e.tensor_scalar(out=nmid.t[:], in0=mid.t[:], scalar1=-1.0, scalar2=None, op0=ALU.mult),
                          R=[mid.tr], W=[nmid.tr])
                    for i_, b in enumerate(range(2, NT)):
                        N = 128 * (b + 1)
                        if i_ % 2 == 0:
                            kb.op("dve", lambda e: e.tensor_scalar(out=junkb.t[:, 0:N], in0=SC.t[:, offs[b]:offs[b] + N], scalar1=mid.t[:, i_:i_ + 1],
                                                                   scalar2=None, op0=ALU.is_ge, op1=ALU.add, accum_out=cnt.t[:, i_:i_ + 1]),
                                  R=[SC.k(b), mid.tr], W=[junkb.tr], Wa=[cnt.k(i_)])
                        else:
                            kb.op("act", lambda e: e.activation(out=junka.t[:, 0:N], in_=SC.t[:, offs[b]:offs[b] + N], func=AF.Sign,
                                                                bias=nmid.t[:, i_:i_ + 1], scale=1.0, accum_out=cnt.t[:, i_:i_ + 1]),
                                  R=[SC.k(b), nmid.tr], W=[junka.tr], Wa=[cnt.k(i_)])
                    kb.op("dve", lambda e: e.tensor_tensor(out=gef.t[:], in0=cnt.t[:], in1=thrv.t[:], op=ALU.is_ge),
                          R=[cnt.k(i__) for i__ in range(14)] + [thrv.tr], W=[gef.tr])
                    kb.op("dve", lambda e: e.tensor_tensor(out=tmpb.t[:], in0=gef.t[:], in1=wv.t[:], op=ALU.mult), R=[gef.tr, wv.tr], W=[tmpb.tr])
                    kb.op("dve", lambda e: e.tensor_tensor(out=lo.t[:], in0=lo.t[:], in1=tmpb.t[:], op=ALU.add), R=[lo.tr, tmpb.tr], W=[lo.tr])
                STOP("P3c")
                mq = kb.sbuf("mq", [128, S], BF16, es)
                maskT = kb.sbuf("maskT", [128, NT, 512], BF16, es)
                Pb = [kb.sbuf("Pbd%d" % i_, [128, 512], BF16, es) for i_ in range(2)]
                rden = kb.sbuf("rdend", [128, 4], F32, es)
                tq = 0
                for c in range(4):
                    for j in range(4):
                        b = 4 * c + j
                        N = 128 * (b + 1)
                        if b >= 2:
                            kb.op("dve", lambda e: e.tensor_scalar(out=mq.t[:, 0:N], in0=SC.t[:, offs[b]:offs[b] + N], scalar1=lo.t[:, b - 2:b - 1],
                                                                   scalar2=None, op0=ALU.is_ge), R=[SC.k(b), lo.tr], W=[mq.tr])
                            for kb0 in range(0, b + 1, 8):
                                n_ = min(8, b + 1 - kb0)
                                pm_, pmt_ = psbf(2, tq % 2)
                                tq += 1
                                for i_ in range(n_):
                                    tp(pm_[:, i_ * 128:(i_ + 1) * 128], mq.t[:, (kb0 + i_) * 128:(kb0 + i_ + 1) * 128], [mq.tr], pmt_, i_ == 0)
                                kb.op("act", lambda e: e.copy(out=maskT.t[:, kb0:kb0 + n_, j * 128:(j + 1) * 128],
                                                              in_=pm_[:, 0:n_ * 128].rearrange("p (k q) -> p k q", k=n_)), R=[pmt_], Wa=[maskT.tr])
                        else:
                            for kb_ in range(b + 1):
                                kb.op("dve", lambda e: e.tensor_copy(out=maskT.t[:, kb_, j * 128:(j + 1) * 128],
                                                                     in_=dmask.t[:, 0 if kb_ == b else 7, :]), R=[dmask.tr], Wa=[maskT.tr])

                    def d_mask(c_, kb_, j0):
                        return maskT.t[:, kb_, 128 * j0:512], [maskT.tr], 512 - 128 * j0
                    attention((Pb, rden), 4, [c], lambda h, kb_: kTd.t[0:64, kb_ * 128:(kb_ + 1) * 128],
                              lambda h, q0, q1: qTd.t[0:64, h, q0:q1], lambda h, kb_: VAd.t[:, kb_, :],
                              0.125, d_mask, Osd, 0, [qTd.tr, kTd.tr, VAd.tr])
                for t in range(NT):
                    kb.dma("sp", O_d.t[t * 128:(t + 1) * 128, 512:768], Osd.t[:, t, :], R=[Osd.tr], Wa=[O_d.tr])
                kb.barrier()
            if li == 0 and s == 0:
                tap("O_dsa", O_d.t[:, 512:768], [O_d.tr], [S, 256], BF16)
            STOP("P3")

            es_wo = contextlib.ExitStack()
            w_o = kb.sbuf("w_o", [128, 8, D], BF16, es_wo)
            for kc in range(8):
                kb.dma("pool", w_o.t[:, kc, :], W["w_o"][kc * 128:(kc + 1) * 128, :], R=[Wtr["w_o"]], Wa=[w_o.tr],
                       max_dma_last_dim=4096)
            with contextlib.ExitStack() as es:
                qTc = kb.sbuf("qTc", [64, 12, S], BF16, es)
                kTc = kb.sbuf("kTc", [64, 12, S], BF16, es)
                VAc = kb.sbuf("VAc", [128, NT, 12, 65], BF16, es)
                Osc = kb.sbuf("Osc", [128, NT, 256], BF16, es)
                kb.op("dve", lambda e: e.memset(VAc.t[:].rearrange("p a b c -> p (a b c)"), 1.0), W=[VAc.tr])
                ucs = [kb.sbuf("uc%d" % i_, [128, 2304], F32, es) for i_ in range(2)]
                cbfs = [kb.sbuf("cbf%d" % i_, [128, 1536], BF16, es) for i_ in range(2)]
                tm1 = kb.sbuf("tm1c", [128, 256], F32, es)
                tm2 = kb.sbuf("tm2c", [128, 256], F32, es)
                for t in range(NT):
                    uc, cbf = ucs[t % 2], cbfs[t % 2]
                    ch, sh_ = cosT.t[:, tb + t, 16:24], sinT.t[:, tb + t, 16:24]
                    kb.dma("sp", uc.t[:], u_d.t[t * 128:(t + 1) * 128, 1096:3400], R=[u_d.tr], W=[uc.tr])
                    kb.op("dve", lambda e: e.tensor_copy(out=cbf.t[:], in_=uc.t[:, 0:1536]), R=[uc.tr], W=[cbf.tr])
                    rope2("dve", uc.t[:, 0:1536].rearrange("p (h d) -> p h d", h=24), cbf.t[:].rearrange("p (h d) -> p h d", h=24),
                          24, 8, ch, sh_, tm1, tm2, [uc.tr], [cbf.tr])
                    if t == 0:
                        STOP("P4a1")
                    kb.op("act", lambda e: e.copy(out=VAc.t[:, t, :, 0:64], in_=uc.t[:, 1536:2304].rearrange("p (h d) -> p h d", h=12)),
                          R=[uc.tr], Wa=[VAc.tr])
                    for (src0, dstb, bk) in ((0, qTc, 0), (768, kTc, 2)):
                        pA, ptA = psbf(bk, 0)
                        pB, ptB = psbf(bk, 1)
                        for h in range(12):
                            if h < 8:
                                tp(pA[0:64, h * 128:(h + 1) * 128], cbf.t[:, src0 + h * 64:src0 + (h + 1) * 64], [cbf.tr], ptA, h == 0)
                            else:
                                tp(pB[0:64, (h - 8) * 128:(h - 7) * 128], cbf.t[:, src0 + h * 64:src0 + (h + 1) * 64], [cbf.tr], ptB, h == 8)
                        kb.op("act", lambda e: e.copy(out=dstb.t[0:64, 0:8, t * 128:(t + 1) * 128],
                                                      in_=pA[0:64, 0:1024].rearrange("p (k t) -> p k t", k=8)), R=[ptA], Wa=[dstb.tr])
                        kb.op("act", lambda e: e.copy(out=dstb.t[0:64, 8:12, t * 128:(t + 1) * 128],
                                                      in_=pB[0:64, 0:512].rearrange("p (k t) -> p k t", k=4)), R=[ptB], Wa=[dstb.tr])
                STOP("P4a")
                Pc = [kb.sbuf("Pc%d" % i_, [128, 512], BF16, es) for i_ in range(2)]
                rdc = kb.sbuf("rdc", [128, 4], F32, es)
                Rops = [qTc.tr, kTc.tr, VAc.tr]
                dit = []
                for b in range(NT):
                    blocks = []
                    for kb_ in range(max(0, b - 1), b + 1):
                        blocks.append((0, kb_, 0 if kb_ == b else 1))
                    for kb_ in range(max(0, b - 4), b + 1):
                        d_ = b - kb_
                        blocks.append((1, kb_, 2 if d_ == 0 else (4 if d_ == 4 else 3)))
                    for kb_ in range(0, b + 1):
                        blocks.append((2, kb_, 5 if kb_ == b else 6))
                    for bi, (g, kb_, midx) in enumerate(blocks):
                        dit.append((b, bi, len(blocks), g, kb_, midx))

                def d_qk(i):
                    b, bi, nb, g, kb_, midx = dit[i]
                    sv_, st_ = psb(2, i % 2)
                    for jj in range(4):
                        hh = 4 * g + jj
                        mm(sv_[:, jj * 128:(jj + 1) * 128], kTc.t[0:64, hh, kb_ * 128:(kb_ + 1) * 128],
                           qTc.t[0:64, hh, b * 128:(b + 1) * 128], True, True, Rops, st_, jj == 0)

                d_qk(0)
                for i, (b, bi, nb, g, kb_, midx) in enumerate(dit):
                    if i + 1 < len(dit):
                        d_qk(i + 1)
                    accv, acct = psb(1, b % 2)
                    acc3 = accv[:, 0:260].rearrange("p (j d) -> p j d", j=4)
                    sv_, st_ = psb(2, i % 2)
                    P = Pc[i % 2]
                    kb.op("act", lambda e: e.activation(out=P.t[:], in_=sv_[:, 0:512], func=AF.Exp, scale=0.125), R=[st_], W=[P.tr])
                    kb.op("dve", lambda e: e.tensor_tensor(out=P.t[:].rearrange("p (j q) -> p j q", j=4),
                                                           in0=P.t[:].rearrange("p (j q) -> p j q", j=4),
                                                           in1=dmask.t[:, midx, :].unsqueeze(1).to_broadcast([128, 4, 128]), op=ALU.mult),
                          R=[P.tr, dmask.tr], W=[P.tr])
                    for jj in range(4):
                        mm(acc3[:, jj, :], P.t[:, jj * 128:(jj + 1) * 128], VAc.t[:, kb_, 4 * g + jj, :],
                           (bi == 0 and jj == 0), (bi == nb - 1 and jj == 3), [P.tr] + Rops, acct, (bi == 0 and jj == 0))
                    if bi == nb - 1:
                        kb.op("dve", lambda e: e.reciprocal(out=rdc.t[:, 0:4], in_=acc3[:, :, 64]), R=[acct], W=[rdc.tr])
                        kb.op("dve", lambda e: e.tensor_tensor(out=Osc.t[:, b, :].rearrange("p (j d) -> p j d", j=4), in0=acc3[:, :, 0:64],
                                                               in1=rdc.t[:, 0:4].unsqueeze(2).to_broadcast([128, 4, 64]), op=ALU.mult),
                              R=[acct, rdc.tr], Wa=[Osc.tr])
                for t in range(NT):
                    kb.dma("sp", O_d.t[t * 128:(t + 1) * 128, 768:1024], Osc.t[:, t, :], R=[Osc.tr], Wa=[O_d.tr])
                kb.barrier()
            if li == 0 and s == 0:
                tap("O_dil", O_d.t[:, 768:1024], [O_d.tr], [S, 256], BF16)
            STOP("P4")

            with contextlib.ExitStack() as es:
                yacc = kb.sbuf("yacc", [128, NT, D], F32, es)
                x1T = kb.sbuf("x1T", [128, 8, S], BF16, es)
                gate = kb.sbuf("gate", [128, NT, 32], F32, es)
                lng = kb.sbuf("lng", [128, 4, D], F32, es)
                for i_, n_ in enumerate(("ln1_g", "ln1_b", "ln2_g", "ln2_b")):
                    kb.dma("sp", lng.t[:, i_, :], W[n_].rearrange("(o d) -> o d", o=1).partition_broadcast(128), R=[Wtr[n_]], Wa=[lng.tr])
                zb = kb.sbuf("zb", [128, D], F32, es)
                st6 = kb.sbuf("st6", [128, 2, 6], F32, es)
                mv = kb.sbuf("mv", [128, 2], F32, es)
                r1 = kb.sbuf("r1", [128, 1], F32, es)
                r2 = kb.sbuf("r2", [128, 1], F32, es)
                r3 = kb.sbuf("r3", [128, 1], F32, es)

                def layer_norm(src_ap, src_tr, gi, out_ap, out_tr):
                    for hh in range(2):
                        kb.op("dve", lambda e: e.bn_stats(out=st6.t[:, hh, :], in_=src_ap[:, hh * 512:(hh + 1) * 512]),
                              R=src_tr, W=[st6.tr] if hh == 0 else (), Wa=[st6.tr] if hh else ())
                    kb.op("dve", lambda e: e.bn_aggr(out=mv.t[:], in_=st6.t[:].rearrange("p a b -> p (a b)")), R=[st6.tr], W=[mv.tr])
                    kb.op("dve", lambda e: e.tensor_scalar(out=r1.t[:], in0=mv.t[:, 1:2], scalar1=LN_EPS, scalar2=None, op0=ALU.add),
                          R=[mv.tr], W=[r1.tr])
                    kb.op("act", lambda e: e.activation(out=r2.t[:], in_=r1.t[:], func=AF.Sqrt), R=[r1.tr], W=[r2.tr])
                    kb.op("dve", lambda e: e.reciprocal(out=r3.t[:], in_=r2.t[:]), R=[r2.tr], W=[r3.tr])
                    kb.op("dve", lambda e: e.tensor_scalar(out=zb.t[:], in0=src_ap, scalar1=mv.t[:, 0:1], scalar2=r3.t[:, 0:1],
                                                           op0=ALU.subtract, op1=ALU.mult), R=list(src_tr) + [mv.tr, r3.tr], W=[zb.tr])
                    kb.op("dve", lambda e: e.tensor_tensor(out=zb.t[:], in0=zb.t[:], in1=lng.t[:, gi, :], op=ALU.mult),
                          R=[zb.tr, lng.tr], W=[zb.tr])
                    kb.op("dve", lambda e: e.tensor_tensor(out=out_ap, in0=zb.t[:], in1=lng.t[:, gi + 1, :], op=ALU.add),
                          R=[zb.tr, lng.tr], W=out_tr)

                with contextlib.ExitStack() as es5:
                    rwf = kb.sbuf("rwf", [128, 8, 36], F32, es5)
                    rw = kb.sbuf("rw", [128, 8, 36], BF16, es5)
                    rbias = kb.sbuf("rbias", [128, 36], F32, es5)
                    kb.dma("sp", rwf.t[:, :, 0:4], W["router_group_w"].rearrange("(k p) g -> p k g", p=128), R=[Wtr["router_group_w"]],
                           Wa=[rwf.tr])
                    for g_ in range(4):
                        kb.dma("sp", rwf.t[:, :, 4 + 8 * g_:12 + 8 * g_], W["router_expert_w"][g_].rearrange("(k p) e -> p k e", p=128),
                               R=[Wtr["router_expert_w"]], Wa=[rwf.tr])
                    kb.op("dve", lambda e: e.tensor_copy(out=rw.t[:], in_=rwf.t[:]), R=[rwf.tr], W=[rw.tr])
                    kb.dma("sp", rbias.t[:, 0:4], W["router_group_b"].rearrange("(o g) -> o g", o=1).partition_broadcast(128),
                           R=[Wtr["router_group_b"]], Wa=[rbias.tr])
                    kb.dma("sp", rbias.t[:, 4:36], W["router_expert_b"].rearrange("(o g) e -> o (g e)", o=1).partition_broadcast(128),
                           R=[Wtr["router_expert_b"]], Wa=[rbias.tr])
                    Ots = [kb.sbuf("Ot%d" % i, [128, D], BF16, es5) for i in range(2)]
                    OTs = [kb.sbuf("OT%d" % i, [128, 8, 128], BF16, es5) for i in range(2)]
                    xts = [kb.sbuf("xq%d" % i, [128, D], F32, es5) for i in range(2)]
                    zs = kb.sbuf("zs", [128, D], F32, es5)
                    x1 = kb.sbuf("x1", [128, D], F32, es5)
                    x1b = kb.sbuf("x1b", [128, D], BF16, es5)
                    lgA = kb.sbuf("lgA", [128, NT, 36], F32, es5)
                    for t in range(NT):
                        Ot, OT, xt = Ots[t % 2], OTs[t % 2], xts[t % 2]
                        kb.dma("sp", Ot.t[:], O_d.t[t * 128:(t + 1) * 128, :], R=[O_d.tr], W=[Ot.tr])
                        kb.dma("sp", xt.t[:], x_src.t[s, t * 128:(t + 1) * 128, :], R=[x_src.k(s)], W=[xt.tr])
                        pa, pt = psbf(0, t % 2)
                        for kc in range(8):
                            tp(pa[:, kc * 128:(kc + 1) * 128], Ot.t[:, kc * 128:(kc + 1) * 128], [Ot.tr], pt, kc == 0)
                        kb.op("act", lambda e: e.copy(out=OT.t[:].rearrange("p k t -> p (k t)"), in_=pa), R=[pt], W=[OT.tr])
                        for half in range(2):
                            pm, pmt = psb(1, half)
                            for kc in range(8):
                                mm(pm[:, 0:512], OT.t[:, kc, :], w_o.t[:, kc, half * 512:(half + 1) * 512], kc == 0, kc == 7,
                                   [OT.tr, w_o.tr], pmt, kc == 0)
                            kb.op("dve", lambda e: e.scalar_tensor_tensor(out=zs.t[:, half * 512:(half + 1) * 512],
                                                                          in0=xt.t[:, half * 512:(half + 1) * 512], scalar=ALPHA,
                                                                          in1=pm[:, 0:512], op0=ALU.mult, op1=ALU.add),
                                  R=[xt.tr, pmt], W=[zs.tr] if half == 0 else (), Wa=[zs.tr] if half else ())
                        layer_norm(zs.t[:], [zs.tr], 0, x1.t[:], [x1.tr])
                        kb.op("act", lambda e: e.mul(out=yacc.t[:, t, :], in_=x1.t[:], mul=ALPHA), R=[x1.tr], W=[yacc.k(t)])
                        kb.op("dve", lambda e: e.tensor_copy(out=x1b.t[:], in_=x1.t[:]), R=[x1.tr], W=[x1b.tr])
                        pa2, pt2 = psbf(2, t % 2)
                        for kc in range(8):
                            tp(pa2[:, kc * 128:(kc + 1) * 128], x1b.t[:, kc * 128:(kc + 1) * 128], [x1b.tr], pt2, kc == 0)
                        kb.op("act", lambda e: e.copy(out=x1T.t[:, :, t * 128:(t + 1) * 128],
                                                      in_=pa2.rearrange("p (k t) -> p k t", k=8)), R=[pt2], Wa=[x1T.k(t)])
                        pr, prt = psb(3, t % 2)
                        for kc in range(8):
                            mm(pr[:, 0:36], x1T.t[:, kc, t * 128:(t + 1) * 128], rw.t[:, kc, :], kc == 0, kc == 7, [x1T.k(t), rw.tr], prt, kc == 0)
                        kb.op("act", lambda e: e.copy(out=lgA.t[:, t, :], in_=pr[:, 0:36]), R=[prt], Wa=[lgA.tr])
                    def B3(ap2, n):
                        return ap2.unsqueeze(2).to_broadcast([128, NT, n])
                    rbuf = {n_: kb.sbuf("rb_" + n_, [128, NT, w_], F32, es5) for n_, w_ in
                            (("ghot", 4), ("dg", 4), ("eg", 4), ("le", 8), ("tmp", 8), ("eq", 8), ("le2", 8), ("dl", 8), ("ex", 8),
                             ("sel", 8), ("exm", 8), ("ge", 8))}
                    rv = {n_: kb.sbuf("rv_" + n_, [128, NT], F32, es5) for n_ in ("gm", "sg", "ptop", "m1", "m2", "den", "rd", "fac")}

                    def tt(out, in0, in1, op_, R_, Wt, eng="dve"):
                        kb.op(eng, lambda e: e.tensor_tensor(out=out, in0=in0, in1=in1, op=op_), R=R_, W=[Wt])
                    kb.op("dve", lambda e: e.tensor_tensor(out=lgA.t[:], in0=lgA.t[:], in1=rbias.t[:].unsqueeze(1).to_broadcast([128, NT, 36]),
                                                           op=ALU.add), R=[lgA.tr, rbias.tr], W=[lgA.tr])
                    LG = lgA.t[:, :, 0:4]
                    kb.op("dve", lambda e: e.reduce_max(out=rv["gm"].t[:], in_=LG, axis=AX.X), R=[lgA.tr], W=[rv["gm"].tr])
                    tt(rbuf["ghot"].t[:], LG, B3(rv["gm"].t[:], 4), ALU.is_ge, [lgA.tr, rv["gm"].tr], rbuf["ghot"].tr)
                    tt(rbuf["dg"].t[:], LG, B3(rv["gm"].t[:], 4), ALU.subtract, [lgA.tr, rv["gm"].tr], rbuf["dg"].tr)
                    kb.op("act", lambda e: e.activation(out=rbuf["eg"].t[:], in_=rbuf["dg"].t[:], func=AF.Exp), R=[rbuf["dg"].tr], W=[rbuf["eg"].tr])
                    kb.op("dve", lambda e: e.reduce_sum(out=rv["sg"].t[:], in_=rbuf["eg"].t[:], axis=AX.X), R=[rbuf["eg"].tr], W=[rv["sg"].tr])
                    kb.op("dve", lambda e: e.reciprocal(out=rv["ptop"].t[:], in_=rv["sg"].t[:]), R=[rv["sg"].tr], W=[rv["ptop"].tr])
                    for g_ in range(4):
                        dst = rbuf["le"] if g_ == 0 else rbuf["tmp"]
                        tt(dst.t[:], lgA.t[:, :, 4 + 8 * g_:12 + 8 * g_], rbuf["ghot"].t[:, :, g_:g_ + 1].to_broadcast([128, NT, 8]), ALU.mult,
                           [lgA.tr, rbuf["ghot"].tr], dst.tr)
                        if g_:
                            tt(rbuf["le"].t[:], rbuf["le"].t[:], rbuf["tmp"].t[:], ALU.add, [rbuf["le"].tr, rbuf["tmp"].tr], rbuf["le"].tr)
                    kb.op("dve", lambda e: e.reduce_max(out=rv["m1"].t[:], in_=rbuf["le"].t[:], axis=AX.X), R=[rbuf["le"].tr], W=[rv["m1"].tr])
                    tt(rbuf["eq"].t[:], rbuf["le"].t[:], B3(rv["m1"].t[:], 8), ALU.is_ge, [rbuf["le"].tr, rv["m1"].tr], rbuf["eq"].tr)
                    kb.op("dve", lambda e: e.scalar_tensor_tensor(out=rbuf["le2"].t[:], in0=rbuf["eq"].t[:], scalar=-1e30, in1=rbuf["le"].t[:],
                                                                  op0=ALU.mult, op1=ALU.add), R=[rbuf["eq"].tr, rbuf["le"].tr], W=[rbuf["le2"].tr])
                    kb.op("dve", lambda e: e.reduce_max(out=rv["m2"].t[:], in_=rbuf["le2"].t[:], axis=AX.X), R=[rbuf["le2"].tr], W=[rv["m2"].tr])
                    tt(rbuf["dl"].t[:], rbuf["le"].t[:], B3(rv["m1"].t[:], 8), ALU.subtract, [rbuf["le"].tr, rv["m1"].tr], rbuf["dl"].tr)
                    kb.op("act", lambda e: e.activation(out=rbuf["ex"].t[:], in_=rbuf["dl"].t[:], func=AF.Exp), R=[rbuf["dl"].tr], W=[rbuf["ex"].tr])
                    tt(rbuf["sel"].t[:], rbuf["le"].t[:], B3(rv["m2"].t[:], 8), ALU.is_ge, [rbuf["le"].tr, rv["m2"].tr], rbuf["sel"].tr)
                    tt(rbuf["exm"].t[:], rbuf["ex"].t[:], rbuf["sel"].t[:], ALU.mult, [rbuf["ex"].tr, rbuf["sel"].tr], rbuf["exm"].tr)
                    kb.op("dve", lambda e: e.reduce_sum(out=rv["den"].t[:], in_=rbuf["exm"].t[:], axis=AX.X), R=[rbuf["exm"].tr], W=[rv["den"].tr])
                    kb.op("dve", lambda e: e.reciprocal(out=rv["rd"].t[:], in_=rv["den"].t[:]), R=[rv["den"].tr], W=[rv["rd"].tr])
                    tt(rv["fac"].t[:], rv["rd"].t[:], rv["ptop"].t[:], ALU.mult, [rv["rd"].tr, rv["ptop"].tr], rv["fac"].tr)
                    tt(rbuf["ge"].t[:], rbuf["exm"].t[:], B3(rv["fac"].t[:], 8), ALU.mult, [rbuf["exm"].tr, rv["fac"].tr], rbuf["ge"].tr)
                    for g_ in range(4):
                        kb.op("dve", lambda e: e.tensor_tensor(out=gate.t[:, :, 8 * g_:8 * g_ + 8],
                                                               in0=rbuf["ghot"].t[:, :, g_:g_ + 1].to_broadcast([128, NT, 8]),
                                                               in1=rbuf["ge"].t[:], op=ALU.mult),
                              R=[rbuf["ghot"].tr, rbuf["ge"].tr], W=[gate.tr] if g_ == 0 else (), Wa=[gate.tr] if g_ else ())
                    kb.barrier()
                STOP("P5")
                with contextlib.ExitStack() as es6:
                    wgs = [kb.sbuf("wg%d" % i, [128, 8, 256], BF16, es6) for i in range(2)]
                    wus = [kb.sbuf("wu%d" % i, [128, 8, 256], BF16, es6) for i in range(2)]
                    wds = [kb.sbuf("wd%d" % i, [128, 2, D], BF16, es6) for i in range(2)]
                    sgs = [kb.sbuf("sg%d" % i, [128, 512], F32, es6) for i in range(2)]
                    aTs = [kb.sbuf("aT%d" % i, [128, 512], BF16, es6) for i in range(4)]
                    def w_issue(e_):
                        g_, ee = divmod(e_, 8)
                        wg, wu, wd = wgs[e_ % 2], wus[e_ % 2], wds[e_ % 2]
                        kb.dma("pool", wg.t[:], W["expert_w_gate"][g_, ee].rearrange("(k p) f -> p k f", p=128), R=[Wtr["expert_w_gate"]], W=[wg.tr])
                        kb.dma("pool", wu.t[:], W["expert_w_up"][g_, ee].rearrange("(k p) f -> p k f", p=128), R=[Wtr["expert_w_up"]], W=[wu.tr])
                        kb.dma("pool", wd.t[:], W["expert_w_down"][g_, ee].rearrange("(k p) d -> p k d", p=128), R=[Wtr["expert_w_down"]], W=[wd.tr])

                    mits = [(e_, c) for e_ in range(32) for c in range(4)]

                    def gate_up(i):
                        e_, c = mits[i]
                        wg, wu = wgs[e_ % 2], wus[e_ % 2]
                        x1tr = [x1T.k(4 * c + j) for j in range(4)]
                        for fc in range(2):
                            hg, hgt = psb(0, fc)
                            hu, hut = psb(1, fc)
                            for kc in range(8):
                                mm(hg[:, 0:512], wg.t[:, kc, fc * 128:(fc + 1) * 128], x1T.t[:, kc, c * 512:(c + 1) * 512], kc == 0, kc == 7,
                                   [wg.tr] + x1tr, hgt, kc == 0, inc=(kc == 7))
                            for kc in range(8):
                                mm(hu[:, 0:512], wu.t[:, kc, fc * 128:(fc + 1) * 128], x1T.t[:, kc, c * 512:(c + 1) * 512], kc == 0, kc == 7,
                                   [wu.tr] + x1tr, hut, kc == 0, inc=(kc == 7))
                            sgb = sgs[fc]
                            aT = aTs[(i % 2) * 2 + fc]
                            kb.op("act", lambda e: e.activation(out=sgb.t[:], in_=hg[:, 0:512], func=AF.Silu), R=[hgt], W=[sgb.tr])
                            kb.op("dve", lambda e: e.tensor_tensor(out=aT.t[:], in0=sgb.t[:], in1=hu[:, 0:512], op=ALU.mult),
                                  R=[sgb.tr, hut], W=[aT.tr])

                    def down(i):
                        e_, c = mits[i]
                        wd = wds[e_ % 2]
                        for j in range(4):
                            t = 4 * c + j
                            for half in range(2):
                                py, pyt = psb(2 + j % 2, half)
                                for fc in range(2):
                                    aT = aTs[(i % 2) * 2 + fc]
                                    mm(py[:, 0:512], aT.t[:, j * 128:(j + 1) * 128], wd.t[:, fc, half * 512:(half + 1) * 512], fc == 0, fc == 1,
                                       [aT.tr, wd.tr], pyt, fc == 0, inc=(fc == 1))
                                kb.op("dve", lambda e: e.scalar_tensor_tensor(
                                    out=yacc.t[:, t, half * 512:(half + 1) * 512], in0=py[:, 0:512], scalar=gate.t[:, t, e_:e_ + 1],
                                    in1=yacc.t[:, t, half * 512:(half + 1) * 512], op0=ALU.mult, op1=ALU.add),
                                    R=[pyt, gate.tr, yacc.k(t)], W=[yacc.k(t)])

                    w_issue(0)
                    w_issue(1)
                    gate_up(0)
                    for i in range(len(mits)):
                        if i + 1 < len(mits):
                            gate_up(i + 1)
                        down(i)
                        e_, c = mits[i]
                        if c == 3 and e_ + 2 < 32:
                            w_issue(e_ + 2)
                    outs = [kb.sbuf("ob%d" % i, [128, D], F32, es6) for i in range(2)]
                    for t in range(NT):
                        ob = outs[t % 2]
                        layer_norm(yacc.t[:, t, :], [yacc.k(t)], 2, ob.t[:], [ob.tr])
                        kb.dma("sp", x_dst.t[s, t * 128:(t + 1) * 128, :], ob.t[:], R=[ob.tr], Wa=[x_dst.k(s)])
                    kb.barrier()
            es_wo.close()
            if li == nl - 1:
                out_tracks.append(y_out.k(s))

    try:
        body()
    except _Stop:
        pass
    kb.finish(out_tracks)
    return kb, tapd


def _wshapes(inputs):
    return {n: list(inputs[n].shape) for n in WNAMES}


def kernel(**inputs):
    x = np.ascontiguousarray(inputs["x"], dtype=np.float32)
    pos = np.ascontiguousarray(inputs["positions"], dtype=np.int32)
    ncores = 8
    nseq = x.shape[0] // ncores
    kb, _ = build(nseq, list(range(DEPTH)), _wshapes(inputs))
    hc = host_consts()
    in_maps = []
    for c in range(ncores):
        m = {"x": x[c * nseq:(c + 1) * nseq], "positions": pos[c * nseq:(c + 1) * nseq]}
        for n in WNAMES:
            m[n] = np.ascontiguousarray(inputs[n], dtype=np.float32)
        for n, v in hc.items():
            m["c_" + n] = v
        in_maps.append(m)
    res = run_bass_kernel_spmd(kb.nc, in_maps, core_ids=list(range(ncores)))
    return np.concatenate([r["y"] for r in res.results], axis=0)
```
